# Optimizing a Trainium2 kernel written in Bass

```python
import math, functools
import jax, jax.numpy as jnp
from jax import lax
import numpy as np

D_MODEL = 1024
BATCH = 8
SEQ = 2048
DEPTH = 1
DEC_BATCH = 32
DEC_SEQ = 8
PAST_LEN = 8192
PAGE_SIZE = 128

RET_HEADS = 4
RET_DK = 64
RET_DV = 128
RET_CHUNK = 128
MOBA_HEADS = 4
MOBA_HEAD_DIM = 128
MOBA_BLOCK = 256
MOBA_TOPK = 3
Q_BLOCK = 128
D_FF = 4 * D_MODEL
ROPE_THETA = 10000.0
LN_EPS = 1e-5
GN_EPS = 1e-6
ALPHA = (2 * DEPTH) ** 0.25
BETA = (8 * DEPTH) ** -0.25
RET_WIDTH = RET_HEADS * RET_DV
MOBA_WIDTH = MOBA_HEADS * MOBA_HEAD_DIM
MIX_WIDTH = RET_WIDTH + MOBA_WIDTH
SPLIT_SIZES = (RET_HEADS * RET_DK, RET_HEADS * RET_DK, RET_WIDTH, RET_WIDTH,
               MOBA_WIDTH, MOBA_WIDTH, MOBA_WIDTH)
IN_WIDTH = sum(SPLIT_SIZES)

kernel_name = "hymba_retention_moba_adaln_deepnorm_step"


def layer_norm(x, g, b):
    xf = x.astype(jnp.float32)
    mu = jnp.mean(xf, axis=-1, keepdims=True)
    var = jnp.mean(jnp.square(xf - mu), axis=-1, keepdims=True)
    y = (xf - mu) * lax.rsqrt(var + LN_EPS) * g.astype(jnp.float32) + b.astype(jnp.float32)
    return y.astype(x.dtype)


def rope(x, pos):
    half = x.shape[-1] // 2
    inv_freq = jnp.power(ROPE_THETA, -jnp.arange(half, dtype=jnp.float32) / half)
    ang = pos.astype(jnp.float32)[:, None] * inv_freq[None, :]
    cos = jnp.cos(ang)[None, :, None, :]
    sin = jnp.sin(ang)[None, :, None, :]
    xf = x.astype(jnp.float32)
    x1, x2 = xf[..., :half], xf[..., half:]
    return jnp.concatenate([x1 * cos - x2 * sin, x2 * cos + x1 * sin], axis=-1).astype(x.dtype)


def adaln(c, w, b):
    m = jax.nn.silu(c) @ w + b
    return jnp.split(m[:, None, :], 6, axis=-1)


def project(h, w_in, pos):
    bsz, s, _ = h.shape
    z = h @ w_in
    idx = [int(i) for i in np.cumsum(SPLIT_SIZES)[:-1]]
    rq, rk, rv, rg, mq, mk, mv = jnp.split(z, idx, axis=-1)
    rq = rope(rq.reshape(bsz, s, RET_HEADS, RET_DK), pos)
    rk = rope(rk.reshape(bsz, s, RET_HEADS, RET_DK), pos) * (RET_DK ** -0.5)
    rv = rv.reshape(bsz, s, RET_HEADS, RET_DV)
    rg = rg.reshape(bsz, s, RET_HEADS, RET_DV)
    mq = rope(mq.reshape(bsz, s, MOBA_HEADS, MOBA_HEAD_DIM), pos)
    mk = rope(mk.reshape(bsz, s, MOBA_HEADS, MOBA_HEAD_DIM), pos)
    mv = mv.reshape(bsz, s, MOBA_HEADS, MOBA_HEAD_DIM)
    return rq, rk, rv, rg, mq, mk, mv


def retention_log_decay():
    return jnp.log1p(-jnp.exp2(-5.0 - jnp.arange(RET_HEADS, dtype=jnp.float32)))


def retention_chunk(state, q, k, v, log_g):
    q = q.astype(jnp.float32)
    k = k.astype(jnp.float32)
    v = v.astype(jnp.float32)
    state = state.astype(jnp.float32)
    c = q.shape[1]
    i = jnp.arange(c, dtype=jnp.float32)
    diff = i[:, None] - i[None, :]
    causal = diff >= 0
    dmat = jnp.where(causal[None], jnp.exp(jnp.where(causal, diff, 0.0)[None] * log_g[:, None, None]), 0.0)
    scores = jnp.einsum("bihd,bjhd->bhij", q, k) * dmat[None]
    o_inner = jnp.einsum("bhij,bjhe->bihe", scores, v)
    q_decay = jnp.exp((i[:, None] + 1.0) * log_g[None, :])
    o_cross = jnp.einsum("bihd,bhde->bihe", q * q_decay[None, :, :, None], state)
    k_decay = jnp.exp((c - 1.0 - i)[:, None] * log_g[None, :])
    new_state = (jnp.exp(c * log_g)[None, :, None, None] * state
                 + jnp.einsum("bjhd,bjhe->bhde", k * k_decay[None, :, :, None], v))
    return o_inner + o_cross, new_state


def retention_prompt(q, k, v, log_g):
    bsz, s, h, dk = q.shape
    n_chunks = s // RET_CHUNK

    def to_chunks(t):
        return t.reshape(bsz, n_chunks, RET_CHUNK, h, t.shape[-1]).swapaxes(0, 1)

    def step(state, qkv):
        o, new_state = retention_chunk(state, qkv[0], qkv[1], qkv[2], log_g)
        return new_state, o

    state0 = jnp.zeros((bsz, h, dk, RET_DV), jnp.float32)
    state, o = lax.scan(step, state0, (to_chunks(q), to_chunks(k), to_chunks(v)))
    return o.swapaxes(0, 1).reshape(bsz, s, h, RET_DV), state


def retention_readout(o, g, dtype):
    bsz, s = o.shape[0], o.shape[1]
    mu = jnp.mean(o, axis=-1, keepdims=True)
    var = jnp.mean(jnp.square(o - mu), axis=-1, keepdims=True)
    y = (o - mu) * lax.rsqrt(var + GN_EPS) * jax.nn.silu(g.astype(jnp.float32))
    return y.reshape(bsz, s, RET_WIDTH).astype(dtype)


def moba_blocks(k, v):
    bsz, length, h, d = k.shape
    n_blocks = -(-length // MOBA_BLOCK)
    pad = n_blocks * MOBA_BLOCK - length

    def blockify(t):
        t = jnp.pad(t, ((0, 0), (0, pad), (0, 0), (0, 0)))
        return t.reshape(bsz, n_blocks, MOBA_BLOCK, h, d).transpose(0, 3, 1, 2, 4)

    kb = blockify(k)
    vb = blockify(v)
    kmean = jnp.mean(kb.astype(jnp.float32), axis=3)
    return kb, vb, kmean


def moba_attend(q, q_pos, kb, vb, kmean):
    bsz, nq, h, d = q.shape
    n_blocks = kb.shape[2]
    n_top = min(MOBA_TOPK, n_blocks)
    own = q_pos // MOBA_BLOCK
    gate = jnp.einsum("bqhd,bhnd->bqhn", q.astype(jnp.float32), kmean)
    past_ok = jnp.arange(n_blocks, dtype=jnp.int32)[None, :] < own[:, None]
    gate = jnp.where(past_ok[None, :, None, :], gate, -jnp.inf)
    _, top = lax.top_k(gate, n_top)
    top_valid = top < own[None, :, None, None]
    own_idx = jnp.broadcast_to(own[None, :, None, None], (bsz, nq, h, 1)).astype(top.dtype)
    idx = jnp.concatenate([top, own_idx], axis=-1)
    valid = jnp.concatenate([top_valid, jnp.ones((bsz, nq, h, 1), bool)], axis=-1)
    bi = jnp.arange(bsz)[:, None, None, None]
    hi = jnp.arange(h)[None, None, :, None]
    ksel = kb[bi, hi, idx]
    vsel = vb[bi, hi, idx]
    key_pos = idx[..., None] * MOBA_BLOCK + jnp.arange(MOBA_BLOCK, dtype=idx.dtype)
    mask = valid[..., None] & (key_pos <= q_pos[None, :, None, None, None])
    logits = jnp.einsum("bqhd,bqhsjd->bqhsj", q, ksel, preferred_element_type=jnp.float32) * (d ** -0.5)
    logits = jnp.where(mask, logits, -jnp.inf).reshape(bsz, nq, h, -1)
    p = jax.nn.softmax(logits, axis=-1).reshape(mask.shape)
    out = jnp.einsum("bqhsj,bqhsjd->bqhd", p.astype(vsel.dtype), vsel, preferred_element_type=jnp.float32)
    return out.astype(q.dtype)


def moba_prompt(q, k, v):
    bsz, s, h, d = q.shape
    kb, vb, kmean = moba_blocks(k, v)
    n_qb = s // Q_BLOCK
    qb = q.reshape(bsz, n_qb, Q_BLOCK, h, d).swapaxes(0, 1)
    posb = jnp.arange(s, dtype=jnp.int32).reshape(n_qb, Q_BLOCK)
    out = lax.map(lambda a: moba_attend(a[0], a[1], kb, vb, kmean), (qb, posb))
    return out.swapaxes(0, 1).reshape(bsz, s, h, d)


def moba_sample(q, k_new, v_new, cache_k, cache_v, page_table):
    dbs, t, h, d = q.shape
    past_len = page_table.shape[1] * PAGE_SIZE
    k_past = cache_k[page_table].reshape(dbs, past_len, h, d)
    v_past = cache_v[page_table].reshape(dbs, past_len, h, d)
    k_all = jnp.concatenate([k_past, k_new.astype(k_past.dtype)], axis=1)
    v_all = jnp.concatenate([v_past, v_new.astype(v_past.dtype)], axis=1)
    kb, vb, kmean = moba_blocks(k_all, v_all)
    q_pos = past_len + jnp.arange(t, dtype=jnp.int32)
    return moba_attend(q, q_pos, kb, vb, kmean)


def mixer_sublayer(x, shift, scale, gate, pos, w_in, w_o, ln_g, ln_b, log_g, retention_fn, moba_fn):
    bsz, s, _ = x.shape
    h = x * (1.0 + scale) + shift
    rq, rk, rv, rg, mq, mk, mv = project(h, w_in, pos)
    o_ret, ret_state = retention_fn(rq, rk, rv, log_g)
    o_moba = moba_fn(mq, mk, mv)
    mixed = jnp.concatenate([retention_readout(o_ret, rg, x.dtype),
                             o_moba.reshape(bsz, s, MOBA_WIDTH)], axis=-1) @ w_o
    x = layer_norm(ALPHA * x + gate * mixed, ln_g, ln_b)
    return x, mk, mv, ret_state


def ffn_sublayer(x, shift, scale, gate, w_up, w_down, ln_g, ln_b):
    h = x * (1.0 + scale) + shift
    u = jnp.square(jax.nn.relu(h @ w_up))
    return layer_norm(ALPHA * x + gate * (u @ w_down), ln_g, ln_b)


def setup_inputs(seed: int = 0) -> dict:
    key = jax.random.key(seed)
    ks = jax.random.split(key, 20)
    n_pages = PAST_LEN // PAGE_SIZE
    n_used = DEC_BATCH * n_pages
    n_pool = n_used + (n_used + 3) // 4
    f32 = jnp.float32
    x_prompt = jax.random.normal(ks[0], (BATCH, SEQ, D_MODEL), f32)
    x_sample = jax.random.normal(ks[1], (DEC_BATCH, DEC_SEQ, D_MODEL), f32)
    cache_k = jax.random.normal(ks[2], (DEPTH, n_pool, PAGE_SIZE, MOBA_HEADS, MOBA_HEAD_DIM), f32)
    cache_v = jax.random.normal(ks[3], (DEPTH, n_pool, PAGE_SIZE, MOBA_HEADS, MOBA_HEAD_DIM), f32)
    state_ret = jax.random.normal(ks[4], (DEPTH, DEC_BATCH, RET_HEADS, RET_DK, RET_DV), f32)
    page_table = jax.random.permutation(ks[5], n_pool)[:n_used].reshape(DEC_BATCH, n_pages).astype(jnp.int32)
    c_prompt = jax.random.normal(ks[6], (BATCH, D_MODEL), f32)
    c_sample = jax.random.normal(ks[7], (DEC_BATCH, D_MODEL), f32)
    w_ada = jax.random.normal(ks[8], (DEPTH, D_MODEL, 6 * D_MODEL), f32) * D_MODEL ** -0.5
    b_ada = 0.02 * jax.random.normal(ks[9], (DEPTH, 6 * D_MODEL), f32)
    col_scale = jnp.concatenate([
        jnp.ones((2 * RET_HEADS * RET_DK,), f32), jnp.full((RET_WIDTH,), BETA, f32),
        jnp.ones((RET_WIDTH + 2 * MOBA_WIDTH,), f32), jnp.full((MOBA_WIDTH,), BETA, f32)])
    w_in = jax.random.normal(ks[10], (DEPTH, D_MODEL, IN_WIDTH), f32) * D_MODEL ** -0.5 * col_scale
    w_o = jax.random.normal(ks[11], (DEPTH, MIX_WIDTH, D_MODEL), f32) * MIX_WIDTH ** -0.5 * BETA
    ln1_g = 1.0 + 0.02 * jax.random.normal(ks[12], (DEPTH, D_MODEL), f32)
    ln1_b = 0.02 * jax.random.normal(ks[13], (DEPTH, D_MODEL), f32)
    w_up = jax.random.normal(ks[14], (DEPTH, D_MODEL, D_FF), f32) * D_MODEL ** -0.5
    w_down = jax.random.normal(ks[15], (DEPTH, D_FF, D_MODEL), f32) * D_FF ** -0.5 * BETA
    ln2_g = 1.0 + 0.02 * jax.random.normal(ks[16], (DEPTH, D_MODEL), f32)
    ln2_b = 0.02 * jax.random.normal(ks[17], (DEPTH, D_MODEL), f32)
    return {"x_prompt": x_prompt, "x_sample": x_sample, "cache_k": cache_k, "cache_v": cache_v,
            "state_ret": state_ret, "page_table": page_table, "c_prompt": c_prompt,
            "c_sample": c_sample, "w_ada": w_ada, "b_ada": b_ada, "w_in": w_in, "w_o": w_o,
            "ln1_g": ln1_g, "ln1_b": ln1_b, "w_up": w_up, "w_down": w_down,
            "ln2_g": ln2_g, "ln2_b": ln2_b}


def reference(x_prompt, x_sample, cache_k, cache_v, state_ret, page_table, c_prompt, c_sample,
              w_ada, b_ada, w_in, w_o, ln1_g, ln1_b, w_up, w_down, ln2_g, ln2_b):
    log_g = retention_log_decay()
    pos_p = jnp.arange(x_prompt.shape[1], dtype=jnp.int32)
    pos_s = page_table.shape[1] * PAGE_SIZE + jnp.arange(x_sample.shape[1], dtype=jnp.int32)
    xp, xs = x_prompt, x_sample
    k_p, v_p, s_p, k_s, v_s, s_s = [], [], [], [], [], []
    for layer in range(DEPTH):
        mod_p = adaln(c_prompt, w_ada[layer], b_ada[layer])
        mod_s = adaln(c_sample, w_ada[layer], b_ada[layer])
        xp, kp, vp, sp = mixer_sublayer(
            xp, mod_p[0], mod_p[1], mod_p[2], pos_p, w_in[layer], w_o[layer],
            ln1_g[layer], ln1_b[layer], log_g, retention_prompt, moba_prompt)
        st_l, ck_l, cv_l = state_ret[layer], cache_k[layer], cache_v[layer]
        xs, ksn, vsn, ssn = mixer_sublayer(
            xs, mod_s[0], mod_s[1], mod_s[2], pos_s, w_in[layer], w_o[layer],
            ln1_g[layer], ln1_b[layer], log_g,
            lambda q, k, v, lg: retention_chunk(st_l, q, k, v, lg),
            lambda q, k, v: moba_sample(q, k, v, ck_l, cv_l, page_table))
        xp = ffn_sublayer(xp, mod_p[3], mod_p[4], mod_p[5], w_up[layer], w_down[layer], ln2_g[layer], ln2_b[layer])
        xs = ffn_sublayer(xs, mod_s[3], mod_s[4], mod_s[5], w_up[layer], w_down[layer], ln2_g[layer], ln2_b[layer])
        k_p.append(kp)
        v_p.append(vp)
        s_p.append(sp)
        k_s.append(ksn)
        v_s.append(vsn)
        s_s.append(ssn)
    return (xp, xs, jnp.stack(k_p), jnp.stack(v_p), jnp.stack(s_p), jnp.stack(k_s), jnp.stack(v_s), jnp.stack(s_s))
```

```python
import math
import numpy as np
import concourse.bass as bass
import concourse.mybir as mybir
from concourse.bass_utils import run_bass_kernel_spmd

F32 = mybir.dt.float32
BF16 = mybir.dt.bfloat16
I32 = mybir.dt.int32
AF = mybir.ActivationFunctionType
ALU = mybir.AluOpType
AX = mybir.AxisListType

NT = 16
ALPHA = 2.0 ** 0.25
LN_EPS = 1e-5
GN_EPS = 1e-6
NEG = -30000.0
SCALE = 128.0 ** -0.5
GAM = [1.0 - 2.0 ** (-5.0 - h) for h in range(4)]


class Sched:
    ENG = ("pe", "act", "dve", "pool", "sp")

    def __init__(self, n_dma):
        self.ops = {e: [] for e in self.ENG}
        self.cnt = {e: 0 for e in self.ENG}
        self.lastw = {}
        self.readers = {}
        self.n_dma = n_dma
        self.dma_val = [0] * n_dma
        self.dma_next = 0
        self.seq = 0
        self.cut = None
        self.marks = []
        self.alias = {"ta": ["xnA"], "tb": ["xnB"], "sq": ["xnA", "xnB"], "xn": ["xnA", "xnB"],
                      "yn": ["rA"], "yret": ["rB"], "r": ["rA", "rB"], "sg": ["x1A"], "x1": ["x1A", "x1B"],
                      "rl0": ["z0"], "rl1": ["z1"], "uT": ["Pm", "PT0", "PT1"], "mkr1": ["rB"], "mkr0": ["rB"],
                      "mqr": ["rA"], "scTb": ["Pm"],
                      "KT": ["KT", "Kg0", "Kg1", "KTg0", "KTg1"],
                      "Vb": ["Vb", "Vg0", "Vg1", "PTs", "selBs", "Dm"]}

    def _exp(self, names):
        out = []
        for n in names:
            out += self.alias.get(n, [n])
        return out

    def _deps(self, reads, writes):
        reads, writes = self._exp(reads), self._exp(writes)
        toks = []
        for r in reads:
            if r in self.lastw:
                toks.append(self.lastw[r])
        for w in writes:
            if w in self.lastw:
                toks.append(self.lastw[w])
            toks += self.readers.get(w, [])
        return toks

    def _commit(self, tok, reads, writes):
        reads, writes = self._exp(reads), self._exp(writes)
        for r in reads:
            self.readers.setdefault(r, []).append(tok)
        for w in writes:
            self.lastw[w] = tok
            self.readers[w] = []

    def op(self, eng, fn, reads=(), writes=()):
        writes = list(writes) + [r for r in reads if r.startswith("ps")]
        toks = self._deps(reads, writes)
        self.cnt[eng] += 1
        tok = (eng, self.cnt[eng])
        self.seq += 1
        self.ops[eng].append((fn, toks, tok, self.seq))
        self._commit(tok, reads, writes)

    def dma(self, q, fn, reads=(), writes=()):
        toks = self._deps(reads, writes)
        i = self.dma_next
        self.dma_next = (i + 1) % self.n_dma
        if self.dma_val[i] > 0:
            toks.append(("d%d" % i, self.dma_val[i]))
        self.dma_val[i] += 16
        tok = ("d%d" % i, self.dma_val[i])
        self.seq += 1
        self.ops[q].append((fn, toks, tok, self.seq))
        self._commit(tok, reads, writes)

    def emit(self, eng_name, e, sems, final_wait=False):
        seen = {}
        for fn, toks, tok, seq in self.ops[eng_name]:
            if self.cut is not None and seq > self.cut:
                continue
            need = {}
            for k, v in toks:
                if k == eng_name and eng_name in ("pe", "sp"):
                    continue
                if v > need.get(k, 0):
                    need[k] = v
            for k, v in need.items():
                if seen.get(k, 0) >= v:
                    continue
                e.wait_ge(sems[k], v)
                seen[k] = v
            ins = fn(e)
            ins.then_inc(sems[tok[0]], 16 if tok[0][1:].isdigit() else 1)
        if final_wait:
            fin = {}
            for en in self.ENG:
                for fn, toks, tok, seq in self.ops[en]:
                    if self.cut is not None and seq > self.cut:
                        continue
                    if tok[0][1:].isdigit():
                        fin[tok[0]] = max(fin.get(tok[0], 0), tok[1])
            for k, v in fin.items():
                e.wait_ge(sems[k], v)


def build_nc(cut=None, marks=None):
    nc = bass.Bass("TRN2", target_bir_lowering=False)

    def din(name, shape, dt=F32):
        return nc.dram_tensor(name, shape, dt, kind="ExternalInput").ap()

    def dout(name, shape, dt=F32):
        return nc.dram_tensor(name, shape, dt, kind="ExternalOutput").ap()

    x = din("x", [2048, 1024])
    cT = din("cT", [128, 8])
    w_ada = din("w_ada", [1024, 6144])
    badaT = din("badaT", [128, 48])
    bga = din("bga", [128, 1024])
    bgf = din("bgf", [128, 1024])
    w_in = din("w_in", [1024, 3072])
    w_o = din("w_o", [1024, 1024])
    w_up = din("w_up", [1024, 4096])
    w_down = din("w_down", [4096, 1024])
    l1g = din("l1g", [128, 1024])
    l1b = din("l1b", [128, 1024])
    l2g = din("l2g", [128, 1024])
    l2b = din("l2b", [128, 1024])
    ident_d = din("ident", [128, 128])
    ropeM = din("ropeM", [2048, 192])
    ropeR = din("ropeR", [2048, 96])
    dqk_d = din("dqk", [128, 8])
    triT_d = din("triT", [128, 128])
    tribias_d = din("tribias", [128, 128])
    y = dout("y", [2048, 1024])
    kout = dout("kout", [2048, 512])
    vout = dout("vout", [2048, 512])
    sout = dout("sout", [256, 128])
    x1d = nc.dram_tensor("x1d", [2080, 1024], F32).ap()
    gfd = nc.dram_tensor("gfd", [128, 1024], F32).ap()
    modsd = nc.dram_tensor("modsd", [32, 6144], F32).ap()
    xs_d = din("xs", [32, 1024])
    csT_d = din("csT", [128, 256])
    bada32 = din("bada32", [32, 6144])
    ptb_d = din("ptb", [128, 256], I32)
    pcol_d = din("pcol", [128, 1])
    st_in = din("st_in", [4, 256, 128])
    ck = din("ck", [327680, 512])
    cv = din("cv", [327680, 512])
    ropeS_d = din("ropeS", [32, 288])
    dqks_d = din("dqks", [32, 8])
    maskS_d = din("maskS", [32, 32])
    cm_d = din("cm", [128, 128])
    rm_d = din("rm", [32, 4])
    gtab_d = din("gtab", [128, 2])
    cmaskN_d = din("cmaskN", [32, 128])
    ys = dout("ys", [32, 1024])
    ks = dout("ks", [32, 512])
    vs = dout("vs", [32, 512])
    ss = dout("ss", [4, 256, 128])

    def sb(name, shape, dt=F32):
        return nc.alloc_sbuf_tensor(name, shape, dt)

    W = sb("W", [128, 65536], BF16)
    w_in_b = W[:, 0:24576].rearrange("p (k n) -> p k n", k=8)
    w_o_b = W[:, 24576:32768].rearrange("p (k n) -> p k n", k=8)
    KT = W[:, 32768:40960].rearrange("p (h n) -> p h n", h=4)
    Vb = W[:, 40960:49152].rearrange("p (t n) -> p t n", t=16)
    wada = [W[:, 49152 + i * 4096:49152 + (i + 1) * 4096].rearrange("p (k n) -> p k n", k=8) for i in range(2)]
    w_up_b = W[:, 0:32768].rearrange("p (k n) -> p k n", k=8)
    w_down_b = W[:, 32768:65536].rearrange("p (k n) -> p k n", k=32)

    ident = sb("ident_s", [128, 128])
    identb = sb("identb", [128, 128], BF16)
    triT = sb("triT_s", [128, 128])
    tribias = sb("tribias_s", [128, 128])
    dqk = sb("dqk_s", [128, 8])
    cTs = sb("cTs", [128, 8])
    scT = sb("scT", [128, 8], BF16)
    badaTs = sb("badaTs", [128, 48])
    modT = sb("modT", [128, 48])
    gate_a = sb("gate_a", [128, 1024])
    lng = sb("lng", [128, 1024])
    lnb = sb("lnb", [128, 1024])
    xt = sb("xt", [128, 1024])
    hT = sb("hT", [128, 8, 128], BF16)
    z = sb("z", [128, 3072])
    Bar = sb("Bar", [128, 4096], BF16)
    rtab = [sb("rtab%d" % i, [128, 288]) for i in range(2)]
    qr = sb("qr", [128, 256])
    kr = sb("kr", [128, 256])
    krb = sb("krb", [128, 256], BF16)
    rvb = sb("rvb", [128, 512], BF16)
    qT = sb("qT", [128, 2, 128], BF16)
    kT = sb("kT", [128, 2, 128], BF16)
    mqT = sb("mqT", [128, 4, 128], BF16)
    kmT = sb("kmT", [128, 4, 16])
    kmsum = sb("kmsum", [128, 4, 8])
    kmb = sb("kmb", [128, 4, 8], BF16)
    st32 = sb("st32", [128, 2, 128])
    sttmp = sb("sttmp", [128, 2, 128])
    stb = sb("stb", [128, 2, 128], BF16)
    scTm = [sb("scTm%d" % i, [128, 128], BF16) for i in range(2)]
    mixT = sb("mixT", [128, 8, 128], BF16)
    small = sb("small", [128, 64])
    g8 = sb("g8", [128, 8])
    mx8 = sb("mx8", [128, 8])
    biasn = sb("biasn", [128, 8])
    rs = sb("rs", [128, 16])
    Pm = Bar[:, 0:2048]
    sd = sb("sd", [128, 128])
    PT = [Bar[:, 2048 + i * 1024:3072 + i * 1024].rearrange("p (a b) -> p a b", a=8) for i in range(2)]
    r = sb("r", [128, 1024])
    xn = sb("xn", [128, 1024])
    x1 = sb("x1", [128, 1024])
    ta = xn[:, 0:512]
    tb = xn[:, 512:1024]
    sq = xn
    yn = r[:, 0:512]
    yret = r[:, 512:1024]
    sg = x1[:, 0:512]
    rl = [z[:, i * 512:(i + 1) * 512] for i in range(2)]
    uT = Bar[:, :].rearrange("p (a b) -> p a b", a=32)
    scTb = Bar[:, 0:1024].rearrange("p (a b) -> p a b", a=8)
    mqr = r[:, 0:512]
    mkr = [r[:, 512:1024]] * 2
    Kg = [W[:, 32768 + i * 2048:32768 + (i + 1) * 2048].rearrange("p (a b) -> p a b", a=4) for i in range(2)]
    KTg = [W[:, 36864 + i * 2048:36864 + (i + 1) * 2048].rearrange("p (a b) -> p a b", a=16) for i in range(2)]
    Vg = [W[:, 40960 + i * 2048:40960 + (i + 1) * 2048].rearrange("p (a b) -> p a b", a=4) for i in range(2)]
    PTs = W[:, 45056:47104].rearrange("p (a b) -> p a b", a=64)
    selBs = W[:, 47104:48128].rearrange("p (a b) -> p a b", a=32)
    Dm = W[:, 48128:49152]
    sts32 = W[:, 57344:59392].bitcast(F32).rearrange("p (s a e) -> p s a e", s=4, a=2)
    Sall = [W[:, 59392 + i * 2048:59392 + (i + 1) * 2048].bitcast(F32).rearrange("p (a b) -> p a b", a=32)
            for i in range(2)]
    stsb = W[:, 63488:64512].rearrange("p (s a e) -> p s a e", s=4, a=2)
    krbm = W[:, 64512:65536].rearrange("p (s n) -> p s n", s=4)
    TAIL = ["sts32", "Sall0", "Sall1", "stsb", "krbm"]
    csTs = sb("csTs", [128, 256])
    scTs = sb("scTs", [128, 8, 32], BF16)
    ptb_s = sb("ptb_s", [128, 256], I32)
    idx = sb("idx", [128, 256], I32)
    pcol_s = sb("pcol_s", [128, 1])
    dqks = sb("dqks_s", [32, 8])
    maskS = sb("maskS_s", [32, 32])
    cm = sb("cm_s", [128, 4, 32])
    rm = sb("rm_s", [32, 4])
    gtab = sb("gtab_s", [128, 2])
    cmaskN = sb("cmaskN_s", [32, 4, 32])
    qTs = sb("qTs", [128, 2, 32], BF16)
    kTs = sb("kTs", [128, 2, 32], BF16)
    qTsm = sb("qTsm", [128, 2, 4, 32], BF16)
    mqTs = sb("mqTs", [128, 4, 32], BF16)
    mkTs = sb("mkTs", [128, 4, 32], BF16)
    vnb = sb("vnb", [32, 512], BF16)
    PN = sb("PN", [32, 32], BF16)
    En = sb("En", [32, 32])
    gsb = sb("gsb", [32, 64])
    g32 = sb("g32", [32, 32])
    mxs = sb("mxs", [32, 8])
    sel = sb("sel", [32, 32])
    rinv = sb("rinv", [128, 32])
    ones_f = sb("ones_f", [128, 1])
    ones_b = sb("ones_b", [128, 128], BF16)

    psf = [nc.alloc_psum_tensor("psf%d" % i, [128, 512], F32) for i in range(6)]
    psb = [nc.alloc_psum_tensor("psb%d" % i, [128, 1024], BF16) for i in range(2)]

    S = Sched(n_dma=20)
    S.cut = cut
    pc = [0, 0]

    def nps():
        i = pc[0] % 4
        pc[0] += 1
        return psf[i], "psf%d" % i

    def npsb():
        i = pc[1] % 2
        pc[1] += 1
        return psb[i], "psb%d" % i

    def bc(ap, shape):
        return ap.to_broadcast(shape)

    def ld(q, dst, src, res, reads=()):
        S.dma(q, lambda e, d=dst, s=src: e.dma_start(out=d, in_=s), reads=reads, writes=res)

    ld("sp", ident[:], ident_d, ["ident"])
    ld("sp", triT[:], triT_d, ["triT"])
    ld("sp", tribias[:], tribias_d, ["tribias"])
    ld("sp", dqk[:], dqk_d, ["dqk"])
    ld("sp", cTs[:], cT, ["cTs"])
    ld("sp", badaTs[:], badaT, ["badaTs"])
    ld("sp", gate_a[:], bga, ["gate_a"])
    ld("sp", xn[:], bgf, ["xn"])
    S.op("dve", lambda e: e.tensor_copy(identb[:], ident[:]), reads=["ident"], writes=["identb"])
    S.op("dve", lambda e: e.memset(st32[:], 0.0), writes=["st32"])
    S.op("dve", lambda e: e.memset(stb[:], 0.0), writes=["stb"])
    S.op("dve", lambda e: e.memset(kmsum[:], 0.0), writes=["kmsum"])
    S.op("dve", lambda e: e.memset(kmb[:], 0.0), writes=["kmb"])

    S.op("act", lambda e: e.activation(scT[:], cTs[:], AF.Silu), reads=["cTs"], writes=["scT"])
    S.op("dve", lambda e: e.tensor_copy(scTb[:], bc(scT[:].unsqueeze(2), [128, 8, 128])),
         reads=["scT"], writes=["scTb"])
    ld("sp", csTs[:], csT_d, ["csTs"])
    ld("sp", ptb_s[:], ptb_d, ["ptb_s"])
    ld("sp", pcol_s[:], pcol_d, ["pcol_s"])
    ld("sp", dqks[:], dqks_d, ["dqks"])
    ld("sp", maskS[:], maskS_d, ["maskS"])
    ld("sp", cm[:], cm_d.rearrange("p (a b) -> p a b", a=4), ["cm"])
    ld("sp", rm[:], rm_d, ["rm"])
    ld("sp", gtab[:], gtab_d, ["gtab"])
    ld("sp", cmaskN[:], cmaskN_d.rearrange("p (a b) -> p a b", a=4), ["cmaskN"])
    S.op("dve", lambda e: e.memset(ones_f[:], 1.0), writes=["ones_f"])
    S.op("dve", lambda e: e.memset(ones_b[:], 1.0), writes=["ones_b"])
    S.op("act", lambda e: e.activation(scTs[:], csTs[:].rearrange("p (k r) -> p k r", k=8), AF.Silu),
         reads=["csTs"], writes=["scTs"])
    S.op("dve", lambda e: e.tensor_scalar(idx[:], ptb_s[:], 128.0, pcol_s[:, 0:1], ALU.mult, ALU.add),
         reads=["ptb_s", "pcol_s"], writes=["idx"])
    w_ada_v = w_ada.rearrange("(k p) n -> p k n", p=128)
    psmod, psmod_r = psf[5], "psf5"
    for g in range(12):
        sl = g % 2
        ld("pool", wada[sl], w_ada_v[:, :, g * 512:(g + 1) * 512], ["wada%d" % sl])
        ps2, pr2 = nps()

        def f(e, ps2=ps2, sl=sl):
            for k in range(8):
                ins = e.matmul(ps2[0:32, :], scTs[:, k, :], wada[sl][:, k, :], start=(k == 0), stop=(k == 7))
            return ins
        S.op("pe", f, reads=["scTs", "wada%d" % sl], writes=[pr2])
        ld("sp", mqr[0:32, :], bada32[:, g * 512:(g + 1) * 512], ["mqr"])
        S.op("dve", lambda e, ps2=ps2: e.tensor_tensor(mkr[0][0:32, :], ps2[0:32, :], mqr[0:32, :], ALU.add),
             reads=[pr2, "mqr"], writes=["mkr0"])
        S.dma("sp", lambda e, g=g: e.dma_start(out=modsd[:, g * 512:(g + 1) * 512], in_=mkr[0][0:32, :]),
              reads=["mkr0"], writes=["modsd"])
        if g in (4, 5, 10, 11):
            ps, pr = nps()

            def f(e, ps=ps, sl=sl):
                for k in range(8):
                    ins = e.matmul(ps[:, :], scTb[:, k, :], wada[sl][:, k, :], start=(k == 0), stop=(k == 7))
                return ins
            S.op("pe", f, reads=["scTb", "wada%d" % sl], writes=[pr])
            dst = gate_a if g < 6 else xn
            half = g % 2
            S.op("dve", lambda e, ps=ps, dst=dst, half=half: e.tensor_tensor(
                dst[:, half * 512:(half + 1) * 512], ps[:, :], dst[:, half * 512:(half + 1) * 512], ALU.add),
                reads=[pr], writes=["gate_a" if g < 6 else "xn"])
        else:
            def f(e, sl=sl, g=g):
                for j in range(4):
                    col = g * 4 + j
                    for k in range(8):
                        ins = e.matmul(psmod[:, col:col + 1], wada[sl][:, k, j * 128:(j + 1) * 128],
                                       scT[:, k:k + 1], start=(k == 0), stop=(k == 7))
                return ins
            S.op("pe", f, reads=["scT", "wada%d" % sl], writes=[psmod_r])
    S.op("dve", lambda e: e.tensor_tensor(modT[:], psmod[:, 0:48], badaTs[:], ALU.add),
         reads=[psmod_r, "badaTs"], writes=["modT"])
    S.op("dve", lambda e: e.tensor_scalar_add(modT[:, 8:16], modT[:, 8:16], 1.0), reads=["modT"], writes=["modT"])
    S.op("dve", lambda e: e.tensor_scalar_add(modT[:, 32:40], modT[:, 32:40], 1.0), reads=["modT"], writes=["modT"])

    S.dma("sp", lambda e: e.dma_start(out=gfd, in_=xn[:]), reads=["xn"], writes=["gfd"])
    w_in_v = w_in.rearrange("(k p) n -> p k n", p=128)
    for k2 in range(4):
        ld("pool", w_in_b[:, 2 * k2:2 * k2 + 2, :], w_in_v[:, 2 * k2:2 * k2 + 2, :], ["w_in%d" % k2])
    WIN = ["w_in%d" % i for i in range(4)]
    ld("pool", w_o_b, w_o.rearrange("(k p) n -> p k n", p=128), ["w_o"])
    ld("sp", lng[:], l1g, ["lng"])
    ld("sp", lnb[:], l1b, ["lnb"])

    def transposes_to(dst_fn, src_fn, n, reads, writes_res, evac_eng="act", scale_fn=None):
        for b0 in range(0, n, 4):
            nb = min(4, n - b0)
            ps, pr = nps()

            def f(e, ps=ps, b0=b0, nb=nb):
                for j in range(nb):
                    ins = e.transpose(ps[:, j * 128:(j + 1) * 128], src_fn(b0 + j), ident[:])
                return ins
            S.op("pe", f, reads=list(reads) + ["ident"], writes=[pr])
            if scale_fn is None:
                if evac_eng == "act":
                    S.op("act", lambda e, ps=ps, b0=b0, nb=nb: e.copy(dst_fn(b0, nb), ps[:, 0:nb * 128].rearrange("p (a b) -> p a b", a=nb)),
                         reads=[pr], writes=writes_res)
                else:
                    S.op("dve", lambda e, ps=ps, b0=b0, nb=nb: e.tensor_copy(dst_fn(b0, nb), ps[:, 0:nb * 128].rearrange("p (a b) -> p a b", a=nb)),
                         reads=[pr], writes=writes_res)
            else:
                for j in range(nb):
                    sc, bi = scale_fn(b0 + j)
                    S.op("act", lambda e, ps=ps, j=j, b0=b0, sc=sc, bi=bi: e.activation(
                        dst_fn(b0 + j, 1), ps[:, j * 128:(j + 1) * 128], AF.Identity, bias=bi, scale=sc),
                        reads=[pr, "modT"], writes=writes_res)

    def layer_norm(src_ps_list, src_res, resid, resid_res, gate, gate_res, out_t, out_res, P=128):
        for hf in range(2):
            S.op("dve", lambda e, hf=hf: e.tensor_tensor(
                r[0:P, hf * 512:(hf + 1) * 512], src_ps_list[hf][0:P, :], gate[0:P, hf * 512:(hf + 1) * 512], ALU.mult),
                reads=[src_res[hf]] + list(gate_res), writes=["r"])
        S.op("dve", lambda e: e.scalar_tensor_tensor(r[0:P, :], resid[0:P, :], ALPHA, r[0:P, :], ALU.mult, ALU.add),
             reads=[resid_res, "r"], writes=["r"])
        S.op("dve", lambda e: e.reduce_sum(small[0:P, 0:1], r[0:P, :], AX.X), reads=["r"], writes=["sm0"])
        S.op("act", lambda e: e.activation(sq[0:P, :], r[0:P, :], AF.Square), reads=["r"], writes=["sq"])
        S.op("dve", lambda e: e.reduce_sum(small[0:P, 1:2], sq[0:P, :], AX.X), reads=["sq"], writes=["sm1"])
        S.op("dve", lambda e: e.tensor_scalar_mul(small[0:P, 2:3], small[0:P, 0:1], 1.0 / 1024), reads=["sm0"], writes=["sm2"])
        S.op("dve", lambda e: e.tensor_tensor(small[0:P, 3:4], small[0:P, 2:3], small[0:P, 2:3], ALU.mult),
             reads=["sm2"], writes=["sm3"])
        S.op("dve", lambda e: e.scalar_tensor_tensor(small[0:P, 4:5], small[0:P, 1:2], 1.0 / 1024, small[0:P, 3:4],
                                                     ALU.mult, ALU.subtract), reads=["sm1", "sm3"], writes=["sm4"])
        S.op("dve", lambda e: e.tensor_scalar_add(small[0:P, 4:5], small[0:P, 4:5], LN_EPS), reads=["sm4"], writes=["sm4"])
        S.op("act", lambda e: e.sqrt(small[0:P, 7:8], small[0:P, 4:5]), reads=["sm4"], writes=["sm7"])
        S.op("dve", lambda e: e.reciprocal(small[0:P, 5:6], small[0:P, 7:8]), reads=["sm7"], writes=["sm5"])
        S.op("dve", lambda e: e.scalar_tensor_tensor(small[0:P, 6:7], small[0:P, 2:3], -1.0, small[0:P, 5:6],
                                                     ALU.mult, ALU.mult), reads=["sm2", "sm5"], writes=["sm6"])
        S.op("act", lambda e: e.activation(xn[0:P, :], r[0:P, :], AF.Identity, bias=small[0:P, 6:7], scale=small[0:P, 5:6]),
             reads=["r", "sm5", "sm6"], writes=["xn"])
        S.op("pool", lambda e: e.tensor_tensor(xn[0:P, :], xn[0:P, :], lng[0:P, :], ALU.mult), reads=["xn", "lng"], writes=["xn"])
        S.op("pool", lambda e: e.tensor_tensor(out_t[0:P, :], xn[0:P, :], lnb[0:P, :], ALU.add), reads=["xn", "lnb"], writes=[out_res])

    def do_rope(P, rt, rtr, src, H, c0, dst, dec, zres, dres, dq_t, dq_r):
        n = 8 * H
        s4 = src.rearrange("p (h t j) -> p h t j", h=4, t=2)
        ta4 = ta[0:P, 0:n].rearrange("p (h t j) -> p h t j", h=4, t=2)
        tb4 = tb[0:P, 0:n].rearrange("p (h t j) -> p h t j", h=4, t=2)
        cosb = bc(rt[0:P, c0:c0 + H].unsqueeze(1).unsqueeze(1), [P, 4, 2, H])
        snb = bc(rt[0:P, c0 + H:c0 + 2 * H].unsqueeze(1), [P, 4, H])
        spb = bc(rt[0:P, c0 + 2 * H:c0 + 3 * H].unsqueeze(1), [P, 4, H])
        S.op("dve", lambda e: e.tensor_tensor(ta4, s4, cosb, ALU.mult), reads=[zres, rtr], writes=["ta"])
        S.op("dve", lambda e: e.tensor_tensor(tb4[:, :, 0, :], s4[:, :, 1, :], snb, ALU.mult),
             reads=[zres, rtr], writes=["tb"])
        S.op("dve", lambda e: e.tensor_tensor(tb4[:, :, 1, :], s4[:, :, 0, :], spb, ALU.mult),
             reads=[zres, rtr], writes=["tb"])
        if dec is None:
            S.op("dve", lambda e: e.tensor_tensor(dst, ta[0:P, 0:n], tb[0:P, 0:n], ALU.add),
                 reads=["ta", "tb"], writes=[dres])
        else:
            S.op("dve", lambda e: e.tensor_tensor(ta[0:P, 0:n], ta[0:P, 0:n], tb[0:P, 0:n], ALU.add),
                 reads=["ta", "tb"], writes=["ta"])
            decb = bc(dq_t[0:P, dec:dec + 4].unsqueeze(2), [P, 4, 2 * H])
            S.op("dve", lambda e: e.tensor_tensor(dst.rearrange("p (h j) -> p h j", h=4),
                                                  ta[0:P, 0:n].rearrange("p (h j) -> p h j", h=4), decb, ALU.mult),
                 reads=["ta", dq_r], writes=[dres])

    def group_norm(P, pso, pso_r):
        pso4 = pso[0:P, :].rearrange("p (h n) -> p h n", h=4)
        S.op("dve", lambda e: e.reduce_sum(small[0:P, 8:12], pso4, AX.X), reads=[pso_r], writes=["g0"])
        S.op("act", lambda e: e.activation(sq[0:P, 0:512], pso[0:P, :], AF.Square), reads=[pso_r], writes=["sq"])
        S.op("dve", lambda e: e.reduce_sum(small[0:P, 12:16], sq[0:P, 0:512].rearrange("p (h n) -> p h n", h=4), AX.X),
             reads=["sq"], writes=["g1"])
        S.op("dve", lambda e: e.tensor_scalar_mul(small[0:P, 16:20], small[0:P, 8:12], 1.0 / 128), reads=["g0"], writes=["g2"])
        S.op("dve", lambda e: e.tensor_tensor(small[0:P, 20:24], small[0:P, 16:20], small[0:P, 16:20], ALU.mult),
             reads=["g2"], writes=["g3"])
        S.op("dve", lambda e: e.scalar_tensor_tensor(small[0:P, 24:28], small[0:P, 12:16], 1.0 / 128, small[0:P, 20:24],
                                                     ALU.mult, ALU.subtract), reads=["g1", "g3"], writes=["g4"])
        S.op("dve", lambda e: e.tensor_scalar_add(small[0:P, 24:28], small[0:P, 24:28], GN_EPS), reads=["g4"], writes=["g4"])
        S.op("act", lambda e: e.sqrt(small[0:P, 36:40], small[0:P, 24:28]), reads=["g4"], writes=["g7"])
        S.op("dve", lambda e: e.reciprocal(small[0:P, 28:32], small[0:P, 36:40]), reads=["g7"], writes=["g5"])
        S.op("dve", lambda e: e.scalar_tensor_tensor(small[0:P, 32:36], small[0:P, 16:20], -1.0, small[0:P, 28:32],
                                                     ALU.mult, ALU.mult), reads=["g2", "g5"], writes=["g6"])
        for h in range(4):
            S.op("act", lambda e, h=h: e.activation(yn[0:P, h * 128:(h + 1) * 128], pso[0:P, h * 128:(h + 1) * 128],
                                                    AF.Identity, bias=small[0:P, 32 + h:33 + h],
                                                    scale=small[0:P, 28 + h:29 + h]),
                 reads=[pso_r, "g5", "g6"], writes=["yn"])
        S.op("dve", lambda e: e.tensor_tensor(yret[0:P, :], yn[0:P, :], sg[0:P, :], ALU.mult),
             reads=["yn", "sg"], writes=["yret"])

    S.marks.append((S.seq, 'sample mixer'))
    P = 32
    IOA = bass.IndirectOffsetOnAxis
    rtS, rtSr = rtab[1], "rtab1"
    ld("sp", xt[0:P, :], xs_d, ["xt"])
    ld("sp", rtS[0:P, :], ropeS_d, [rtSr])
    ld("sp", sts32, st_in.rearrange("s (a q) e -> q s a e", q=128), ["sts32"])
    S.op("act", lambda e: e.copy(stsb, sts32), reads=["sts32"], writes=["stsb"])
    ld("sp", r[0:P, :], modsd[:, 1024:2048], ["r"], reads=["modsd"])
    ld("sp", xn[0:P, :], modsd[:, 0:1024], ["xn"], reads=["modsd"])
    S.op("dve", lambda e: e.scalar_tensor_tensor(r[0:P, :], r[0:P, :], 1.0, xt[0:P, :], ALU.add, ALU.mult),
         reads=["r", "xt"], writes=["r"])
    S.op("dve", lambda e: e.tensor_tensor(r[0:P, :], r[0:P, :], xn[0:P, :], ALU.add), reads=["r", "xn"], writes=["r"])

    def tr32(src_fn, n, reads, dst, dst_res, eng="act"):
        ps, pr = nps()

        def f(e, ps=ps):
            for j in range(n):
                ins = e.transpose(ps[:, j * 32:(j + 1) * 32], src_fn(j), ident[0:32, 0:32])
            return ins
        S.op("pe", f, reads=list(reads) + ["ident"], writes=[pr])
        src = ps[:, 0:n * 32].rearrange("p (a b) -> p a b", a=n)
        if eng == "act":
            S.op("act", lambda e: e.copy(dst, src), reads=[pr], writes=[dst_res])
        else:
            S.op("dve", lambda e: e.tensor_copy(dst, src), reads=[pr], writes=[dst_res])

    tr32(lambda k: r[0:P, k * 128:(k + 1) * 128], 8, ["r"], hT[:, :, 0:32], "hT")
    for g in range(6):
        ps, pr = nps()

        def f(e, ps=ps, g=g):
            for k in range(8):
                ins = e.matmul(ps[0:P, :], hT[:, k, 0:32], w_in_b[:, k, g * 512:(g + 1) * 512],
                               start=(k == 0), stop=(k == 7))
            return ins
        S.op("pe", f, reads=["hT"] + WIN, writes=[pr])
        S.op("act", lambda e, ps=ps, g=g: e.copy(z[0:P, g * 512:(g + 1) * 512], ps[0:P, :]),
             reads=[pr], writes=["z%d" % g])
    S.dma("sp", lambda e: e.dma_start(out=vs, in_=z[0:P, 2560:3072]), reads=["z5"])
    S.op("act", lambda e: e.activation(sg[0:P, :], z[0:P, 1024:1536], AF.Silu), reads=["z2"], writes=["sg"])
    S.op("act", lambda e: e.copy(rvb[0:P, :], z[0:P, 512:1024]), reads=["z1"], writes=["rvb"])
    S.op("act", lambda e: e.copy(vnb[:], z[0:P, 2560:3072]), reads=["z5"], writes=["vnb"])
    mks = mkr[0]
    do_rope(P, rtS, rtSr, z[0:P, 0:256], 32, 192, qr[0:P, :], 0, "z0", "qr", dqks, "dqks")
    do_rope(P, rtS, rtSr, z[0:P, 256:512], 32, 192, kr[0:P, :], 4, "z0", "kr", dqks, "dqks")
    do_rope(P, rtS, rtSr, z[0:P, 1536:2048], 64, 0, mqr[0:P, :], None, "z3", "mqr", dqks, "dqks")
    do_rope(P, rtS, rtSr, z[0:P, 2048:2560], 64, 0, mks[0:P, :], None, "z4", "mkr0", dqks, "dqks")
    S.dma("sp", lambda e: e.dma_start(out=ks, in_=mks[0:P, :]), reads=["mkr0"])
    S.op("dve", lambda e: e.tensor_tensor(krbm[0:P, :, :], bc(kr[0:P, :].unsqueeze(1), [P, 4, 256]),
                                          bc(rm[0:P, :].unsqueeze(2), [P, 4, 256]), ALU.mult),
         reads=["kr", "rm"], writes=["krbm"])
    tr32(lambda j: qr[0:P, j * 128:(j + 1) * 128], 2, ["qr"], qTs[:], "qTs")
    tr32(lambda j: kr[0:P, j * 128:(j + 1) * 128], 2, ["kr"], kTs[:], "kTs")
    tr32(lambda j: mqr[0:P, j * 128:(j + 1) * 128], 4, ["mqr"], mqTs[:], "mqTs")
    tr32(lambda j: mks[0:P, j * 128:(j + 1) * 128], 4, ["mkr0"], mkTs[:], "mkTs")
    S.op("dve", lambda e: e.tensor_tensor(qTsm[:], bc(qTs[:].unsqueeze(2), [128, 2, 4, 32]),
                                          bc(cm[:].unsqueeze(1), [128, 2, 4, 32]), ALU.mult),
         reads=["qTs", "cm"], writes=["qTsm"])
    pso, pso_r = psf[4], "psf4"
    for h in range(4):
        p_, hh = h // 2, h % 2
        lo, hi = hh * 64, hh * 64 + 64
        ps, pr = nps()
        S.op("pe", lambda e, ps=ps, p_=p_, lo=lo, hi=hi: e.matmul(
            ps[0:P, 0:32], kTs[lo:hi, p_, :], qTs[lo:hi, p_, :], start=True, stop=True),
            reads=["kTs", "qTs"], writes=[pr])
        sm = scTm[h % 2]
        smr = "scTm%d" % (h % 2)
        S.op("dve", lambda e, ps=ps, sm=sm: e.tensor_tensor(sm[0:P, 0:32], ps[0:P, 0:32], maskS[:], ALU.mult),
             reads=[pr, "maskS"], writes=[smr])

        def f(e, sm=sm, h=h, p_=p_, lo=lo, hi=hi):
            e.matmul(pso[0:P, h * 128:(h + 1) * 128], sm[0:P, 0:32], rvb[0:P, h * 128:(h + 1) * 128],
                     start=True, stop=False)
            for s_ in range(4):
                ins = e.matmul(pso[0:P, h * 128:(h + 1) * 128], qTsm[lo:hi, p_, s_, :], stsb[lo:hi, s_, p_, :],
                               start=False, stop=(s_ == 3))
            return ins
        S.op("pe", f, reads=[smr, "rvb", "qTsm", "stsb"], writes=[pso_r])
    for s_ in range(4):
        ps, pr = nps()

        def f(e, ps=ps, s_=s_):
            for p_ in range(2):
                ins = e.matmul(ps[:, p_ * 256:(p_ + 1) * 256], krbm[0:P, s_, p_ * 128:(p_ + 1) * 128],
                               rvb[0:P, p_ * 256:(p_ + 1) * 256], start=True, stop=True)
            return ins
        S.op("pe", f, reads=["krbm", "rvb"], writes=[pr])
        for hh in range(2):
            lo, hi = hh * 64, hh * 64 + 64
            S.op("dve", lambda e, ps=ps, s_=s_, hh=hh, lo=lo, hi=hi: e.tensor_tensor(
                sttmp[lo:hi, :, :], sts32[lo:hi, s_, :, :],
                ps[lo:hi, :].rearrange("q (a h e) -> q a h e", a=2, h=2)[:, :, hh, :], ALU.add),
                reads=["sts32", pr, pso_r], writes=["sttmp"])
            S.op("dve", lambda e, s_=s_, lo=lo, hi=hi: e.tensor_tensor(
                sts32[lo:hi, s_, :, :], sttmp[lo:hi, :, :], bc(gtab[lo:hi, :].unsqueeze(2), [64, 2, 128]), ALU.mult),
                reads=["sttmp", "gtab"], writes=["sts32"])
    S.dma("sp", lambda e: e.dma_start(out=ss.rearrange("s (a q) e -> q s a e", q=128), in_=sts32), reads=["sts32"])
    group_norm(P, pso, pso_r)
    tr32(lambda j: yret[0:P, j * 128:(j + 1) * 128], 4, ["yret"], mixT[:, 0:4, 0:32], "mixT")

    psO, psO_r = psf[5], "psf5"
    gcnt = [0, 0]
    for s_ in range(4):
        S.marks.append((S.seq, 'smoba %d' % s_))
        qsl = slice(s_ * 8, (s_ + 1) * 8)
        for gi in range(16):
            sl = gcnt[0] % 2
            gcnt[0] += 1
            for j in range(4):
                col = s_ * 64 + gi * 4 + j
                S.dma("pool", lambda e, sl=sl, j=j, col=col: e.indirect_dma_start(
                    out=Kg[sl][:, j, :], out_offset=None, in_=ck, in_offset=IOA(ap=idx[:, col:col + 1], axis=0)),
                    reads=["idx"], writes=["Kg%d" % sl])
            for half in range(2):
                pb, pbr = npsb()

                def f(e, pb=pb, sl=sl, half=half):
                    for i in range(8):
                        jj, hh_ = (half * 8 + i) // 4, (half * 8 + i) % 4
                        ins = e.transpose(pb[:, i * 128:(i + 1) * 128], Kg[sl][:, jj, hh_ * 128:(hh_ + 1) * 128], identb[:])
                    return ins
                S.op("pe", f, reads=["Kg%d" % sl, "identb"], writes=[pbr])
                srcv = pb[:, :].rearrange("p (a b) -> p a b", a=8)
                if half == 0:
                    S.op("act", lambda e, sl=sl, srcv=srcv: e.copy(KTg[sl][:, 0:8, :], srcv),
                         reads=[pbr], writes=["KTg%d" % sl])
                else:
                    S.op("dve", lambda e, sl=sl, srcv=srcv: e.tensor_copy(KTg[sl][:, 8:16, :], srcv),
                         reads=[pbr], writes=["KTg%d" % sl])
            ps, pr = nps()

            def f(e, ps=ps, sl=sl, qsl=qsl):
                for i in range(16):
                    ins = e.matmul(ps[:, i * 8:(i + 1) * 8], KTg[sl][:, i, :], mqTs[:, i % 4, qsl], start=True, stop=True)
                return ins
            S.op("pe", f, reads=["KTg%d" % sl, "mqTs"], writes=[pr])
            hfS, pg0 = gi // 8, (gi % 8) * 4
            S.op("dve", lambda e, ps=ps, hfS=hfS, pg0=pg0: e.tensor_copy(
                Sall[hfS][:, pg0:pg0 + 4, :], ps[:, 0:128].rearrange("p (a b) -> p a b", a=4)),
                reads=[pr], writes=["Sall%d" % hfS])
        ps, pr = nps()

        def f(e, ps=ps):
            for pg in range(64):
                ins = e.matmul(ps[0:32, pg:pg + 1], Sall[pg // 32][:, pg % 32, :], ones_f[:, 0:1], start=True, stop=True)
            return ins
        S.op("pe", f, reads=["Sall0", "Sall1", "ones_f"], writes=[pr])
        S.op("act", lambda e, ps=ps: e.copy(gsb[:], ps[0:32, 0:64]), reads=[pr], writes=["gsb"])
        gs3 = gsb[:].rearrange("p (n t) -> p n t", t=2)
        S.op("dve", lambda e, gs3=gs3: e.tensor_tensor(g32[:], gs3[:, :, 0], gs3[:, :, 1], ALU.add),
             reads=["gsb"], writes=["g32"])
        S.op("dve", lambda e: e.max(mxs[:], g32[:]), reads=["g32"], writes=["mxs"])
        S.op("dve", lambda e: e.tensor_scalar(sel[:], g32[:], mxs[:, 2:3], None, ALU.is_ge),
             reads=["g32", "mxs"], writes=["sel"])
        Dm3 = Dm[0:32, :].rearrange("p (n q) -> p n q", n=32)
        S.op("dve", lambda e, Dm3=Dm3: e.tensor_tensor(Dm3, bc(sel[:].unsqueeze(2), [32, 32, 32]),
                                                       bc(ident[0:32, 0:32].unsqueeze(1), [32, 32, 32]), ALU.mult),
             reads=["sel", "ident"], writes=["Dm"])
        for half in range(2):
            ps, pr = nps()
            S.op("pe", lambda e, ps=ps, half=half: e.matmul(ps[:, :], ones_b[0:32, :], Dm[0:32, half * 512:(half + 1) * 512],
                                                            start=True, stop=True),
                 reads=["ones_b", "Dm"], writes=[pr])
            S.op("act", lambda e, ps=ps, half=half: e.copy(selBs[:, half * 16:(half + 1) * 16, :],
                                                           ps[:, :].rearrange("p (a b) -> p a b", a=16)),
                 reads=[pr], writes=["selBs"])
        for hfS in range(2):
            S.op("act", lambda e, hfS=hfS: e.activation(Sall[hfS], Sall[hfS], AF.Exp, scale=SCALE),
                 reads=["Sall%d" % hfS], writes=["Sall%d" % hfS])
            S.op("dve", lambda e, hfS=hfS: e.tensor_tensor(
                PTs[:, hfS * 32:(hfS + 1) * 32, :].rearrange("p (n t) q -> p n t q", t=2),
                Sall[hfS].rearrange("p (n t) q -> p n t q", t=2),
                bc(selBs[:, hfS * 16:(hfS + 1) * 16, :].unsqueeze(2), [128, 16, 2, 32]), ALU.mult),
                reads=["Sall%d" % hfS, "selBs"], writes=["PTs"])
        ps, pr = nps()

        def f(e, ps=ps, qsl=qsl):
            for h in range(4):
                ins = e.matmul(ps[0:32, h * 8:(h + 1) * 8], mkTs[:, h, :], mqTs[:, h, qsl], start=True, stop=True)
            return ins
        S.op("pe", f, reads=["mkTs", "mqTs"], writes=[pr])
        S.op("act", lambda e, ps=ps: e.activation(En[:], ps[0:32, 0:32], AF.Exp, scale=SCALE), reads=[pr], writes=["En"])
        S.op("dve", lambda e, s_=s_: e.tensor_tensor(PN[:], En[:], cmaskN[:, s_, :], ALU.mult),
             reads=["En", "cmaskN"], writes=["PN"])
        for gi in range(16):
            sl = gcnt[1] % 2
            gcnt[1] += 1
            for j in range(4):
                col = s_ * 64 + gi * 4 + j
                S.dma("pool", lambda e, sl=sl, j=j, col=col: e.indirect_dma_start(
                    out=Vg[sl][:, j, :], out_offset=None, in_=cv, in_offset=IOA(ap=idx[:, col:col + 1], axis=0)),
                    reads=["idx"], writes=["Vg%d" % sl])

            def f(e, sl=sl, gi=gi):
                for j in range(4):
                    pg = gi * 4 + j
                    for h in range(4):
                        e.matmul(psO[:, h * 8:(h + 1) * 8], Vg[sl][:, j, h * 128:(h + 1) * 128], PTs[:, pg, h * 8:(h + 1) * 8],
                                 start=(pg == 0), stop=False)
                    ins = e.matmul(psO[:, 32:64], ones_b[:, :], PTs[:, pg, :], start=(pg == 0), stop=False)
                return ins
            S.op("pe", f, reads=["Vg%d" % sl, "PTs", "ones_b"], writes=[psO_r])

        def f(e):
            for h in range(4):
                e.matmul(psO[:, h * 8:(h + 1) * 8], vnb[0:32, h * 128:(h + 1) * 128], PN[0:32, h * 8:(h + 1) * 8],
                         start=False, stop=True)
            return e.matmul(psO[:, 32:64], ones_b[0:32, :], PN[0:32, :], start=False, stop=True)
        S.op("pe", f, reads=["vnb", "PN", "ones_b"], writes=[psO_r])
        S.op("dve", lambda e: e.reciprocal(rinv[:], psO[:, 32:64]), reads=[psO_r], writes=["rinv"])
        S.op("dve", lambda e, qsl=qsl: e.tensor_tensor(mixT[:, 4:8, qsl], psO[:, 0:32].rearrange("p (h q) -> p h q", h=4),
                                                       rinv[:].rearrange("p (h q) -> p h q", h=4), ALU.mult),
             reads=[psO_r, "rinv"], writes=["mixT"])
    S.marks.append((S.seq, 'sample oproj'))
    ld("sp", z[0:P, 0:1024], modsd[:, 2048:3072], ["z0", "z1"], reads=["modsd"])
    pss = [nps(), nps()]
    for hf in range(2):
        def f(e, hf=hf, ps=pss[hf][0]):
            for c in range(8):
                ins = e.matmul(ps[0:P, :], mixT[:, c, 0:32], w_o_b[:, c, hf * 512:(hf + 1) * 512],
                               start=(c == 0), stop=(c == 7))
            return ins
        S.op("pe", f, reads=["mixT", "w_o"], writes=[pss[hf][1]])
    layer_norm([pss[0][0], pss[1][0]], [pss[0][1], pss[1][1]], xt, "xt", z[:, 0:1024], ["z0", "z1"], x1, "x1", P=P)
    S.dma("sp", lambda e: e.dma_start(out=x1d[2048:2080, :], in_=x1[0:P, :]), reads=["x1"], writes=["x1ds"])

    for t in range(NT):
        S.marks.append((S.seq, 'p1 tile %d' % t))
        rt = rtab[t % 2]
        rtr = "rtab%d" % (t % 2)
        mk = mkr[t % 2]
        mkres = "mkr%d" % (t % 2)
        ld("sp", xt[:], x[t * 128:(t + 1) * 128, :], ["xt"])
        ld("sp", rt[:, 0:192], ropeM[t * 128:(t + 1) * 128, :], [rtr])
        ld("sp", rt[:, 192:288], ropeR[t * 128:(t + 1) * 128, :], [rtr])
        transposes_to(lambda b, n: hT[:, b, :], lambda k: xt[:, k * 128:(k + 1) * 128], 8, ["xt"], ["hT"],
                      scale_fn=lambda k: (modT[:, 8 + k:9 + k], modT[:, k:k + 1]))
        for g in range(6):
            ps, pr = nps()

            def f(e, ps=ps, g=g):
                for k in range(8):
                    ins = e.matmul(ps[:, :], hT[:, k, :], w_in_b[:, k, g * 512:(g + 1) * 512],
                                   start=(k == 0), stop=(k == 7))
                return ins
            S.op("pe", f, reads=["hT"] + WIN, writes=[pr])
            if g % 2 == 0:
                S.op("act", lambda e, ps=ps, g=g: e.copy(z[:, g * 512:(g + 1) * 512], ps[:, :]),
                     reads=[pr], writes=["z%d" % g])
            else:
                S.op("dve", lambda e, ps=ps, g=g: e.tensor_copy(z[:, g * 512:(g + 1) * 512], ps[:, :]),
                     reads=[pr], writes=["z%d" % g])
        S.dma("sp", lambda e, t=t: e.dma_start(out=vout[t * 128:(t + 1) * 128, :], in_=z[:, 2560:3072]),
              reads=["z5"])
        S.op("act", lambda e: e.activation(sg[:], z[:, 1024:1536], AF.Silu), reads=["z2"], writes=["sg"])
        S.op("act", lambda e: e.copy(rvb[:], z[:, 512:1024]), reads=["z1"], writes=["rvb"])
        S.op("act", lambda e, t=t: e.copy(Vb[:, t, :], z[:, 2560:3072]), reads=["z5"], writes=["Vb"])

        do_rope(128, rt, rtr, z[:, 0:256], 32, 192, qr[:], 0, "z0", "qr", dqk, "dqk")
        do_rope(128, rt, rtr, z[:, 256:512], 32, 192, kr[:], 4, "z0", "kr", dqk, "dqk")
        do_rope(128, rt, rtr, z[:, 1536:2048], 64, 0, mqr[:], None, "z3", "mqr", dqk, "dqk")
        do_rope(128, rt, rtr, z[:, 2048:2560], 64, 0, mk[:], None, "z4", mkres, dqk, "dqk")
        S.dma("sp", lambda e, t=t, mk=mk: e.dma_start(out=kout[t * 128:(t + 1) * 128, :], in_=mk[:]), reads=[mkres])
        S.op("act", lambda e: e.copy(krb[:], kr[:]), reads=["kr"], writes=["krb"])
        transposes_to(lambda b, n: qT[:, b:b + n, :], lambda j: qr[:, j * 128:(j + 1) * 128], 2, ["qr"], ["qT"])
        transposes_to(lambda b, n: kT[:, b:b + n, :], lambda j: kr[:, j * 128:(j + 1) * 128], 2, ["kr"], ["kT"])
        transposes_to(lambda b, n: mqT[:, b:b + n, :], lambda j: mqr[:, j * 128:(j + 1) * 128], 4, ["mqr"], ["mqT"])
        ps, pr = nps()

        def f(e, ps=ps, mk=mk):
            for j in range(4):
                ins = e.transpose(ps[:, j * 128:(j + 1) * 128], mk[:, j * 128:(j + 1) * 128], ident[:])
            return ins
        S.op("pe", f, reads=[mkres, "ident"], writes=[pr])
        S.op("act", lambda e, ps=ps, t=t: e.copy(KT[:, :, t * 128:(t + 1) * 128],
                                                  ps[:, :].rearrange("p (h n) -> p h n", h=4)),
             reads=[pr], writes=["KT"])
        S.op("dve", lambda e, ps=ps, t=t: e.reduce_sum(kmT[:, :, t], ps[:, :].rearrange("p (h n) -> p h n", h=4), AX.X),
             reads=[pr, "KT"], writes=["kmT"])
        if t % 2 == 1:
            n = t // 2
            S.op("dve", lambda e, n=n: e.tensor_tensor(kmsum[:, :, n], kmT[:, :, 2 * n], kmT[:, :, 2 * n + 1], ALU.add),
                 reads=["kmT"], writes=["kmsum"])
            S.op("dve", lambda e, n=n: e.tensor_scalar_mul(kmb[:, :, n], kmsum[:, :, n], 1.0 / 256),
                 reads=["kmsum"], writes=["kmb"])

        S.marks.append((S.seq, 'ret %d' % t))
        pso, pso_r = psf[4], "psf4"
        for h in range(4):
            p_, hh = h // 2, h % 2
            lo, hi = hh * 64, hh * 64 + 64
            ps, pr = nps()
            S.op("pe", lambda e, ps=ps, p_=p_, lo=lo, hi=hi: e.matmul(
                ps[:, 0:128], kT[lo:hi, p_, :], qT[lo:hi, p_, :], start=True, stop=True),
                reads=["kT", "qT"], writes=[pr])
            sm = scTm[h % 2]
            smr = "scTm%d" % (h % 2)
            S.op("dve", lambda e, ps=ps, sm=sm: e.tensor_tensor(sm[:], ps[:, 0:128], triT[:], ALU.mult),
                 reads=[pr, "triT"], writes=[smr])

            def f(e, sm=sm, h=h, p_=p_, lo=lo, hi=hi):
                e.matmul(pso[:, h * 128:(h + 1) * 128], sm[:], rvb[:, h * 128:(h + 1) * 128], start=True, stop=False)
                return e.matmul(pso[:, h * 128:(h + 1) * 128], qT[lo:hi, p_, :], stb[lo:hi, p_, :],
                                start=False, stop=True)
            S.op("pe", f, reads=[smr, "rvb", "qT", "stb"], writes=[pso_r])
        for p_ in range(2):
            ps, pr = nps()
            S.op("pe", lambda e, ps=ps, p_=p_: e.matmul(
                ps[:, 0:256], krb[:, p_ * 128:(p_ + 1) * 128], rvb[:, p_ * 256:(p_ + 1) * 256], start=True, stop=True),
                reads=["krb", "rvb"], writes=[pr])
            for hh in range(2):
                h = 2 * p_ + hh
                lo, hi = hh * 64, hh * 64 + 64
                gC = GAM[h] ** 128
                S.op("dve", lambda e, ps=ps, p_=p_, hh=hh, lo=lo, hi=hi: e.tensor_tensor(
                    sttmp[lo:hi, p_, :], st32[lo:hi, p_, :], ps[lo:hi, hh * 128:(hh + 1) * 128], ALU.add),
                    reads=["st32", pr, pso_r], writes=["sttmp"])
                S.op("dve", lambda e, p_=p_, lo=lo, hi=hi, gC=gC: e.tensor_scalar_mul(
                    st32[lo:hi, p_, :], sttmp[lo:hi, p_, :], gC), reads=["sttmp"], writes=["st32"])
                S.op("act", lambda e, p_=p_, lo=lo, hi=hi, gC=gC: e.mul(
                    stb[lo:hi, p_, :], sttmp[lo:hi, p_, :], gC), reads=["sttmp"], writes=["stb"])
        S.marks.append((S.seq, 'gn %d' % t))
        group_norm(128, pso, pso_r)
        transposes_to(lambda b, n: mixT[:, b:b + n, :], lambda j: yret[:, j * 128:(j + 1) * 128], 4,
                      ["yret"], ["mixT"])

        S.marks.append((S.seq, 'moba %d' % t))
        own = t // 2
        nkt = t + 1
        psO, psO_r = psf[5], "psf5"
        for h in range(4):
            if own >= 4:
                ps, pr = nps()
                S.op("pe", lambda e, ps=ps, h=h: e.matmul(ps[:, 0:8], mqT[:, h, :], kmb[:, h, :], start=True, stop=True),
                     reads=["mqT", "kmb"], writes=[pr])
                S.op("dve", lambda e: e.memset(g8[:], -1e30), writes=["g8"])
                S.op("dve", lambda e, ps=ps, own=own: e.tensor_copy(g8[:, 0:own], ps[:, 0:own]),
                     reads=[pr], writes=["g8"])
                S.op("dve", lambda e: e.max(mx8[:], g8[:]), reads=["g8"], writes=["mx8"])
                S.op("dve", lambda e: e.tensor_scalar(biasn[:], g8[:], mx8[:, 2:3], 1.0, ALU.is_ge, ALU.subtract),
                     reads=["g8", "mx8"], writes=["biasn"])
                S.op("dve", lambda e: e.tensor_scalar_mul(biasn[:], biasn[:], -NEG), reads=["biasn"], writes=["biasn"])
            else:
                S.op("dve", lambda e: e.memset(biasn[:], 0.0), writes=["biasn"])
            S.op("dve", lambda e: e.memset(rs[:], 0.0), writes=["rs"])
            for n0 in range(0, own, 2):
                nb = min(2, own - n0)
                ps, pr = nps()
                S.op("pe", lambda e, ps=ps, h=h, n0=n0, nb=nb: e.matmul(
                    ps[:, 0:nb * 256], mqT[:, h, :], KT[:, h, n0 * 256:(n0 + nb) * 256], start=True, stop=True),
                    reads=["mqT", "KT"], writes=[pr])
                for j in range(nb):
                    n = n0 + j
                    S.op("act", lambda e, ps=ps, j=j, n=n: e.activation(
                        Pm[:, n * 256:(n + 1) * 256], ps[:, j * 256:(j + 1) * 256], AF.Exp,
                        bias=biasn[:, n:n + 1], scale=SCALE, accum_out=rs[:, n:n + 1]),
                        reads=[pr, "biasn", "rs"], writes=["Pm", "rs"])
            ps, pr = nps()
            k0 = own * 256
            nown = (t + 1) * 128 - k0
            S.op("pe", lambda e, ps=ps, h=h, k0=k0, nown=nown: e.matmul(
                ps[:, 0:nown], mqT[:, h, :], KT[:, h, k0:k0 + nown], start=True, stop=True),
                reads=["mqT", "KT"], writes=[pr])
            if nown == 256:
                S.op("act", lambda e, ps=ps, k0=k0: e.activation(
                    Pm[:, k0:k0 + 128], ps[:, 0:128], AF.Exp, scale=SCALE, accum_out=rs[:, 8:9]),
                    reads=[pr, "rs"], writes=["Pm", "rs"])
            d0 = nown - 128
            S.op("dve", lambda e, ps=ps, d0=d0: e.tensor_tensor(sd[:], ps[:, d0:d0 + 128], tribias[:], ALU.add),
                 reads=[pr, "tribias"], writes=["sd"])
            S.op("act", lambda e, t=t: e.activation(Pm[:, t * 128:(t + 1) * 128], sd[:], AF.Exp, scale=SCALE,
                                                    accum_out=rs[:, 9:10]),
                 reads=["sd", "rs"], writes=["Pm", "rs"])
            S.op("dve", lambda e: e.reduce_sum(small[:, 40:41], rs[:, 0:10], AX.X), reads=["rs"], writes=["m0"])
            S.op("dve", lambda e: e.reciprocal(small[:, 41:42], small[:, 40:41]), reads=["m0"], writes=["m1"])
            S.op("dve", lambda e, nkt=nkt: e.tensor_scalar_mul(Pm[:, 0:nkt * 128], Pm[:, 0:nkt * 128], small[:, 41:42]),
                 reads=["Pm", "m1"], writes=["Pm"])
            for k8 in range(0, nkt, 8):
                nn = min(8, nkt - k8)
                pb, pbr = npsb()
                ptt = PT[(k8 // 8) % 2]
                ptr = "PT%d" % ((k8 // 8) % 2)

                def f(e, pb=pb, k8=k8, nn=nn):
                    for j in range(nn):
                        ins = e.transpose(pb[:, j * 128:(j + 1) * 128], Pm[:, (k8 + j) * 128:(k8 + j + 1) * 128],
                                          identb[:])
                    return ins
                S.op("pe", f, reads=["Pm", "identb"], writes=[pbr])
                S.op("dve", lambda e, pb=pb, nn=nn, ptt=ptt: e.tensor_copy(
                    ptt[:, 0:nn, :], pb[:, 0:nn * 128].rearrange("p (a b) -> p a b", a=nn)),
                    reads=[pbr], writes=[ptr])

                def f2(e, ptt=ptt, k8=k8, nn=nn, h=h, nkt=nkt):
                    for j in range(nn):
                        kt = k8 + j
                        ins = e.matmul(psO[:, h * 128:(h + 1) * 128], Vb[:, kt, h * 128:(h + 1) * 128], ptt[:, j, :],
                                       start=(kt == 0), stop=(kt == nkt - 1))
                    return ins
                S.op("pe", f2, reads=[ptr, "Vb"], writes=[psO_r])
        S.op("act", lambda e: e.copy(mixT[:, 4:8, :], psO[:, :].rearrange("p (h n) -> p h n", h=4)),
             reads=[psO_r], writes=["mixT"])

        S.marks.append((S.seq, 'oproj %d' % t))
        pss = [nps(), nps()]
        for hf in range(2):
            def f(e, hf=hf, ps=pss[hf][0]):
                for c in range(8):
                    ins = e.matmul(ps[:, :], mixT[:, c, :], w_o_b[:, c, hf * 512:(hf + 1) * 512],
                                   start=(c == 0), stop=(c == 7))
                return ins
            S.op("pe", f, reads=["mixT", "w_o"], writes=[pss[hf][1]])
        layer_norm([pss[0][0], pss[1][0]], [pss[0][1], pss[1][1]], xt, "xt", gate_a, ["gate_a"], x1, "x1")
        S.dma("sp", lambda e, t=t: e.dma_start(out=x1d[t * 128:(t + 1) * 128, :], in_=x1[:]), reads=["x1"],
              writes=["x1d%d" % t])

    for p_ in range(2):
        S.dma("sp", lambda e, p_=p_: e.dma_start(out=sout[p_ * 128:(p_ + 1) * 128, :], in_=st32[:, p_, :]),
              reads=["st32"])

    S.marks.append((S.seq, 'phase2'))
    P1 = WIN + ["w_o"]
    w_up_v = w_up.rearrange("(k p) n -> p k n", p=128)
    for k2 in range(4):
        ld("pool", w_up_b[:, 2 * k2:2 * k2 + 2, :], w_up_v[:, 2 * k2:2 * k2 + 2, :], ["w_up%d" % k2] + P1)
    WUP = ["w_up%d" % i for i in range(4)]
    w_down_v = w_down.rearrange("(k p) n -> p k n", p=128)
    for k4 in range(4):
        ld("pool", w_down_b[:, 8 * k4:8 * k4 + 8, :], w_down_v[:, 8 * k4:8 * k4 + 8, :],
           ["w_dn%d" % k4, "KT", "Vb", "wada0", "wada1"] + TAIL)
    WDN = ["w_dn%d" % i for i in range(4)]
    ld("sp", lng[:], l2g, ["lng"])
    ld("sp", lnb[:], l2b, ["lnb"])
    ld("sp", gate_a[:], gfd, ["gate_a"], reads=["gfd"])
    S.marks.append((S.seq, 'sample ffn'))
    ld("sp", xt[0:P, :], x1d[2048:2080, :], ["xt"], reads=["x1ds"])
    ld("sp", r[0:P, :], modsd[:, 4096:5120], ["r"], reads=["modsd"])
    ld("sp", xn[0:P, :], modsd[:, 3072:4096], ["xn"], reads=["modsd"])
    ld("sp", z[0:P, 2048:3072], modsd[:, 5120:6144], ["z4", "z5"], reads=["modsd"])
    S.op("dve", lambda e: e.scalar_tensor_tensor(r[0:P, :], r[0:P, :], 1.0, xt[0:P, :], ALU.add, ALU.mult),
         reads=["r", "xt"], writes=["r"])
    S.op("dve", lambda e: e.tensor_tensor(r[0:P, :], r[0:P, :], xn[0:P, :], ALU.add), reads=["r", "xn"], writes=["r"])
    tr32(lambda k: r[0:P, k * 128:(k + 1) * 128], 8, ["r"], hT[:, :, 0:32], "hT")
    for g in range(8):
        ps, pr = nps()

        def f(e, ps=ps, g=g):
            for j in range(4):
                fc = g * 4 + j
                for k in range(8):
                    ins = e.matmul(ps[:, j * 32:(j + 1) * 32], w_up_b[:, k, fc * 128:(fc + 1) * 128], hT[:, k, 0:32],
                                   start=(k == 0), stop=(k == 7))
            return ins
        S.op("pe", f, reads=["hT"] + WUP, writes=[pr])
        rr = rl[g % 2]
        rrr = "rl%d" % (g % 2)
        S.op("dve", lambda e, ps=ps, rr=rr: e.tensor_scalar_max(rr[:, 0:128], ps[:, 0:128], 0.0), reads=[pr], writes=[rrr])
        S.op("act", lambda e, rr=rr, g=g: e.activation(
            uT[:, 4 * g:4 * g + 4, 0:32], rr[:, 0:128].rearrange("p (a b) -> p a b", a=4), AF.Square),
            reads=[rrr], writes=["uT"])
    pss = [nps(), nps()]
    for hf in range(2):
        def f(e, hf=hf, ps=pss[hf][0]):
            for c in range(32):
                ins = e.matmul(ps[0:P, :], uT[:, c, 0:32], w_down_b[:, c, hf * 512:(hf + 1) * 512],
                               start=(c == 0), stop=(c == 31))
            return ins
        S.op("pe", f, reads=["uT"] + WDN, writes=[pss[hf][1]])
    layer_norm([pss[0][0], pss[1][0]], [pss[0][1], pss[1][1]], xt, "xt", z[:, 2048:3072], ["z4", "z5"], x1, "x1", P=P)
    S.dma("sp", lambda e: e.dma_start(out=ys, in_=x1[0:P, :]), reads=["x1"])
    for t in range(NT):
        ld("sp", xt[:], x1d[t * 128:(t + 1) * 128, :], ["xt"], reads=["x1d%d" % t])
        transposes_to(lambda b, n: hT[:, b, :], lambda k: xt[:, k * 128:(k + 1) * 128], 8, ["xt"], ["hT"],
                      scale_fn=lambda k: (modT[:, 32 + k:33 + k], modT[:, 24 + k:25 + k]))
        for g in range(8):
            ps, pr = nps()

            def f(e, ps=ps, g=g):
                for j in range(4):
                    fc = g * 4 + j
                    for k in range(8):
                        ins = e.matmul(ps[:, j * 128:(j + 1) * 128], w_up_b[:, k, fc * 128:(fc + 1) * 128], hT[:, k, :],
                                       start=(k == 0), stop=(k == 7))
                return ins
            S.op("pe", f, reads=["hT"] + WUP, writes=[pr])
            rr = rl[g % 2]
            rrr = "rl%d" % (g % 2)
            S.op("dve", lambda e, ps=ps, rr=rr: e.tensor_scalar_max(rr[:], ps[:, :], 0.0), reads=[pr], writes=[rrr])
            S.op("act", lambda e, rr=rr, g=g: e.activation(
                uT[:, 4 * g:4 * g + 4, :], rr[:].rearrange("p (a b) -> p a b", a=4), AF.Square),
                reads=[rrr], writes=["uT"])
        pss = [nps(), nps()]
        for hf in range(2):
            def f(e, hf=hf, ps=pss[hf][0]):
                for c in range(32):
                    ins = e.matmul(ps[:, :], uT[:, c, :], w_down_b[:, c, hf * 512:(hf + 1) * 512],
                                   start=(c == 0), stop=(c == 31))
                return ins
            S.op("pe", f, reads=["uT"] + WDN, writes=[pss[hf][1]])
        layer_norm([pss[0][0], pss[1][0]], [pss[0][1], pss[1][1]], xt, "xt", gate_a, ["gate_a"], x1, "x1")
        S.dma("sp", lambda e, t=t: e.dma_start(out=y[t * 128:(t + 1) * 128, :], in_=x1[:]), reads=["x1"])

    S.marks.append((S.seq, 'end'))
    if marks is not None:
        marks.extend(S.marks)
    sems = {}
    for k in ("pe", "act", "dve", "pool"):
        sems[k] = nc.alloc_semaphore("s_" + k)
    for i in range(S.n_dma):
        sems["d%d" % i] = nc.alloc_semaphore("s_d%d" % i)
    with nc.Block() as block:
        @block.sync
        def _(e):
            S.emit("sp", e, sems, final_wait=True)

        @block.tensor
        def _(e):
            S.emit("pe", e, sems)

        @block.scalar
        def _(e):
            S.emit("act", e, sems)

        @block.vector
        def _(e):
            S.emit("dve", e, sems)

        @block.gpsimd
        def _(e):
            S.emit("pool", e, sems)
    return nc


_CONST = {}


def _consts():
    if _CONST:
        return _CONST
    pos = np.arange(2048, dtype=np.float32)

    def tab(half):
        inv = np.power(np.float32(10000.0), -np.arange(half, dtype=np.float32) / np.float32(half)).astype(np.float32)
        ang = (pos[:, None] * inv[None, :]).astype(np.float32)
        c, s = np.cos(ang).astype(np.float32), np.sin(ang).astype(np.float32)
        return np.ascontiguousarray(np.concatenate([c, -s, s], axis=1))
    _CONST["ropeM"] = tab(64)
    _CONST["ropeR"] = tab(32)
    i = np.arange(128, dtype=np.float64)
    dq = np.stack([np.power(GAM[h], i + 1.0) for h in range(4)], axis=1)
    dk = np.stack([np.power(GAM[h], -(i + 1.0)) / 8.0 for h in range(4)], axis=1)
    _CONST["dqk"] = np.ascontiguousarray(np.concatenate([dq, dk], axis=1).astype(np.float32))
    jj, ii = np.meshgrid(np.arange(128), np.arange(128), indexing="ij")
    _CONST["triT"] = (ii >= jj).astype(np.float32)
    _CONST["tribias"] = np.where(ii >= jj, 0.0, NEG).astype(np.float32).T.copy()
    _CONST["ident"] = np.eye(128, dtype=np.float32)
    return _CONST


def _sample_consts():
    if "ropeS" in _CONST:
        return _CONST
    pos = (8192 + (np.arange(32) % 8)).astype(np.float32)

    def tab(half):
        inv = np.power(np.float32(10000.0), -np.arange(half, dtype=np.float32) / np.float32(half)).astype(np.float32)
        ang = (pos[:, None] * inv[None, :]).astype(np.float32)
        c, s = np.cos(ang).astype(np.float32), np.sin(ang).astype(np.float32)
        return np.concatenate([c, -s, s], axis=1)
    _CONST["ropeS"] = np.ascontiguousarray(np.concatenate([tab(64), tab(32)], axis=1))
    i = (np.arange(32) % 8).astype(np.float64)
    dq = np.stack([np.power(GAM[h], i + 1.0) for h in range(4)], axis=1)
    dk = np.stack([np.power(GAM[h], -(i + 1.0)) / 8.0 for h in range(4)], axis=1)
    _CONST["dqks"] = np.ascontiguousarray(np.concatenate([dq, dk], axis=1).astype(np.float32))
    rr = np.arange(32)
    _CONST["maskS"] = ((rr[:, None] // 8 == rr[None, :] // 8) & (rr[None, :] >= rr[:, None])).astype(np.float32)
    cm = (rr[None, :] // 8 == np.arange(4)[:, None]).astype(np.float32).reshape(1, 128)
    _CONST["cm"] = np.ascontiguousarray(np.broadcast_to(cm, (128, 128)))
    _CONST["rm"] = (rr[:, None] // 8 == np.arange(4)[None, :]).astype(np.float32)
    q = np.arange(128)
    _CONST["gtab"] = np.array([[GAM[2 * a + (qq // 64)] ** 8 for a in range(2)] for qq in q], dtype=np.float32)
    cmn = np.zeros((32, 4, 4, 8), np.float32)
    for sp in range(4):
        for j in range(8):
            for qq in range(8):
                if j <= qq:
                    cmn[sp * 8 + j, sp, :, qq] = 1.0
    _CONST["cmaskN"] = cmn.reshape(32, 128)
    _CONST["pcol"] = np.arange(128, dtype=np.float32)[:, None].copy()
    return _CONST


def make_in_maps(x_prompt, x_sample, cache_k, cache_v, state_ret, page_table, c_prompt, c_sample,
                 w_ada, b_ada, w_in, w_o, ln1_g, ln1_b, w_up, w_down, ln2_g, ln2_b, cores=range(8)):
    f = lambda a: np.ascontiguousarray(np.asarray(a, dtype=np.float32))
    C = _consts()
    _sample_consts()
    b_ada0 = f(b_ada)[0]
    ckf = f(cache_k).reshape(-1, 512)
    cvf = f(cache_v).reshape(-1, 512)
    shared = {
        "w_ada": f(w_ada)[0], "badaT": np.ascontiguousarray(b_ada0.reshape(48, 128).T),
        "bga": np.ascontiguousarray(np.broadcast_to(b_ada0[2048:3072], (128, 1024))),
        "bgf": np.ascontiguousarray(np.broadcast_to(b_ada0[5120:6144], (128, 1024))),
        "bada32": np.ascontiguousarray(np.broadcast_to(b_ada0, (32, 6144))),
        "w_in": f(w_in)[0], "w_o": f(w_o)[0], "w_up": f(w_up)[0], "w_down": f(w_down)[0],
        "l1g": np.ascontiguousarray(np.broadcast_to(f(ln1_g)[0], (128, 1024))),
        "l1b": np.ascontiguousarray(np.broadcast_to(f(ln1_b)[0], (128, 1024))),
        "l2g": np.ascontiguousarray(np.broadcast_to(f(ln2_g)[0], (128, 1024))),
        "l2b": np.ascontiguousarray(np.broadcast_to(f(ln2_b)[0], (128, 1024))),
        "ck": ckf, "cv": cvf,
    }
    for k in ("ident", "ropeM", "ropeR", "dqk", "triT", "tribias", "ropeS", "dqks", "maskS", "cm", "rm", "gtab",
              "cmaskN", "pcol"):
        shared[k] = C[k]
    xp, xsm, cp, csm = f(x_prompt), f(x_sample), f(c_prompt), f(c_sample)
    st = f(state_ret)[0]
    pt = np.asarray(page_table).astype(np.int32)
    in_maps = []
    for c in cores:
        m = dict(shared)
        m["x"] = xp[c]
        m["cT"] = np.ascontiguousarray(cp[c].reshape(8, 128).T)
        m["xs"] = np.ascontiguousarray(xsm[4 * c:4 * c + 4].reshape(32, 1024))
        crow = np.repeat(csm[4 * c:4 * c + 4], 8, axis=0)
        m["csT"] = np.ascontiguousarray(crow.reshape(32, 8, 128).transpose(2, 1, 0).reshape(128, 256))
        m["ptb"] = np.ascontiguousarray(np.broadcast_to(pt[4 * c:4 * c + 4].reshape(1, 256), (128, 256)))
        m["st_in"] = np.ascontiguousarray(st[4 * c:4 * c + 4].reshape(4, 256, 128))
        in_maps.append(m)
    return in_maps


def kernel(x_prompt, x_sample, cache_k, cache_v, state_ret, page_table, c_prompt, c_sample,
           w_ada, b_ada, w_in, w_o, ln1_g, ln1_b, w_up, w_down, ln2_g, ln2_b):
    nc = build_nc()
    in_maps = make_in_maps(x_prompt, x_sample, cache_k, cache_v, state_ret, page_table, c_prompt, c_sample,
                           w_ada, b_ada, w_in, w_o, ln1_g, ln1_b, w_up, w_down, ln2_g, ln2_b)
    res = run_bass_kernel_spmd(nc, in_maps, core_ids=list(range(8)))
    R = res.results
    g = lambda c, k: np.asarray(R[c][k]).astype(np.float32)
    y_p = np.stack([g(c, "y") for c in range(8)])
    k_p = np.stack([g(c, "kout").reshape(2048, 4, 128) for c in range(8)])[None]
    v_p = np.stack([g(c, "vout").reshape(2048, 4, 128) for c in range(8)])[None]
    s_p = np.stack([g(c, "sout").reshape(4, 64, 128) for c in range(8)])[None]
    y_s = np.concatenate([g(c, "ys").reshape(4, 8, 1024) for c in range(8)])
    k_s = np.concatenate([g(c, "ks").reshape(4, 8, 4, 128) for c in range(8)])[None]
    v_s = np.concatenate([g(c, "vs").reshape(4, 8, 4, 128) for c in range(8)])[None]
    s_s = np.concatenate([g(c, "ss").reshape(4, 4, 64, 128) for c in range(8)])[None]
    return (y_p, y_s, k_p, v_p, s_p, k_s, v_s, s_s)
```

```python
import math
import numpy as np
import concourse.bass as bass
import concourse.mybir as mybir
from concourse.bass_utils import run_bass_kernel_spmd

F32 = mybir.dt.float32
BF16 = mybir.dt.bfloat16
I32 = mybir.dt.int32
AF = mybir.ActivationFunctionType
ALU = mybir.AluOpType
AX = mybir.AxisListType

NT = 16
ALPHA = 2.0 ** 0.25
LN_EPS = 1e-5
GN_EPS = 1e-6
NEG = -30000.0
SCALE = 128.0 ** -0.5
GAM = [1.0 - 2.0 ** (-5.0 - h) for h in range(4)]


class Sched:
    ENG = ("pe", "act", "dve", "pool", "sp")

    def __init__(self, n_dma):
        self.ops = {e: [] for e in self.ENG}
        self.cnt = {e: 0 for e in self.ENG}
        self.lastw = {}
        self.readers = {}
        self.n_dma = n_dma
        self.dma_val = [0] * n_dma
        self.dma_next = 0
        self.seq = 0
        self.cut = None
        self.marks = []
        self.alias = {"ta": ["xnA"], "tb": ["xnB"], "sq": ["xnA", "xnB"], "xn": ["xnA", "xnB"],
                      "yn": ["rA"], "yret": ["rB"], "r": ["rA", "rB"], "sg": ["x1A"], "x1": ["x1A", "x1B"],
                      "rl0": ["z0"], "rl1": ["z1"], "uT": ["Pm", "PT0", "PT1"], "mkr1": ["rB"], "mkr0": ["rB"],
                      "mqr": ["rA"], "scTb": ["Pm"],
                      "KT": ["KT", "KTg0", "KTg1"] + ["Kg%d_%d" % (a, b) for a in range(2) for b in range(4)],
                      "Vb": ["Vb", "PTs", "selBs", "Dm"] + ["Vg%d_%d" % (a, b) for a in range(2) for b in range(4)],
                      "cm": ["rtab0"], "cmaskN": ["rtab0"], "xt": ["xt0"], "hT": ["hT0"]}

    def _exp(self, names):
        out = []
        for n in names:
            out += self.alias.get(n, [n])
        return out

    def _deps(self, reads, writes):
        reads, writes = self._exp(reads), self._exp(writes)
        toks = []
        for r in reads:
            if r in self.lastw:
                toks.append(self.lastw[r])
        for w in writes:
            if w in self.lastw:
                toks.append(self.lastw[w])
            toks += self.readers.get(w, [])
        return toks

    def _commit(self, tok, reads, writes):
        reads, writes = self._exp(reads), self._exp(writes)
        for r in reads:
            self.readers.setdefault(r, []).append(tok)
        for w in writes:
            self.lastw[w] = tok
            self.readers[w] = []

    def op(self, eng, fn, reads=(), writes=()):
        writes = list(writes) + [r for r in reads if r.startswith("ps")]
        toks = self._deps(reads, writes)
        self.cnt[eng] += 1
        tok = (eng, self.cnt[eng])
        self.seq += 1
        self.ops[eng].append((fn, toks, tok, self.seq))
        self._commit(tok, reads, writes)

    def dma(self, q, fn, reads=(), writes=()):
        toks = self._deps(reads, writes)
        i = self.dma_next
        self.dma_next = (i + 1) % self.n_dma
        if self.dma_val[i] > 0:
            toks.append(("d%d" % i, self.dma_val[i]))
        self.dma_val[i] += 16
        tok = ("d%d" % i, self.dma_val[i])
        self.seq += 1
        self.ops[q].append((fn, toks, tok, self.seq))
        self._commit(tok, reads, writes)

    def emit(self, eng_name, e, sems, final_wait=False):
        seen = {}
        for fn, toks, tok, seq in self.ops[eng_name]:
            if self.cut is not None and seq > self.cut:
                continue
            need = {}
            for k, v in toks:
                if k == eng_name and eng_name in ("pe", "sp"):
                    continue
                if v > need.get(k, 0):
                    need[k] = v
            for k, v in need.items():
                if seen.get(k, 0) >= v:
                    continue
                e.wait_ge(sems[k], v)
                seen[k] = v
            ins = fn(e)
            ins.then_inc(sems[tok[0]], 16 if tok[0][1:].isdigit() else 1)
        if final_wait:
            fin = {}
            for en in self.ENG:
                for fn, toks, tok, seq in self.ops[en]:
                    if self.cut is not None and seq > self.cut:
                        continue
                    if tok[0][1:].isdigit():
                        fin[tok[0]] = max(fin.get(tok[0], 0), tok[1])
            for k, v in fin.items():
                e.wait_ge(sems[k], v)


def build_nc(cut=None, marks=None):
    nc = bass.Bass("TRN2", target_bir_lowering=False)

    def din(name, shape, dt=F32):
        return nc.dram_tensor(name, shape, dt, kind="ExternalInput").ap()

    def dout(name, shape, dt=F32):
        return nc.dram_tensor(name, shape, dt, kind="ExternalOutput").ap()

    x = din("x", [2048, 1024])
    cT = din("cT", [128, 8])
    w_ada = din("w_ada", [1024, 6144])
    badaT = din("badaT", [128, 48])
    bga = din("bga", [128, 1024])
    bgf = din("bgf", [128, 1024])
    w_in = din("w_in", [1024, 3072])
    w_o = din("w_o", [1024, 1024])
    w_up = din("w_up", [1024, 4096])
    w_down = din("w_down", [4096, 1024])
    l1g = din("l1g", [128, 1024])
    l1b = din("l1b", [128, 1024])
    l2g = din("l2g", [128, 1024])
    l2b = din("l2b", [128, 1024])
    ident_d = din("ident", [128, 128])
    ropeM = din("ropeM", [2048, 192])
    ropeR = din("ropeR", [2048, 96])
    dqk_d = din("dqk", [128, 8])
    triT_d = din("triT", [128, 128])
    tribias_d = din("tribias", [128, 128])
    y = dout("y", [2048, 1024])
    kout = dout("kout", [2048, 512])
    vout = dout("vout", [2048, 512])
    sout = dout("sout", [256, 128])
    x1d = nc.dram_tensor("x1d", [2080, 1024], F32).ap()
    gfd = nc.dram_tensor("gfd", [128, 1024], F32).ap()
    modsd = nc.dram_tensor("modsd", [32, 6144], F32).ap()
    xs_d = din("xs", [32, 1024])
    csT_d = din("csT", [128, 256])
    bada32 = din("bada32", [32, 6144])
    ptb_d = din("ptb", [128, 256], I32)
    pcol_d = din("pcol", [128, 1])
    st_in = din("st_in", [4, 256, 128])
    ck = din("ck", [327680, 512])
    cv = din("cv", [327680, 512])
    ropeS_d = din("ropeS", [32, 288])
    dqks_d = din("dqks", [32, 8])
    maskS_d = din("maskS", [32, 32])
    cm_d = din("cm", [128, 128])
    rm_d = din("rm", [32, 4])
    gtab_d = din("gtab", [128, 2])
    cmaskN_d = din("cmaskN", [32, 128])
    ys = dout("ys", [32, 1024])
    ks = dout("ks", [32, 512])
    vs = dout("vs", [32, 512])
    ss = dout("ss", [4, 256, 128])

    def sb(name, shape, dt=F32):
        return nc.alloc_sbuf_tensor(name, shape, dt)

    W = sb("W", [128, 65536], BF16)
    w_in_b = W[:, 0:24576].rearrange("p (k n) -> p k n", k=8)
    w_o_b = W[:, 24576:32768].rearrange("p (k n) -> p k n", k=8)
    KT = W[:, 32768:40960].rearrange("p (h n) -> p h n", h=4)
    Vb = W[:, 40960:49152].rearrange("p (t n) -> p t n", t=16)
    wada = [W[:, 49152 + i * 4096:49152 + (i + 1) * 4096].rearrange("p (k n) -> p k n", k=8) for i in range(2)]
    w_up_b = W[:, 0:32768].rearrange("p (k n) -> p k n", k=8)
    w_down_b = W[:, 32768:65536].rearrange("p (k n) -> p k n", k=32)

    ident = sb("ident_s", [128, 128])
    identb = sb("identb", [128, 128], BF16)
    triT = sb("triT_s", [128, 128])
    tribias = sb("tribias_s", [128, 128])
    dqk = sb("dqk_s", [128, 8])
    cTs = sb("cTs", [128, 8])
    scT = sb("scT", [128, 8], BF16)
    badaTs = sb("badaTs", [128, 48])
    modT = sb("modT", [128, 48])
    gate_a = sb("gate_a", [128, 1024])
    lng = sb("lng", [128, 1024])
    lnb = sb("lnb", [128, 1024])
    xts = [sb("xt%d" % i, [128, 1024]) for i in range(2)]
    hTs = [sb("hT%d" % i, [128, 8, 128], BF16) for i in range(2)]
    xt, hT = xts[0], hTs[0]
    z = sb("z", [128, 3072])
    Bar = sb("Bar", [128, 4096], BF16)
    rtab = [sb("rtab%d" % i, [128, 288]) for i in range(2)]
    cm = rtab[0][:, 0:128].rearrange("p (a b) -> p a b", a=4)
    cmaskN = rtab[0][0:32, 128:256].rearrange("p (a b) -> p a b", a=4)
    qr = sb("qr", [128, 256])
    kr = sb("kr", [128, 256])
    krb = sb("krb", [128, 256], BF16)
    rvb = sb("rvb", [128, 512], BF16)
    qT = sb("qT", [128, 2, 128], BF16)
    kT = sb("kT", [128, 2, 128], BF16)
    mqT = sb("mqT", [128, 4, 128], BF16)
    kmT = sb("kmT", [128, 4, 16])
    kmsum = sb("kmsum", [128, 4, 8])
    kmb = sb("kmb", [128, 4, 8], BF16)
    st32 = sb("st32", [128, 2, 128])
    sttmp = sb("sttmp", [128, 2, 128])
    stb = sb("stb", [128, 2, 128], BF16)
    scTm = [sb("scTm%d" % i, [128, 128], BF16) for i in range(2)]
    mixT = sb("mixT", [128, 8, 128], BF16)
    small = sb("small", [128, 64])
    g8 = sb("g8", [128, 8])
    mx8 = sb("mx8", [128, 8])
    biasn = sb("biasn", [128, 8])
    rs = sb("rs", [128, 16])
    Pm = Bar[:, 0:2048]
    sd = sb("sd", [128, 128])
    PT = [Bar[:, 2048 + i * 1024:3072 + i * 1024].rearrange("p (a b) -> p a b", a=8) for i in range(2)]
    r = sb("r", [128, 1024])
    xn = sb("xn", [128, 1024])
    x1 = sb("x1", [128, 1024])
    ta = xn[:, 0:512]
    tb = xn[:, 512:1024]
    sq = xn
    yn = r[:, 0:512]
    yret = r[:, 512:1024]
    sg = x1[:, 0:512]
    rl = [z[:, i * 512:(i + 1) * 512] for i in range(2)]
    uT = Bar[:, :].rearrange("p (a b) -> p a b", a=32)
    scTb = Bar[:, 0:1024].rearrange("p (a b) -> p a b", a=8)
    mqr = r[:, 0:512]
    mkr = [r[:, 512:1024]] * 2
    Kg = [W[:, 32768 + i * 2048:32768 + (i + 1) * 2048].rearrange("p (a b) -> p a b", a=4) for i in range(2)]
    KTg = [W[:, 36864 + i * 2048:36864 + (i + 1) * 2048].rearrange("p (a b) -> p a b", a=16) for i in range(2)]
    Vg = [W[:, 40960 + i * 2048:40960 + (i + 1) * 2048].rearrange("p (a b) -> p a b", a=4) for i in range(2)]
    PTs = W[:, 45056:47104].rearrange("p (a b) -> p a b", a=64)
    selBs = W[:, 47104:48128].rearrange("p (a b) -> p a b", a=32)
    Dm = W[:, 48128:49152]
    sts32 = W[:, 57344:59392].bitcast(F32).rearrange("p (s a e) -> p s a e", s=4, a=2)
    Sall = [W[:, 59392 + i * 2048:59392 + (i + 1) * 2048].bitcast(F32).rearrange("p (a b) -> p a b", a=32)
            for i in range(2)]
    stsb = W[:, 63488:64512].rearrange("p (s a e) -> p s a e", s=4, a=2)
    krbm = W[:, 64512:65536].rearrange("p (s n) -> p s n", s=4)
    TAIL = ["sts32", "Sall0", "Sall1", "stsb", "krbm"]
    csTs = sb("csTs", [128, 256])
    scTs = sb("scTs", [128, 8, 32], BF16)
    ptb_s = sb("ptb_s", [128, 256], I32)
    idx = sb("idx", [128, 256], I32)
    pcol_s = sb("pcol_s", [128, 1])
    dqks = sb("dqks_s", [32, 8])
    maskS = sb("maskS_s", [32, 32])
    rm = sb("rm_s", [32, 4])
    gtab = sb("gtab_s", [128, 2])
    qTs = sb("qTs", [128, 2, 32], BF16)
    kTs = sb("kTs", [128, 2, 32], BF16)
    qTsm = sb("qTsm", [128, 2, 4, 32], BF16)
    mqTs = sb("mqTs", [128, 4, 32], BF16)
    mkTs = sb("mkTs", [128, 4, 32], BF16)
    vnb = sb("vnb", [32, 512], BF16)
    PN = sb("PN", [32, 32], BF16)
    En = sb("En", [32, 32])
    gsb = sb("gsb", [32, 64])
    g32 = sb("g32", [32, 32])
    mxs = sb("mxs", [32, 8])
    sel = sb("sel", [32, 32])
    rinv = sb("rinv", [128, 32])
    ones_f = sb("ones_f", [128, 1])
    ones_b = sb("ones_b", [128, 128], BF16)

    psf = [nc.alloc_psum_tensor("psf%d" % i, [128, 512], F32) for i in range(6)]
    psb = [nc.alloc_psum_tensor("psb%d" % i, [128, 1024], BF16) for i in range(2)]

    S = Sched(n_dma=20)
    S.cut = cut
    pc = [0, 0]

    def nps():
        i = pc[0] % 4
        pc[0] += 1
        return psf[i], "psf%d" % i

    def npsb():
        i = pc[1] % 2
        pc[1] += 1
        return psb[i], "psb%d" % i

    def bc(ap, shape):
        return ap.to_broadcast(shape)

    def ld(q, dst, src, res, reads=()):
        S.dma(q, lambda e, d=dst, s=src: e.dma_start(out=d, in_=s), reads=reads, writes=res)

    ld("sp", ident[:], ident_d, ["ident"])
    ld("sp", triT[:], triT_d, ["triT"])
    ld("sp", tribias[:], tribias_d, ["tribias"])
    ld("sp", dqk[:], dqk_d, ["dqk"])
    ld("sp", cTs[:], cT, ["cTs"])
    ld("sp", badaTs[:], badaT, ["badaTs"])
    ld("sp", gate_a[:], bga, ["gate_a"])
    ld("sp", xn[:], bgf, ["xn"])
    S.op("dve", lambda e: e.tensor_copy(identb[:], ident[:]), reads=["ident"], writes=["identb"])
    S.op("dve", lambda e: e.memset(st32[:], 0.0), writes=["st32"])
    S.op("dve", lambda e: e.memset(stb[:], 0.0), writes=["stb"])
    S.op("dve", lambda e: e.memset(kmsum[:], 0.0), writes=["kmsum"])
    S.op("dve", lambda e: e.memset(kmb[:], 0.0), writes=["kmb"])

    S.op("act", lambda e: e.activation(scT[:], cTs[:], AF.Silu), reads=["cTs"], writes=["scT"])
    S.op("dve", lambda e: e.tensor_copy(scTb[:], bc(scT[:].unsqueeze(2), [128, 8, 128])),
         reads=["scT"], writes=["scTb"])
    ld("sp", csTs[:], csT_d, ["csTs"])
    ld("sp", ptb_s[:], ptb_d, ["ptb_s"])
    ld("sp", pcol_s[:], pcol_d, ["pcol_s"])
    ld("sp", dqks[:], dqks_d, ["dqks"])
    ld("sp", maskS[:], maskS_d, ["maskS"])
    ld("sp", cm, cm_d.rearrange("p (a b) -> p a b", a=4), ["cm"])
    ld("sp", rm[:], rm_d, ["rm"])
    ld("sp", gtab[:], gtab_d, ["gtab"])
    ld("sp", cmaskN, cmaskN_d.rearrange("p (a b) -> p a b", a=4), ["cmaskN"])
    S.op("dve", lambda e: e.memset(ones_f[:], 1.0), writes=["ones_f"])
    S.op("dve", lambda e: e.memset(ones_b[:], 1.0), writes=["ones_b"])
    S.op("act", lambda e: e.activation(scTs[:], csTs[:].rearrange("p (k r) -> p k r", k=8), AF.Silu),
         reads=["csTs"], writes=["scTs"])
    S.op("dve", lambda e: e.tensor_scalar(idx[:], ptb_s[:], 128.0, pcol_s[:, 0:1], ALU.mult, ALU.add),
         reads=["ptb_s", "pcol_s"], writes=["idx"])
    w_ada_v = w_ada.rearrange("(k p) n -> p k n", p=128)
    psmod, psmod_r = psf[5], "psf5"
    for g in range(12):
        sl = g % 2
        ld("pool", wada[sl], w_ada_v[:, :, g * 512:(g + 1) * 512], ["wada%d" % sl])
        ps2, pr2 = nps()

        def f(e, ps2=ps2, sl=sl):
            for k in range(8):
                ins = e.matmul(ps2[0:32, :], scTs[:, k, :], wada[sl][:, k, :], start=(k == 0), stop=(k == 7))
            return ins
        S.op("pe", f, reads=["scTs", "wada%d" % sl], writes=[pr2])
        ld("sp", mqr[0:32, :], bada32[:, g * 512:(g + 1) * 512], ["mqr"])
        S.op("dve", lambda e, ps2=ps2: e.tensor_tensor(mkr[0][0:32, :], ps2[0:32, :], mqr[0:32, :], ALU.add),
             reads=[pr2, "mqr"], writes=["mkr0"])
        S.dma("sp", lambda e, g=g: e.dma_start(out=modsd[:, g * 512:(g + 1) * 512], in_=mkr[0][0:32, :]),
              reads=["mkr0"], writes=["modsd"])
        if g in (4, 5, 10, 11):
            ps, pr = nps()

            def f(e, ps=ps, sl=sl):
                for k in range(8):
                    ins = e.matmul(ps[:, :], scTb[:, k, :], wada[sl][:, k, :], start=(k == 0), stop=(k == 7))
                return ins
            S.op("pe", f, reads=["scTb", "wada%d" % sl], writes=[pr])
            dst = gate_a if g < 6 else xn
            half = g % 2
            S.op("dve", lambda e, ps=ps, dst=dst, half=half: e.tensor_tensor(
                dst[:, half * 512:(half + 1) * 512], ps[:, :], dst[:, half * 512:(half + 1) * 512], ALU.add),
                reads=[pr], writes=["gate_a" if g < 6 else "xn"])
        else:
            def f(e, sl=sl, g=g):
                for j in range(4):
                    col = g * 4 + j
                    for k in range(8):
                        ins = e.matmul(psmod[:, col:col + 1], wada[sl][:, k, j * 128:(j + 1) * 128],
                                       scT[:, k:k + 1], start=(k == 0), stop=(k == 7))
                return ins
            S.op("pe", f, reads=["scT", "wada%d" % sl], writes=[psmod_r])
    S.op("dve", lambda e: e.tensor_tensor(modT[:], psmod[:, 0:48], badaTs[:], ALU.add),
         reads=[psmod_r, "badaTs"], writes=["modT"])
    S.op("dve", lambda e: e.tensor_scalar_add(modT[:, 8:16], modT[:, 8:16], 1.0), reads=["modT"], writes=["modT"])
    S.op("dve", lambda e: e.tensor_scalar_add(modT[:, 32:40], modT[:, 32:40], 1.0), reads=["modT"], writes=["modT"])

    S.dma("sp", lambda e: e.dma_start(out=gfd, in_=xn[:]), reads=["xn"], writes=["gfd"])
    w_in_v = w_in.rearrange("(k p) n -> p k n", p=128)
    for k2 in range(4):
        ld("pool", w_in_b[:, 2 * k2:2 * k2 + 2, :], w_in_v[:, 2 * k2:2 * k2 + 2, :], ["w_in%d" % k2])
    WIN = ["w_in%d" % i for i in range(4)]
    ld("pool", w_o_b, w_o.rearrange("(k p) n -> p k n", p=128), ["w_o"])
    ld("sp", lng[:], l1g, ["lng"])
    ld("sp", lnb[:], l1b, ["lnb"])

    def transposes_to(dst_fn, src_fn, n, reads, writes_res, evac_eng="act", scale_fn=None):
        for b0 in range(0, n, 4):
            nb = min(4, n - b0)
            ps, pr = nps()

            def f(e, ps=ps, b0=b0, nb=nb):
                for j in range(nb):
                    ins = e.transpose(ps[:, j * 128:(j + 1) * 128], src_fn(b0 + j), ident[:])
                return ins
            S.op("pe", f, reads=list(reads) + ["ident"], writes=[pr])
            if scale_fn is None:
                if evac_eng == "act":
                    S.op("act", lambda e, ps=ps, b0=b0, nb=nb: e.copy(dst_fn(b0, nb), ps[:, 0:nb * 128].rearrange("p (a b) -> p a b", a=nb)),
                         reads=[pr], writes=writes_res)
                else:
                    S.op("dve", lambda e, ps=ps, b0=b0, nb=nb: e.tensor_copy(dst_fn(b0, nb), ps[:, 0:nb * 128].rearrange("p (a b) -> p a b", a=nb)),
                         reads=[pr], writes=writes_res)
            else:
                for j in range(nb):
                    sc, bi = scale_fn(b0 + j)
                    S.op("act", lambda e, ps=ps, j=j, b0=b0, sc=sc, bi=bi: e.activation(
                        dst_fn(b0 + j, 1), ps[:, j * 128:(j + 1) * 128], AF.Identity, bias=bi, scale=sc),
                        reads=[pr, "modT"], writes=writes_res)

    def layer_norm(src_ps_list, src_res, resid, resid_res, gate, gate_res, out_t, out_res, P=128):
        for hf in range(2):
            S.op("dve", lambda e, hf=hf: e.tensor_tensor(
                r[0:P, hf * 512:(hf + 1) * 512], src_ps_list[hf][0:P, :], gate[0:P, hf * 512:(hf + 1) * 512], ALU.mult),
                reads=[src_res[hf]] + list(gate_res), writes=["r"])
        S.op("dve", lambda e: e.scalar_tensor_tensor(r[0:P, :], resid[0:P, :], ALPHA, r[0:P, :], ALU.mult, ALU.add),
             reads=[resid_res, "r"], writes=["r"])
        S.op("dve", lambda e: e.reduce_sum(small[0:P, 0:1], r[0:P, :], AX.X), reads=["r"], writes=["sm0"])
        S.op("act", lambda e: e.activation(sq[0:P, :], r[0:P, :], AF.Square), reads=["r"], writes=["sq"])
        S.op("dve", lambda e: e.reduce_sum(small[0:P, 1:2], sq[0:P, :], AX.X), reads=["sq"], writes=["sm1"])
        S.op("dve", lambda e: e.tensor_scalar_mul(small[0:P, 2:3], small[0:P, 0:1], 1.0 / 1024), reads=["sm0"], writes=["sm2"])
        S.op("dve", lambda e: e.tensor_tensor(small[0:P, 3:4], small[0:P, 2:3], small[0:P, 2:3], ALU.mult),
             reads=["sm2"], writes=["sm3"])
        S.op("dve", lambda e: e.scalar_tensor_tensor(small[0:P, 4:5], small[0:P, 1:2], 1.0 / 1024, small[0:P, 3:4],
                                                     ALU.mult, ALU.subtract), reads=["sm1", "sm3"], writes=["sm4"])
        S.op("dve", lambda e: e.tensor_scalar_add(small[0:P, 4:5], small[0:P, 4:5], LN_EPS), reads=["sm4"], writes=["sm4"])
        S.op("act", lambda e: e.sqrt(small[0:P, 7:8], small[0:P, 4:5]), reads=["sm4"], writes=["sm7"])
        S.op("dve", lambda e: e.reciprocal(small[0:P, 5:6], small[0:P, 7:8]), reads=["sm7"], writes=["sm5"])
        S.op("dve", lambda e: e.scalar_tensor_tensor(small[0:P, 6:7], small[0:P, 2:3], -1.0, small[0:P, 5:6],
                                                     ALU.mult, ALU.mult), reads=["sm2", "sm5"], writes=["sm6"])
        S.op("act", lambda e: e.activation(xn[0:P, :], r[0:P, :], AF.Identity, bias=small[0:P, 6:7], scale=small[0:P, 5:6]),
             reads=["r", "sm5", "sm6"], writes=["xn"])
        S.op("pool", lambda e: e.tensor_tensor(xn[0:P, :], xn[0:P, :], lng[0:P, :], ALU.mult), reads=["xn", "lng"], writes=["xn"])
        S.op("pool", lambda e: e.tensor_tensor(out_t[0:P, :], xn[0:P, :], lnb[0:P, :], ALU.add), reads=["xn", "lnb"], writes=[out_res])

    def do_rope(P, rt, rtr, src, H, c0, dst, dec, zres, dres, dq_t, dq_r):
        n = 8 * H
        s4 = src.rearrange("p (h t j) -> p h t j", h=4, t=2)
        ta4 = ta[0:P, 0:n].rearrange("p (h t j) -> p h t j", h=4, t=2)
        tb4 = tb[0:P, 0:n].rearrange("p (h t j) -> p h t j", h=4, t=2)
        cosb = bc(rt[0:P, c0:c0 + H].unsqueeze(1).unsqueeze(1), [P, 4, 2, H])
        snb = bc(rt[0:P, c0 + H:c0 + 2 * H].unsqueeze(1), [P, 4, H])
        spb = bc(rt[0:P, c0 + 2 * H:c0 + 3 * H].unsqueeze(1), [P, 4, H])
        S.op("dve", lambda e: e.tensor_tensor(ta4, s4, cosb, ALU.mult), reads=[zres, rtr], writes=["ta"])
        S.op("dve", lambda e: e.tensor_tensor(tb4[:, :, 0, :], s4[:, :, 1, :], snb, ALU.mult),
             reads=[zres, rtr], writes=["tb"])
        S.op("dve", lambda e: e.tensor_tensor(tb4[:, :, 1, :], s4[:, :, 0, :], spb, ALU.mult),
             reads=[zres, rtr], writes=["tb"])
        if dec is None:
            S.op("dve", lambda e: e.tensor_tensor(dst, ta[0:P, 0:n], tb[0:P, 0:n], ALU.add),
                 reads=["ta", "tb"], writes=[dres])
        else:
            S.op("dve", lambda e: e.tensor_tensor(ta[0:P, 0:n], ta[0:P, 0:n], tb[0:P, 0:n], ALU.add),
                 reads=["ta", "tb"], writes=["ta"])
            decb = bc(dq_t[0:P, dec:dec + 4].unsqueeze(2), [P, 4, 2 * H])
            S.op("dve", lambda e: e.tensor_tensor(dst.rearrange("p (h j) -> p h j", h=4),
                                                  ta[0:P, 0:n].rearrange("p (h j) -> p h j", h=4), decb, ALU.mult),
                 reads=["ta", dq_r], writes=[dres])

    def group_norm(P, pso, pso_r):
        pso4 = pso[0:P, :].rearrange("p (h n) -> p h n", h=4)
        S.op("dve", lambda e: e.reduce_sum(small[0:P, 8:12], pso4, AX.X), reads=[pso_r], writes=["g0"])
        S.op("act", lambda e: e.activation(sq[0:P, 0:512], pso[0:P, :], AF.Square), reads=[pso_r], writes=["sq"])
        S.op("dve", lambda e: e.reduce_sum(small[0:P, 12:16], sq[0:P, 0:512].rearrange("p (h n) -> p h n", h=4), AX.X),
             reads=["sq"], writes=["g1"])
        S.op("dve", lambda e: e.tensor_scalar_mul(small[0:P, 16:20], small[0:P, 8:12], 1.0 / 128), reads=["g0"], writes=["g2"])
        S.op("dve", lambda e: e.tensor_tensor(small[0:P, 20:24], small[0:P, 16:20], small[0:P, 16:20], ALU.mult),
             reads=["g2"], writes=["g3"])
        S.op("dve", lambda e: e.scalar_tensor_tensor(small[0:P, 24:28], small[0:P, 12:16], 1.0 / 128, small[0:P, 20:24],
                                                     ALU.mult, ALU.subtract), reads=["g1", "g3"], writes=["g4"])
        S.op("dve", lambda e: e.tensor_scalar_add(small[0:P, 24:28], small[0:P, 24:28], GN_EPS), reads=["g4"], writes=["g4"])
        S.op("act", lambda e: e.sqrt(small[0:P, 36:40], small[0:P, 24:28]), reads=["g4"], writes=["g7"])
        S.op("dve", lambda e: e.reciprocal(small[0:P, 28:32], small[0:P, 36:40]), reads=["g7"], writes=["g5"])
        S.op("dve", lambda e: e.scalar_tensor_tensor(small[0:P, 32:36], small[0:P, 16:20], -1.0, small[0:P, 28:32],
                                                     ALU.mult, ALU.mult), reads=["g2", "g5"], writes=["g6"])
        for h in range(4):
            S.op("act", lambda e, h=h: e.activation(yn[0:P, h * 128:(h + 1) * 128], pso[0:P, h * 128:(h + 1) * 128],
                                                    AF.Identity, bias=small[0:P, 32 + h:33 + h],
                                                    scale=small[0:P, 28 + h:29 + h]),
                 reads=[pso_r, "g5", "g6"], writes=["yn"])
        S.op("dve", lambda e: e.tensor_tensor(yret[0:P, :], yn[0:P, :], sg[0:P, :], ALU.mult),
             reads=["yn", "sg"], writes=["yret"])

    S.marks.append((S.seq, 'sample mixer'))
    P = 32
    IOA = bass.IndirectOffsetOnAxis
    rtS, rtSr = rtab[1], "rtab1"
    ld("sp", xt[0:P, :], xs_d, ["xt"])
    ld("sp", rtS[0:P, :], ropeS_d, [rtSr])
    ld("sp", sts32, st_in.rearrange("s (a q) e -> q s a e", q=128), ["sts32"])
    S.op("act", lambda e: e.copy(stsb, sts32), reads=["sts32"], writes=["stsb"])
    ld("sp", r[0:P, :], modsd[:, 1024:2048], ["r"], reads=["modsd"])
    ld("sp", xn[0:P, :], modsd[:, 0:1024], ["xn"], reads=["modsd"])
    S.op("dve", lambda e: e.scalar_tensor_tensor(r[0:P, :], r[0:P, :], 1.0, xt[0:P, :], ALU.add, ALU.mult),
         reads=["r", "xt"], writes=["r"])
    S.op("dve", lambda e: e.tensor_tensor(r[0:P, :], r[0:P, :], xn[0:P, :], ALU.add), reads=["r", "xn"], writes=["r"])

    def tr32(src_fn, n, reads, dst, dst_res, eng="act"):
        ps, pr = nps()

        def f(e, ps=ps):
            for j in range(n):
                ins = e.transpose(ps[:, j * 32:(j + 1) * 32], src_fn(j), ident[0:32, 0:32])
            return ins
        S.op("pe", f, reads=list(reads) + ["ident"], writes=[pr])
        src = ps[:, 0:n * 32].rearrange("p (a b) -> p a b", a=n)
        if eng == "act":
            S.op("act", lambda e: e.copy(dst, src), reads=[pr], writes=[dst_res])
        else:
            S.op("dve", lambda e: e.tensor_copy(dst, src), reads=[pr], writes=[dst_res])

    tr32(lambda k: r[0:P, k * 128:(k + 1) * 128], 8, ["r"], hT[:, :, 0:32], "hT")
    for g in range(6):
        ps, pr = nps()

        def f(e, ps=ps, g=g):
            for k in range(8):
                ins = e.matmul(ps[0:P, :], hT[:, k, 0:32], w_in_b[:, k, g * 512:(g + 1) * 512],
                               start=(k == 0), stop=(k == 7))
            return ins
        S.op("pe", f, reads=["hT"] + WIN, writes=[pr])
        S.op("act", lambda e, ps=ps, g=g: e.copy(z[0:P, g * 512:(g + 1) * 512], ps[0:P, :]),
             reads=[pr], writes=["z%d" % g])
    S.dma("sp", lambda e: e.dma_start(out=vs, in_=z[0:P, 2560:3072]), reads=["z5"])
    S.op("act", lambda e: e.activation(sg[0:P, :], z[0:P, 1024:1536], AF.Silu), reads=["z2"], writes=["sg"])
    S.op("act", lambda e: e.copy(rvb[0:P, :], z[0:P, 512:1024]), reads=["z1"], writes=["rvb"])
    S.op("act", lambda e: e.copy(vnb[:], z[0:P, 2560:3072]), reads=["z5"], writes=["vnb"])
    mks = mkr[0]
    do_rope(P, rtS, rtSr, z[0:P, 0:256], 32, 192, qr[0:P, :], 0, "z0", "qr", dqks, "dqks")
    do_rope(P, rtS, rtSr, z[0:P, 256:512], 32, 192, kr[0:P, :], 4, "z0", "kr", dqks, "dqks")
    do_rope(P, rtS, rtSr, z[0:P, 1536:2048], 64, 0, mqr[0:P, :], None, "z3", "mqr", dqks, "dqks")
    do_rope(P, rtS, rtSr, z[0:P, 2048:2560], 64, 0, mks[0:P, :], None, "z4", "mkr0", dqks, "dqks")
    S.dma("sp", lambda e: e.dma_start(out=ks, in_=mks[0:P, :]), reads=["mkr0"])
    S.op("dve", lambda e: e.tensor_tensor(krbm[0:P, :, :], bc(kr[0:P, :].unsqueeze(1), [P, 4, 256]),
                                          bc(rm[0:P, :].unsqueeze(2), [P, 4, 256]), ALU.mult),
         reads=["kr", "rm"], writes=["krbm"])
    tr32(lambda j: qr[0:P, j * 128:(j + 1) * 128], 2, ["qr"], qTs[:], "qTs")
    tr32(lambda j: kr[0:P, j * 128:(j + 1) * 128], 2, ["kr"], kTs[:], "kTs")
    tr32(lambda j: mqr[0:P, j * 128:(j + 1) * 128], 4, ["mqr"], mqTs[:], "mqTs")
    tr32(lambda j: mks[0:P, j * 128:(j + 1) * 128], 4, ["mkr0"], mkTs[:], "mkTs")
    S.op("dve", lambda e: e.tensor_tensor(qTsm[:], bc(qTs[:].unsqueeze(2), [128, 2, 4, 32]),
                                          bc(cm.unsqueeze(1), [128, 2, 4, 32]), ALU.mult),
         reads=["qTs", "cm"], writes=["qTsm"])
    pso, pso_r = psf[4], "psf4"
    for h in range(4):
        p_, hh = h // 2, h % 2
        lo, hi = hh * 64, hh * 64 + 64
        ps, pr = nps()
        S.op("pe", lambda e, ps=ps, p_=p_, lo=lo, hi=hi: e.matmul(
            ps[0:P, 0:32], kTs[lo:hi, p_, :], qTs[lo:hi, p_, :], start=True, stop=True),
            reads=["kTs", "qTs"], writes=[pr])
        sm = scTm[h % 2]
        smr = "scTm%d" % (h % 2)
        S.op("dve", lambda e, ps=ps, sm=sm: e.tensor_tensor(sm[0:P, 0:32], ps[0:P, 0:32], maskS[:], ALU.mult),
             reads=[pr, "maskS"], writes=[smr])

        def f(e, sm=sm, h=h, p_=p_, lo=lo, hi=hi):
            e.matmul(pso[0:P, h * 128:(h + 1) * 128], sm[0:P, 0:32], rvb[0:P, h * 128:(h + 1) * 128],
                     start=True, stop=False)
            for s_ in range(4):
                ins = e.matmul(pso[0:P, h * 128:(h + 1) * 128], qTsm[lo:hi, p_, s_, :], stsb[lo:hi, s_, p_, :],
                               start=False, stop=(s_ == 3))
            return ins
        S.op("pe", f, reads=[smr, "rvb", "qTsm", "stsb"], writes=[pso_r])
    for s_ in range(4):
        ps, pr = nps()

        def f(e, ps=ps, s_=s_):
            for p_ in range(2):
                ins = e.matmul(ps[:, p_ * 256:(p_ + 1) * 256], krbm[0:P, s_, p_ * 128:(p_ + 1) * 128],
                               rvb[0:P, p_ * 256:(p_ + 1) * 256], start=True, stop=True)
            return ins
        S.op("pe", f, reads=["krbm", "rvb"], writes=[pr])
        for hh in range(2):
            lo, hi = hh * 64, hh * 64 + 64
            S.op("dve", lambda e, ps=ps, s_=s_, hh=hh, lo=lo, hi=hi: e.tensor_tensor(
                sttmp[lo:hi, :, :], sts32[lo:hi, s_, :, :],
                ps[lo:hi, :].rearrange("q (a h e) -> q a h e", a=2, h=2)[:, :, hh, :], ALU.add),
                reads=["sts32", pr, pso_r], writes=["sttmp"])
            S.op("dve", lambda e, s_=s_, lo=lo, hi=hi: e.tensor_tensor(
                sts32[lo:hi, s_, :, :], sttmp[lo:hi, :, :], bc(gtab[lo:hi, :].unsqueeze(2), [64, 2, 128]), ALU.mult),
                reads=["sttmp", "gtab"], writes=["sts32"])
    S.dma("sp", lambda e: e.dma_start(out=ss.rearrange("s (a q) e -> q s a e", q=128), in_=sts32), reads=["sts32"])
    group_norm(P, pso, pso_r)
    tr32(lambda j: yret[0:P, j * 128:(j + 1) * 128], 4, ["yret"], mixT[:, 0:4, 0:32], "mixT")

    psO, psO_r = psf[5], "psf5"
    gcnt = [0, 0]
    for s_ in range(4):
        S.marks.append((S.seq, 'smoba %d' % s_))
        qsl = slice(s_ * 8, (s_ + 1) * 8)
        for gi in range(16):
            sl = gcnt[0] % 2
            gcnt[0] += 1
            for j in range(4):
                col = s_ * 64 + gi * 4 + j
                S.dma("pool", lambda e, sl=sl, j=j, col=col: e.indirect_dma_start(
                    out=Kg[sl][:, j, :], out_offset=None, in_=ck, in_offset=IOA(ap=idx[:, col:col + 1], axis=0)),
                    reads=["idx"], writes=["Kg%d_%d" % (sl, j)])
            for half in range(2):
                pb, pbr = npsb()

                def f(e, pb=pb, sl=sl, half=half):
                    for i in range(8):
                        jj, hh_ = (half * 8 + i) // 4, (half * 8 + i) % 4
                        ins = e.transpose(pb[:, i * 128:(i + 1) * 128], Kg[sl][:, jj, hh_ * 128:(hh_ + 1) * 128], identb[:])
                    return ins
                S.op("pe", f, reads=["Kg%d_%d" % (sl, jx) for jx in range(4)] + ["identb"], writes=[pbr])
                srcv = pb[:, :].rearrange("p (a b) -> p a b", a=8)
                if half == 0:
                    S.op("act", lambda e, sl=sl, srcv=srcv: e.copy(KTg[sl][:, 0:8, :], srcv),
                         reads=[pbr], writes=["KTg%d" % sl])
                else:
                    S.op("dve", lambda e, sl=sl, srcv=srcv: e.tensor_copy(KTg[sl][:, 8:16, :], srcv),
                         reads=[pbr], writes=["KTg%d" % sl])
            ps, pr = nps()

            def f(e, ps=ps, sl=sl, qsl=qsl):
                for i in range(16):
                    ins = e.matmul(ps[:, i * 8:(i + 1) * 8], KTg[sl][:, i, :], mqTs[:, i % 4, qsl], start=True, stop=True)
                return ins
            S.op("pe", f, reads=["KTg%d" % sl, "mqTs"], writes=[pr])
            hfS, pg0 = gi // 8, (gi % 8) * 4
            S.op("dve", lambda e, ps=ps, hfS=hfS, pg0=pg0: e.tensor_copy(
                Sall[hfS][:, pg0:pg0 + 4, :], ps[:, 0:128].rearrange("p (a b) -> p a b", a=4)),
                reads=[pr], writes=["Sall%d" % hfS])
        ps, pr = nps()

        def f(e, ps=ps):
            for pg in range(64):
                ins = e.matmul(ps[0:32, pg:pg + 1], Sall[pg // 32][:, pg % 32, :], ones_f[:, 0:1], start=True, stop=True)
            return ins
        S.op("pe", f, reads=["Sall0", "Sall1", "ones_f"], writes=[pr])
        S.op("act", lambda e, ps=ps: e.copy(gsb[:], ps[0:32, 0:64]), reads=[pr], writes=["gsb"])
        gs3 = gsb[:].rearrange("p (n t) -> p n t", t=2)
        S.op("dve", lambda e, gs3=gs3: e.tensor_tensor(g32[:], gs3[:, :, 0], gs3[:, :, 1], ALU.add),
             reads=["gsb"], writes=["g32"])
        S.op("dve", lambda e: e.max(mxs[:], g32[:]), reads=["g32"], writes=["mxs"])
        S.op("dve", lambda e: e.tensor_scalar(sel[:], g32[:], mxs[:, 2:3], None, ALU.is_ge),
             reads=["g32", "mxs"], writes=["sel"])
        Dm3 = Dm[0:32, :].rearrange("p (n q) -> p n q", n=32)
        S.op("dve", lambda e, Dm3=Dm3: e.tensor_tensor(Dm3, bc(sel[:].unsqueeze(2), [32, 32, 32]),
                                                       bc(ident[0:32, 0:32].unsqueeze(1), [32, 32, 32]), ALU.mult),
             reads=["sel", "ident"], writes=["Dm"])
        for half in range(2):
            ps, pr = nps()
            S.op("pe", lambda e, ps=ps, half=half: e.matmul(ps[:, :], ones_b[0:32, :], Dm[0:32, half * 512:(half + 1) * 512],
                                                            start=True, stop=True),
                 reads=["ones_b", "Dm"], writes=[pr])
            S.op("act", lambda e, ps=ps, half=half: e.copy(selBs[:, half * 16:(half + 1) * 16, :],
                                                           ps[:, :].rearrange("p (a b) -> p a b", a=16)),
                 reads=[pr], writes=["selBs"])
        for hfS in range(2):
            S.op("act", lambda e, hfS=hfS: e.activation(Sall[hfS], Sall[hfS], AF.Exp, scale=SCALE),
                 reads=["Sall%d" % hfS], writes=["Sall%d" % hfS])
            S.op("dve", lambda e, hfS=hfS: e.tensor_tensor(
                PTs[:, hfS * 32:(hfS + 1) * 32, :].rearrange("p (n t) q -> p n t q", t=2),
                Sall[hfS].rearrange("p (n t) q -> p n t q", t=2),
                bc(selBs[:, hfS * 16:(hfS + 1) * 16, :].unsqueeze(2), [128, 16, 2, 32]), ALU.mult),
                reads=["Sall%d" % hfS, "selBs"], writes=["PTs"])
        ps, pr = nps()

        def f(e, ps=ps, qsl=qsl):
            for h in range(4):
                ins = e.matmul(ps[0:32, h * 8:(h + 1) * 8], mkTs[:, h, :], mqTs[:, h, qsl], start=True, stop=True)
            return ins
        S.op("pe", f, reads=["mkTs", "mqTs"], writes=[pr])
        S.op("act", lambda e, ps=ps: e.activation(En[:], ps[0:32, 0:32], AF.Exp, scale=SCALE), reads=[pr], writes=["En"])
        S.op("dve", lambda e, s_=s_: e.tensor_tensor(PN[:], En[:], cmaskN[:, s_, :], ALU.mult),
             reads=["En", "cmaskN"], writes=["PN"])
        for gi in range(16):
            sl = gcnt[1] % 2
            gcnt[1] += 1
            for j in range(4):
                col = s_ * 64 + gi * 4 + j
                S.dma("pool", lambda e, sl=sl, j=j, col=col: e.indirect_dma_start(
                    out=Vg[sl][:, j, :], out_offset=None, in_=cv, in_offset=IOA(ap=idx[:, col:col + 1], axis=0)),
                    reads=["idx"], writes=["Vg%d_%d" % (sl, j)])

            def f(e, sl=sl, gi=gi):
                for j in range(4):
                    pg = gi * 4 + j
                    for h in range(4):
                        e.matmul(psO[:, h * 8:(h + 1) * 8], Vg[sl][:, j, h * 128:(h + 1) * 128], PTs[:, pg, h * 8:(h + 1) * 8],
                                 start=(pg == 0), stop=False)
                    ins = e.matmul(psO[:, 32:64], ones_b[:, :], PTs[:, pg, :], start=(pg == 0), stop=False)
                return ins
            S.op("pe", f, reads=["Vg%d_%d" % (sl, jx) for jx in range(4)] + ["PTs", "ones_b"], writes=[psO_r])

        def f(e):
            for h in range(4):
                e.matmul(psO[:, h * 8:(h + 1) * 8], vnb[0:32, h * 128:(h + 1) * 128], PN[0:32, h * 8:(h + 1) * 8],
                         start=False, stop=True)
            return e.matmul(psO[:, 32:64], ones_b[0:32, :], PN[0:32, :], start=False, stop=True)
        S.op("pe", f, reads=["vnb", "PN", "ones_b"], writes=[psO_r])
        S.op("dve", lambda e: e.reciprocal(rinv[:], psO[:, 32:64]), reads=[psO_r], writes=["rinv"])
        S.op("dve", lambda e, qsl=qsl: e.tensor_tensor(mixT[:, 4:8, qsl], psO[:, 0:32].rearrange("p (h q) -> p h q", h=4),
                                                       rinv[:].rearrange("p (h q) -> p h q", h=4), ALU.mult),
             reads=[psO_r, "rinv"], writes=["mixT"])
    S.marks.append((S.seq, 'sample oproj'))
    ld("sp", z[0:P, 0:1024], modsd[:, 2048:3072], ["z0", "z1"], reads=["modsd"])
    pss = [nps(), nps()]
    for hf in range(2):
        def f(e, hf=hf, ps=pss[hf][0]):
            for c in range(8):
                ins = e.matmul(ps[0:P, :], mixT[:, c, 0:32], w_o_b[:, c, hf * 512:(hf + 1) * 512],
                               start=(c == 0), stop=(c == 7))
            return ins
        S.op("pe", f, reads=["mixT", "w_o"], writes=[pss[hf][1]])
    layer_norm([pss[0][0], pss[1][0]], [pss[0][1], pss[1][1]], xt, "xt", z[:, 0:1024], ["z0", "z1"], x1, "x1", P=P)
    S.dma("sp", lambda e: e.dma_start(out=x1d[2048:2080, :], in_=x1[0:P, :]), reads=["x1"], writes=["x1ds"])

    def load_tile(t):
        rt_ = rtab[t % 2]
        rtr_ = "rtab%d" % (t % 2)
        ld("sp", xts[t % 2][:], x[t * 128:(t + 1) * 128, :], ["xt%d" % (t % 2)])
        ld("sp", rt_[:, 0:192], ropeM[t * 128:(t + 1) * 128, :], [rtr_])
        ld("sp", rt_[:, 192:288], ropeR[t * 128:(t + 1) * 128, :], [rtr_])

    for t in range(NT):
        S.marks.append((S.seq, 'p1 tile %d' % t))
        rt = rtab[t % 2]
        rtr = "rtab%d" % (t % 2)
        mk = mkr[t % 2]
        mkres = "mkr%d" % (t % 2)
        xt_t, hT_t = xts[t % 2], hTs[t % 2]
        xtr, hTr = "xt%d" % (t % 2), "hT%d" % (t % 2)
        if t == 0:
            load_tile(0)
        transposes_to(lambda b, n, hT_t=hT_t: hT_t[:, b, :], lambda k, xt_t=xt_t: xt_t[:, k * 128:(k + 1) * 128],
                      8, [xtr], [hTr], scale_fn=lambda k: (modT[:, 8 + k:9 + k], modT[:, k:k + 1]))
        if t + 1 < NT:
            load_tile(t + 1)
        for g in range(6):
            ps, pr = nps()

            def f(e, ps=ps, g=g, hT_t=hT_t):
                for k in range(8):
                    ins = e.matmul(ps[:, :], hT_t[:, k, :], w_in_b[:, k, g * 512:(g + 1) * 512],
                                   start=(k == 0), stop=(k == 7))
                return ins
            S.op("pe", f, reads=[hTr] + WIN, writes=[pr])
            if g % 2 == 0:
                S.op("act", lambda e, ps=ps, g=g: e.copy(z[:, g * 512:(g + 1) * 512], ps[:, :]),
                     reads=[pr], writes=["z%d" % g])
            else:
                S.op("dve", lambda e, ps=ps, g=g: e.tensor_copy(z[:, g * 512:(g + 1) * 512], ps[:, :]),
                     reads=[pr], writes=["z%d" % g])
        S.dma("sp", lambda e, t=t: e.dma_start(out=vout[t * 128:(t + 1) * 128, :], in_=z[:, 2560:3072]),
              reads=["z5"])
        S.op("act", lambda e: e.activation(sg[:], z[:, 1024:1536], AF.Silu), reads=["z2"], writes=["sg"])
        S.op("act", lambda e: e.copy(rvb[:], z[:, 512:1024]), reads=["z1"], writes=["rvb"])
        S.op("act", lambda e, t=t: e.copy(Vb[:, t, :], z[:, 2560:3072]), reads=["z5"], writes=["Vb"])

        do_rope(128, rt, rtr, z[:, 0:256], 32, 192, qr[:], 0, "z0", "qr", dqk, "dqk")
        do_rope(128, rt, rtr, z[:, 256:512], 32, 192, kr[:], 4, "z0", "kr", dqk, "dqk")
        do_rope(128, rt, rtr, z[:, 1536:2048], 64, 0, mqr[:], None, "z3", "mqr", dqk, "dqk")
        do_rope(128, rt, rtr, z[:, 2048:2560], 64, 0, mk[:], None, "z4", mkres, dqk, "dqk")
        S.dma("sp", lambda e, t=t, mk=mk: e.dma_start(out=kout[t * 128:(t + 1) * 128, :], in_=mk[:]), reads=[mkres])
        S.op("act", lambda e: e.copy(krb[:], kr[:]), reads=["kr"], writes=["krb"])
        transposes_to(lambda b, n: qT[:, b:b + n, :], lambda j: qr[:, j * 128:(j + 1) * 128], 2, ["qr"], ["qT"])
        transposes_to(lambda b, n: kT[:, b:b + n, :], lambda j: kr[:, j * 128:(j + 1) * 128], 2, ["kr"], ["kT"])
        transposes_to(lambda b, n: mqT[:, b:b + n, :], lambda j: mqr[:, j * 128:(j + 1) * 128], 4, ["mqr"], ["mqT"])
        ps, pr = nps()

        def f(e, ps=ps, mk=mk):
            for j in range(4):
                ins = e.transpose(ps[:, j * 128:(j + 1) * 128], mk[:, j * 128:(j + 1) * 128], ident[:])
            return ins
        S.op("pe", f, reads=[mkres, "ident"], writes=[pr])
        S.op("act", lambda e, ps=ps, t=t: e.copy(KT[:, :, t * 128:(t + 1) * 128],
                                                  ps[:, :].rearrange("p (h n) -> p h n", h=4)),
             reads=[pr], writes=["KT"])
        S.op("dve", lambda e, ps=ps, t=t: e.reduce_sum(kmT[:, :, t], ps[:, :].rearrange("p (h n) -> p h n", h=4), AX.X),
             reads=[pr, "KT"], writes=["kmT"])
        if t % 2 == 1:
            n = t // 2
            S.op("dve", lambda e, n=n: e.tensor_tensor(kmsum[:, :, n], kmT[:, :, 2 * n], kmT[:, :, 2 * n + 1], ALU.add),
                 reads=["kmT"], writes=["kmsum"])
            S.op("dve", lambda e, n=n: e.tensor_scalar_mul(kmb[:, :, n], kmsum[:, :, n], 1.0 / 256),
                 reads=["kmsum"], writes=["kmb"])

        S.marks.append((S.seq, 'ret %d' % t))
        pso, pso_r = psf[4], "psf4"
        for h in range(4):
            p_, hh = h // 2, h % 2
            lo, hi = hh * 64, hh * 64 + 64
            ps, pr = nps()
            S.op("pe", lambda e, ps=ps, p_=p_, lo=lo, hi=hi: e.matmul(
                ps[:, 0:128], kT[lo:hi, p_, :], qT[lo:hi, p_, :], start=True, stop=True),
                reads=["kT", "qT"], writes=[pr])
            sm = scTm[h % 2]
            smr = "scTm%d" % (h % 2)
            S.op("dve", lambda e, ps=ps, sm=sm: e.tensor_tensor(sm[:], ps[:, 0:128], triT[:], ALU.mult),
                 reads=[pr, "triT"], writes=[smr])

            def f(e, sm=sm, h=h, p_=p_, lo=lo, hi=hi):
                e.matmul(pso[:, h * 128:(h + 1) * 128], sm[:], rvb[:, h * 128:(h + 1) * 128], start=True, stop=False)
                return e.matmul(pso[:, h * 128:(h + 1) * 128], qT[lo:hi, p_, :], stb[lo:hi, p_, :],
                                start=False, stop=True)
            S.op("pe", f, reads=[smr, "rvb", "qT", "stb"], writes=[pso_r])
        for p_ in range(2):
            ps, pr = nps()
            S.op("pe", lambda e, ps=ps, p_=p_: e.matmul(
                ps[:, 0:256], krb[:, p_ * 128:(p_ + 1) * 128], rvb[:, p_ * 256:(p_ + 1) * 256], start=True, stop=True),
                reads=["krb", "rvb"], writes=[pr])
            for hh in range(2):
                h = 2 * p_ + hh
                lo, hi = hh * 64, hh * 64 + 64
                gC = GAM[h] ** 128
                S.op("dve", lambda e, ps=ps, p_=p_, hh=hh, lo=lo, hi=hi: e.tensor_tensor(
                    sttmp[lo:hi, p_, :], st32[lo:hi, p_, :], ps[lo:hi, hh * 128:(hh + 1) * 128], ALU.add),
                    reads=["st32", pr, pso_r], writes=["sttmp"])
                S.op("dve", lambda e, p_=p_, lo=lo, hi=hi, gC=gC: e.tensor_scalar_mul(
                    st32[lo:hi, p_, :], sttmp[lo:hi, p_, :], gC), reads=["sttmp"], writes=["st32"])
                S.op("act", lambda e, p_=p_, lo=lo, hi=hi, gC=gC: e.mul(
                    stb[lo:hi, p_, :], sttmp[lo:hi, p_, :], gC), reads=["sttmp"], writes=["stb"])
        S.marks.append((S.seq, 'gn %d' % t))
        group_norm(128, pso, pso_r)
        transposes_to(lambda b, n: mixT[:, b:b + n, :], lambda j: yret[:, j * 128:(j + 1) * 128], 4,
                      ["yret"], ["mixT"])

        S.marks.append((S.seq, 'moba %d' % t))
        own = t // 2
        nkt = t + 1
        psO, psO_r = psf[5], "psf5"
        for h in range(4):
            if own >= 4:
                ps, pr = nps()
                S.op("pe", lambda e, ps=ps, h=h: e.matmul(ps[:, 0:8], mqT[:, h, :], kmb[:, h, :], start=True, stop=True),
                     reads=["mqT", "kmb"], writes=[pr])
                S.op("dve", lambda e: e.memset(g8[:], -1e30), writes=["g8"])
                S.op("dve", lambda e, ps=ps, own=own: e.tensor_copy(g8[:, 0:own], ps[:, 0:own]),
                     reads=[pr], writes=["g8"])
                S.op("dve", lambda e: e.max(mx8[:], g8[:]), reads=["g8"], writes=["mx8"])
                S.op("dve", lambda e: e.tensor_scalar(biasn[:], g8[:], mx8[:, 2:3], 1.0, ALU.is_ge, ALU.subtract),
                     reads=["g8", "mx8"], writes=["biasn"])
                S.op("dve", lambda e: e.tensor_scalar_mul(biasn[:], biasn[:], -NEG), reads=["biasn"], writes=["biasn"])
            else:
                S.op("dve", lambda e: e.memset(biasn[:], 0.0), writes=["biasn"])
            S.op("dve", lambda e: e.memset(rs[:], 0.0), writes=["rs"])
            for n0 in range(0, own, 2):
                nb = min(2, own - n0)
                ps, pr = nps()
                S.op("pe", lambda e, ps=ps, h=h, n0=n0, nb=nb: e.matmul(
                    ps[:, 0:nb * 256], mqT[:, h, :], KT[:, h, n0 * 256:(n0 + nb) * 256], start=True, stop=True),
                    reads=["mqT", "KT"], writes=[pr])
                for j in range(nb):
                    n = n0 + j
                    S.op("act", lambda e, ps=ps, j=j, n=n: e.activation(
                        Pm[:, n * 256:(n + 1) * 256], ps[:, j * 256:(j + 1) * 256], AF.Exp,
                        bias=biasn[:, n:n + 1], scale=SCALE, accum_out=rs[:, n:n + 1]),
                        reads=[pr, "biasn", "rs"], writes=["Pm", "rs"])
            ps, pr = nps()
            k0 = own * 256
            nown = (t + 1) * 128 - k0
            S.op("pe", lambda e, ps=ps, h=h, k0=k0, nown=nown: e.matmul(
                ps[:, 0:nown], mqT[:, h, :], KT[:, h, k0:k0 + nown], start=True, stop=True),
                reads=["mqT", "KT"], writes=[pr])
            if nown == 256:
                S.op("act", lambda e, ps=ps, k0=k0: e.activation(
                    Pm[:, k0:k0 + 128], ps[:, 0:128], AF.Exp, scale=SCALE, accum_out=rs[:, 8:9]),
                    reads=[pr, "rs"], writes=["Pm", "rs"])
            d0 = nown - 128
            S.op("dve", lambda e, ps=ps, d0=d0: e.tensor_tensor(sd[:], ps[:, d0:d0 + 128], tribias[:], ALU.add),
                 reads=[pr, "tribias"], writes=["sd"])
            S.op("act", lambda e, t=t: e.activation(Pm[:, t * 128:(t + 1) * 128], sd[:], AF.Exp, scale=SCALE,
                                                    accum_out=rs[:, 9:10]),
                 reads=["sd", "rs"], writes=["Pm", "rs"])
            S.op("dve", lambda e: e.reduce_sum(small[:, 40:41], rs[:, 0:10], AX.X), reads=["rs"], writes=["m0"])
            S.op("dve", lambda e: e.reciprocal(small[:, 41:42], small[:, 40:41]), reads=["m0"], writes=["m1"])
            S.op("dve", lambda e, nkt=nkt: e.tensor_scalar_mul(Pm[:, 0:nkt * 128], Pm[:, 0:nkt * 128], small[:, 41:42]),
                 reads=["Pm", "m1"], writes=["Pm"])
            for k8 in range(0, nkt, 8):
                nn = min(8, nkt - k8)
                pb, pbr = npsb()
                ptt = PT[(k8 // 8) % 2]
                ptr = "PT%d" % ((k8 // 8) % 2)

                def f(e, pb=pb, k8=k8, nn=nn):
                    for j in range(nn):
                        ins = e.transpose(pb[:, j * 128:(j + 1) * 128], Pm[:, (k8 + j) * 128:(k8 + j + 1) * 128],
                                          identb[:])
                    return ins
                S.op("pe", f, reads=["Pm", "identb"], writes=[pbr])
                S.op("dve", lambda e, pb=pb, nn=nn, ptt=ptt: e.tensor_copy(
                    ptt[:, 0:nn, :], pb[:, 0:nn * 128].rearrange("p (a b) -> p a b", a=nn)),
                    reads=[pbr], writes=[ptr])

                def f2(e, ptt=ptt, k8=k8, nn=nn, h=h, nkt=nkt):
                    for j in range(nn):
                        kt = k8 + j
                        ins = e.matmul(psO[:, h * 128:(h + 1) * 128], Vb[:, kt, h * 128:(h + 1) * 128], ptt[:, j, :],
                                       start=(kt == 0), stop=(kt == nkt - 1))
                    return ins
                S.op("pe", f2, reads=[ptr, "Vb"], writes=[psO_r])
        S.op("act", lambda e: e.copy(mixT[:, 4:8, :], psO[:, :].rearrange("p (h n) -> p h n", h=4)),
             reads=[psO_r], writes=["mixT"])

        S.marks.append((S.seq, 'oproj %d' % t))
        pss = [nps(), nps()]
        for hf in range(2):
            def f(e, hf=hf, ps=pss[hf][0]):
                for c in range(8):
                    ins = e.matmul(ps[:, :], mixT[:, c, :], w_o_b[:, c, hf * 512:(hf + 1) * 512],
                                   start=(c == 0), stop=(c == 7))
                return ins
            S.op("pe", f, reads=["mixT", "w_o"], writes=[pss[hf][1]])
        layer_norm([pss[0][0], pss[1][0]], [pss[0][1], pss[1][1]], xt_t, xtr, gate_a, ["gate_a"], x1, "x1")
        S.dma("sp", lambda e, t=t: e.dma_start(out=x1d[t * 128:(t + 1) * 128, :], in_=x1[:]), reads=["x1"],
              writes=["x1d%d" % t])

    for p_ in range(2):
        S.dma("sp", lambda e, p_=p_: e.dma_start(out=sout[p_ * 128:(p_ + 1) * 128, :], in_=st32[:, p_, :]),
              reads=["st32"])

    S.marks.append((S.seq, 'phase2'))
    P1 = WIN + ["w_o"]
    w_up_v = w_up.rearrange("(k p) n -> p k n", p=128)
    for k2 in range(4):
        ld("pool", w_up_b[:, 2 * k2:2 * k2 + 2, :], w_up_v[:, 2 * k2:2 * k2 + 2, :], ["w_up%d" % k2] + P1)
    WUP = ["w_up%d" % i for i in range(4)]
    w_down_v = w_down.rearrange("(k p) n -> p k n", p=128)
    for k4 in range(4):
        ld("pool", w_down_b[:, 8 * k4:8 * k4 + 8, :], w_down_v[:, 8 * k4:8 * k4 + 8, :],
           ["w_dn%d" % k4, "KT", "Vb", "wada0", "wada1"] + TAIL)
    WDN = ["w_dn%d" % i for i in range(4)]
    ld("sp", lng[:], l2g, ["lng"])
    ld("sp", lnb[:], l2b, ["lnb"])
    ld("sp", gate_a[:], gfd, ["gate_a"], reads=["gfd"])
    S.marks.append((S.seq, 'sample ffn'))
    ld("sp", xt[0:P, :], x1d[2048:2080, :], ["xt"], reads=["x1ds"])
    ld("sp", r[0:P, :], modsd[:, 4096:5120], ["r"], reads=["modsd"])
    ld("sp", xn[0:P, :], modsd[:, 3072:4096], ["xn"], reads=["modsd"])
    ld("sp", z[0:P, 2048:3072], modsd[:, 5120:6144], ["z4", "z5"], reads=["modsd"])
    S.op("dve", lambda e: e.scalar_tensor_tensor(r[0:P, :], r[0:P, :], 1.0, xt[0:P, :], ALU.add, ALU.mult),
         reads=["r", "xt"], writes=["r"])
    S.op("dve", lambda e: e.tensor_tensor(r[0:P, :], r[0:P, :], xn[0:P, :], ALU.add), reads=["r", "xn"], writes=["r"])
    tr32(lambda k: r[0:P, k * 128:(k + 1) * 128], 8, ["r"], hT[:, :, 0:32], "hT")
    for g in range(8):
        ps, pr = nps()

        def f(e, ps=ps, g=g):
            for j in range(4):
                fc = g * 4 + j
                for k in range(8):
                    ins = e.matmul(ps[:, j * 32:(j + 1) * 32], w_up_b[:, k, fc * 128:(fc + 1) * 128], hT[:, k, 0:32],
                                   start=(k == 0), stop=(k == 7))
            return ins
        S.op("pe", f, reads=["hT"] + WUP, writes=[pr])
        rr = rl[g % 2]
        rrr = "rl%d" % (g % 2)
        S.op("dve", lambda e, ps=ps, rr=rr: e.tensor_scalar_max(rr[:, 0:128], ps[:, 0:128], 0.0), reads=[pr], writes=[rrr])
        S.op("act", lambda e, rr=rr, g=g: e.activation(
            uT[:, 4 * g:4 * g + 4, 0:32], rr[:, 0:128].rearrange("p (a b) -> p a b", a=4), AF.Square),
            reads=[rrr], writes=["uT"])
    pss = [nps(), nps()]
    for hf in range(2):
        def f(e, hf=hf, ps=pss[hf][0]):
            for c in range(32):
                ins = e.matmul(ps[0:P, :], uT[:, c, 0:32], w_down_b[:, c, hf * 512:(hf + 1) * 512],
                               start=(c == 0), stop=(c == 31))
            return ins
        S.op("pe", f, reads=["uT"] + WDN, writes=[pss[hf][1]])
    layer_norm([pss[0][0], pss[1][0]], [pss[0][1], pss[1][1]], xt, "xt", z[:, 2048:3072], ["z4", "z5"], x1, "x1", P=P)
    S.dma("sp", lambda e: e.dma_start(out=ys, in_=x1[0:P, :]), reads=["x1"])
    def load_tile2(t):
        ld("sp", xts[t % 2][:], x1d[t * 128:(t + 1) * 128, :], ["xt%d" % (t % 2)], reads=["x1d%d" % t])

    for t in range(NT):
        xt_t, hT_t = xts[t % 2], hTs[t % 2]
        xtr, hTr = "xt%d" % (t % 2), "hT%d" % (t % 2)
        if t == 0:
            load_tile2(0)
        transposes_to(lambda b, n, hT_t=hT_t: hT_t[:, b, :], lambda k, xt_t=xt_t: xt_t[:, k * 128:(k + 1) * 128],
                      8, [xtr], [hTr], scale_fn=lambda k: (modT[:, 32 + k:33 + k], modT[:, 24 + k:25 + k]))
        if t + 1 < NT:
            load_tile2(t + 1)
        for g in range(8):
            ps, pr = nps()

            def f(e, ps=ps, g=g, hT_t=hT_t):
                for j in range(4):
                    fc = g * 4 + j
                    for k in range(8):
                        ins = e.matmul(ps[:, j * 128:(j + 1) * 128], w_up_b[:, k, fc * 128:(fc + 1) * 128], hT_t[:, k, :],
                                       start=(k == 0), stop=(k == 7))
                return ins
            S.op("pe", f, reads=[hTr] + WUP, writes=[pr])
            rr = rl[g % 2]
            rrr = "rl%d" % (g % 2)
            S.op("dve", lambda e, ps=ps, rr=rr: e.tensor_scalar_max(rr[:], ps[:, :], 0.0), reads=[pr], writes=[rrr])
            S.op("act", lambda e, rr=rr, g=g: e.activation(
                uT[:, 4 * g:4 * g + 4, :], rr[:].rearrange("p (a b) -> p a b", a=4), AF.Square),
                reads=[rrr], writes=["uT"])
        pss = [nps(), nps()]
        for hf in range(2):
            def f(e, hf=hf, ps=pss[hf][0]):
                for c in range(32):
                    ins = e.matmul(ps[:, :], uT[:, c, :], w_down_b[:, c, hf * 512:(hf + 1) * 512],
                                   start=(c == 0), stop=(c == 31))
                return ins
            S.op("pe", f, reads=["uT"] + WDN, writes=[pss[hf][1]])
        layer_norm([pss[0][0], pss[1][0]], [pss[0][1], pss[1][1]], xt_t, xtr, gate_a, ["gate_a"], x1, "x1")
        S.dma("sp", lambda e, t=t: e.dma_start(out=y[t * 128:(t + 1) * 128, :], in_=x1[:]), reads=["x1"])

    S.marks.append((S.seq, 'end'))
    if marks is not None:
        marks.extend(S.marks)
    sems = {}
    for k in ("pe", "act", "dve", "pool"):
        sems[k] = nc.alloc_semaphore("s_" + k)
    for i in range(S.n_dma):
        sems["d%d" % i] = nc.alloc_semaphore("s_d%d" % i)
    with nc.Block() as block:
        @block.sync
        def _(e):
            S.emit("sp", e, sems, final_wait=True)

        @block.tensor
        def _(e):
            S.emit("pe", e, sems)

        @block.scalar
        def _(e):
            S.emit("act", e, sems)

        @block.vector
        def _(e):
            S.emit("dve", e, sems)

        @block.gpsimd
        def _(e):
            S.emit("pool", e, sems)
    return nc


_CONST = {}


def _consts():
    if _CONST:
        return _CONST
    pos = np.arange(2048, dtype=np.float32)

    def tab(half):
        inv = np.power(np.float32(10000.0), -np.arange(half, dtype=np.float32) / np.float32(half)).astype(np.float32)
        ang = (pos[:, None] * inv[None, :]).astype(np.float32)
        c, s = np.cos(ang).astype(np.float32), np.sin(ang).astype(np.float32)
        return np.ascontiguousarray(np.concatenate([c, -s, s], axis=1))
    _CONST["ropeM"] = tab(64)
    _CONST["ropeR"] = tab(32)
    i = np.arange(128, dtype=np.float64)
    dq = np.stack([np.power(GAM[h], i + 1.0) for h in range(4)], axis=1)
    dk = np.stack([np.power(GAM[h], -(i + 1.0)) / 8.0 for h in range(4)], axis=1)
    _CONST["dqk"] = np.ascontiguousarray(np.concatenate([dq, dk], axis=1).astype(np.float32))
    jj, ii = np.meshgrid(np.arange(128), np.arange(128), indexing="ij")
    _CONST["triT"] = (ii >= jj).astype(np.float32)
    _CONST["tribias"] = np.where(ii >= jj, 0.0, NEG).astype(np.float32).T.copy()
    _CONST["ident"] = np.eye(128, dtype=np.float32)
    return _CONST


def _sample_consts():
    if "ropeS" in _CONST:
        return _CONST
    pos = (8192 + (np.arange(32) % 8)).astype(np.float32)

    def tab(half):
        inv = np.power(np.float32(10000.0), -np.arange(half, dtype=np.float32) / np.float32(half)).astype(np.float32)
        ang = (pos[:, None] * inv[None, :]).astype(np.float32)
        c, s = np.cos(ang).astype(np.float32), np.sin(ang).astype(np.float32)
        return np.concatenate([c, -s, s], axis=1)
    _CONST["ropeS"] = np.ascontiguousarray(np.concatenate([tab(64), tab(32)], axis=1))
    i = (np.arange(32) % 8).astype(np.float64)
    dq = np.stack([np.power(GAM[h], i + 1.0) for h in range(4)], axis=1)
    dk = np.stack([np.power(GAM[h], -(i + 1.0)) / 8.0 for h in range(4)], axis=1)
    _CONST["dqks"] = np.ascontiguousarray(np.concatenate([dq, dk], axis=1).astype(np.float32))
    rr = np.arange(32)
    _CONST["maskS"] = ((rr[:, None] // 8 == rr[None, :] // 8) & (rr[None, :] >= rr[:, None])).astype(np.float32)
    cm = (rr[None, :] // 8 == np.arange(4)[:, None]).astype(np.float32).reshape(1, 128)
    _CONST["cm"] = np.ascontiguousarray(np.broadcast_to(cm, (128, 128)))
    _CONST["rm"] = (rr[:, None] // 8 == np.arange(4)[None, :]).astype(np.float32)
    q = np.arange(128)
    _CONST["gtab"] = np.array([[GAM[2 * a + (qq // 64)] ** 8 for a in range(2)] for qq in q], dtype=np.float32)
    cmn = np.zeros((32, 4, 4, 8), np.float32)
    for sp in range(4):
        for j in range(8):
            for qq in range(8):
                if j <= qq:
                    cmn[sp * 8 + j, sp, :, qq] = 1.0
    _CONST["cmaskN"] = cmn.reshape(32, 128)
    _CONST["pcol"] = np.arange(128, dtype=np.float32)[:, None].copy()
    return _CONST


def make_in_maps(x_prompt, x_sample, cache_k, cache_v, state_ret, page_table, c_prompt, c_sample,
                 w_ada, b_ada, w_in, w_o, ln1_g, ln1_b, w_up, w_down, ln2_g, ln2_b, cores=range(8)):
    f = lambda a: np.ascontiguousarray(np.asarray(a, dtype=np.float32))
    C = _consts()
    _sample_consts()
    b_ada0 = f(b_ada)[0]
    ckf = f(cache_k).reshape(-1, 512)
    cvf = f(cache_v).reshape(-1, 512)
    shared = {
        "w_ada": f(w_ada)[0], "badaT": np.ascontiguousarray(b_ada0.reshape(48, 128).T),
        "bga": np.ascontiguousarray(np.broadcast_to(b_ada0[2048:3072], (128, 1024))),
        "bgf": np.ascontiguousarray(np.broadcast_to(b_ada0[5120:6144], (128, 1024))),
        "bada32": np.ascontiguousarray(np.broadcast_to(b_ada0, (32, 6144))),
        "w_in": f(w_in)[0], "w_o": f(w_o)[0], "w_up": f(w_up)[0], "w_down": f(w_down)[0],
        "l1g": np.ascontiguousarray(np.broadcast_to(f(ln1_g)[0], (128, 1024))),
        "l1b": np.ascontiguousarray(np.broadcast_to(f(ln1_b)[0], (128, 1024))),
        "l2g": np.ascontiguousarray(np.broadcast_to(f(ln2_g)[0], (128, 1024))),
        "l2b": np.ascontiguousarray(np.broadcast_to(f(ln2_b)[0], (128, 1024))),
        "ck": ckf, "cv": cvf,
    }
    for k in ("ident", "ropeM", "ropeR", "dqk", "triT", "tribias", "ropeS", "dqks", "maskS", "cm", "rm", "gtab",
              "cmaskN", "pcol"):
        shared[k] = C[k]
    xp, xsm, cp, csm = f(x_prompt), f(x_sample), f(c_prompt), f(c_sample)
    st = f(state_ret)[0]
    pt = np.asarray(page_table).astype(np.int32)
    in_maps = []
    for c in cores:
        m = dict(shared)
        m["x"] = xp[c]
        m["cT"] = np.ascontiguousarray(cp[c].reshape(8, 128).T)
        m["xs"] = np.ascontiguousarray(xsm[4 * c:4 * c + 4].reshape(32, 1024))
        crow = np.repeat(csm[4 * c:4 * c + 4], 8, axis=0)
        m["csT"] = np.ascontiguousarray(crow.reshape(32, 8, 128).transpose(2, 1, 0).reshape(128, 256))
        m["ptb"] = np.ascontiguousarray(np.broadcast_to(pt[4 * c:4 * c + 4].reshape(1, 256), (128, 256)))
        m["st_in"] = np.ascontiguousarray(st[4 * c:4 * c + 4].reshape(4, 256, 128))
        in_maps.append(m)
    return in_maps


def kernel(x_prompt, x_sample, cache_k, cache_v, state_ret, page_table, c_prompt, c_sample,
           w_ada, b_ada, w_in, w_o, ln1_g, ln1_b, w_up, w_down, ln2_g, ln2_b):
    nc = build_nc()
    in_maps = make_in_maps(x_prompt, x_sample, cache_k, cache_v, state_ret, page_table, c_prompt, c_sample,
                           w_ada, b_ada, w_in, w_o, ln1_g, ln1_b, w_up, w_down, ln2_g, ln2_b)
    res = run_bass_kernel_spmd(nc, in_maps, core_ids=list(range(8)))
    R = res.results
    g = lambda c, k: np.asarray(R[c][k]).astype(np.float32)
    y_p = np.stack([g(c, "y") for c in range(8)])
    k_p = np.stack([g(c, "kout").reshape(2048, 4, 128) for c in range(8)])[None]
    v_p = np.stack([g(c, "vout").reshape(2048, 4, 128) for c in range(8)])[None]
    s_p = np.stack([g(c, "sout").reshape(4, 64, 128) for c in range(8)])[None]
    y_s = np.concatenate([g(c, "ys").reshape(4, 8, 1024) for c in range(8)])
    k_s = np.concatenate([g(c, "ks").reshape(4, 8, 4, 128) for c in range(8)])[None]
    v_s = np.concatenate([g(c, "vs").reshape(4, 8, 4, 128) for c in range(8)])[None]
    s_s = np.concatenate([g(c, "ss").reshape(4, 4, 64, 128) for c in range(8)])[None]
    return (y_p, y_s, k_p, v_p, s_p, k_s, v_s, s_s)
```

```python
import math
import numpy as np
import concourse.bass as bass
import concourse.mybir as mybir
from concourse.bass_utils import run_bass_kernel_spmd

F32 = mybir.dt.float32
BF16 = mybir.dt.bfloat16
I32 = mybir.dt.int32
AF = mybir.ActivationFunctionType
ALU = mybir.AluOpType
AX = mybir.AxisListType

NT = 16
ALPHA = 2.0 ** 0.25
LN_EPS = 1e-5
GN_EPS = 1e-6
NEG = -30000.0
SCALE = 128.0 ** -0.5
GAM = [1.0 - 2.0 ** (-5.0 - h) for h in range(4)]


class Sched:
    ENG = ("pe", "act", "dve", "pool", "sp")

    def __init__(self, n_dma):
        self.ops = {e: [] for e in self.ENG}
        self.cnt = {e: 0 for e in self.ENG}
        self.lastw = {}
        self.readers = {}
        self.n_dma = n_dma
        self.dma_val = [0] * n_dma
        self.dma_next = 0
        self.seq = 0
        self.cut = None
        self.marks = []
        self.alias = {"ta": ["xnA"], "tb": ["xnB"], "sq": ["xnA", "xnB"], "xn": ["xnA", "xnB"],
                      "yn": ["rA"], "yret": ["rB"], "r": ["rA", "rB"], "sg": ["x1A"], "x1": ["x1A", "x1B"],
                      "rl0": ["z0"], "rl1": ["z1"], "uT": ["Pm", "PT0", "PT1"], "mkr1": ["rB"], "mkr0": ["rB"],
                      "mqr": ["rA"], "scTb": ["Pm"],
                      "KT": ["KT", "KTg0", "KTg1"] + ["Kg%d_%d" % (a, b) for a in range(2) for b in range(4)],
                      "Vb": ["Vb", "PTs", "selBs", "Dm"] + ["Vg%d_%d" % (a, b) for a in range(2) for b in range(4)],
                      "cm": ["rtab0"], "cmaskN": ["rtab0"], "xt": ["xt0"], "hT": ["hT0"]}

    def _exp(self, names):
        out = []
        for n in names:
            out += self.alias.get(n, [n])
        return out

    def _deps(self, reads, writes):
        reads, writes = self._exp(reads), self._exp(writes)
        toks = []
        for r in reads:
            if r in self.lastw:
                toks.append(self.lastw[r])
        for w in writes:
            if w in self.lastw:
                toks.append(self.lastw[w])
            toks += self.readers.get(w, [])
        return toks

    def _commit(self, tok, reads, writes):
        reads, writes = self._exp(reads), self._exp(writes)
        for r in reads:
            self.readers.setdefault(r, []).append(tok)
        for w in writes:
            self.lastw[w] = tok
            self.readers[w] = []

    def op(self, eng, fn, reads=(), writes=()):
        writes = list(writes) + [r for r in reads if r.startswith("ps")]
        toks = self._deps(reads, writes)
        self.cnt[eng] += 1
        tok = (eng, self.cnt[eng])
        self.seq += 1
        self.ops[eng].append((fn, toks, tok, self.seq))
        self._commit(tok, reads, writes)

    def dma(self, q, fn, reads=(), writes=()):
        toks = self._deps(reads, writes)
        i = self.dma_next
        self.dma_next = (i + 1) % self.n_dma
        if self.dma_val[i] > 0:
            toks.append(("d%d" % i, self.dma_val[i]))
        self.dma_val[i] += 16
        tok = ("d%d" % i, self.dma_val[i])
        self.seq += 1
        self.ops[q].append((fn, toks, tok, self.seq))
        self._commit(tok, reads, writes)

    def emit(self, eng_name, e, sems, final_wait=False):
        seen = {}
        for fn, toks, tok, seq in self.ops[eng_name]:
            if self.cut is not None and seq > self.cut:
                continue
            need = {}
            for k, v in toks:
                if k == eng_name and eng_name in ("pe", "sp"):
                    continue
                if v > need.get(k, 0):
                    need[k] = v
            for k, v in need.items():
                if seen.get(k, 0) >= v:
                    continue
                e.wait_ge(sems[k], v)
                seen[k] = v
            ins = fn(e)
            ins.then_inc(sems[tok[0]], 16 if tok[0][1:].isdigit() else 1)
        if final_wait:
            fin = {}
            for en in self.ENG:
                for fn, toks, tok, seq in self.ops[en]:
                    if self.cut is not None and seq > self.cut:
                        continue
                    if tok[0][1:].isdigit():
                        fin[tok[0]] = max(fin.get(tok[0], 0), tok[1])
            for k, v in fin.items():
                e.wait_ge(sems[k], v)


def build_nc(cut=None, marks=None):
    nc = bass.Bass("TRN2", target_bir_lowering=False)

    def din(name, shape, dt=F32):
        return nc.dram_tensor(name, shape, dt, kind="ExternalInput").ap()

    def dout(name, shape, dt=F32):
        return nc.dram_tensor(name, shape, dt, kind="ExternalOutput").ap()

    x = din("x", [2048, 1024])
    cT = din("cT", [128, 8])
    w_ada = din("w_ada", [1024, 6144])
    badaT = din("badaT", [128, 48])
    bga = din("bga", [128, 1024])
    bgf = din("bgf", [128, 1024])
    w_in = din("w_in", [1024, 3072])
    w_o = din("w_o", [1024, 1024])
    w_up = din("w_up", [1024, 4096])
    w_down = din("w_down", [4096, 1024])
    l1g = din("l1g", [128, 1024])
    l1b = din("l1b", [128, 1024])
    l2g = din("l2g", [128, 1024])
    l2b = din("l2b", [128, 1024])
    ident_d = din("ident", [128, 128])
    ropeM = din("ropeM", [2048, 192])
    ropeR = din("ropeR", [2048, 96])
    dqk_d = din("dqk", [128, 8])
    triT_d = din("triT", [128, 128])
    tribias_d = din("tribias", [128, 128])
    y = dout("y", [2048, 1024])
    kout = dout("kout", [2048, 512])
    vout = dout("vout", [2048, 512])
    sout = dout("sout", [256, 128])
    x1d = nc.dram_tensor("x1d", [2080, 1024], F32).ap()
    gfd = nc.dram_tensor("gfd", [128, 1024], F32).ap()
    modsd = nc.dram_tensor("modsd", [32, 6144], F32).ap()
    xs_d = din("xs", [32, 1024])
    csT_d = din("csT", [128, 256])
    bada32 = din("bada32", [32, 6144])
    ptb_d = din("ptb", [128, 128], I32)
    pcol_d = din("pcol", [128, 1])
    st_in = din("st_in", [4, 256, 128])
    ck = din("ck", [163840, 1024])
    cv = din("cv", [163840, 1024])
    ropeS_d = din("ropeS", [32, 288])
    dqks_d = din("dqks", [32, 8])
    maskS_d = din("maskS", [32, 32])
    cm_d = din("cm", [128, 128])
    rm_d = din("rm", [32, 4])
    gtab_d = din("gtab", [128, 2])
    cmaskN_d = din("cmaskN", [32, 128])
    ys = dout("ys", [32, 1024])
    ks = dout("ks", [32, 512])
    vs = dout("vs", [32, 512])
    ss = dout("ss", [4, 256, 128])

    def sb(name, shape, dt=F32):
        return nc.alloc_sbuf_tensor(name, shape, dt)

    W = sb("W", [128, 65536], BF16)
    w_in_b = W[:, 0:24576].rearrange("p (k n) -> p k n", k=8)
    w_o_b = W[:, 24576:32768].rearrange("p (k n) -> p k n", k=8)
    KT = W[:, 32768:40960].rearrange("p (h n) -> p h n", h=4)
    Vb = W[:, 40960:49152].rearrange("p (t n) -> p t n", t=16)
    wada = [W[:, 49152 + i * 4096:49152 + (i + 1) * 4096].rearrange("p (k n) -> p k n", k=8) for i in range(2)]
    w_up_b = W[:, 0:32768].rearrange("p (k n) -> p k n", k=8)
    w_down_b = W[:, 32768:65536].rearrange("p (k n) -> p k n", k=32)

    ident = sb("ident_s", [128, 128])
    identb = sb("identb", [128, 128], BF16)
    triT = sb("triT_s", [128, 128])
    tribias = sb("tribias_s", [128, 128])
    dqk = sb("dqk_s", [128, 8])
    cTs = sb("cTs", [128, 8])
    scT = sb("scT", [128, 8], BF16)
    badaTs = sb("badaTs", [128, 48])
    modT = sb("modT", [128, 48])
    gate_a = sb("gate_a", [128, 1024])
    lng = sb("lng", [128, 1024])
    lnb = sb("lnb", [128, 1024])
    xts = [sb("xt%d" % i, [128, 1024]) for i in range(2)]
    hTs = [sb("hT%d" % i, [128, 8, 128], BF16) for i in range(2)]
    xt, hT = xts[0], hTs[0]
    z = sb("z", [128, 3072])
    Bar = sb("Bar", [128, 4096], BF16)
    rtab = [sb("rtab%d" % i, [128, 288]) for i in range(2)]
    cm = rtab[0][:, 0:128].rearrange("p (a b) -> p a b", a=4)
    cmaskN = rtab[0][0:32, 128:256].rearrange("p (a b) -> p a b", a=4)
    qr = sb("qr", [128, 256])
    kr = sb("kr", [128, 256])
    krb = sb("krb", [128, 256], BF16)
    rvb = sb("rvb", [128, 512], BF16)
    qT = sb("qT", [128, 2, 128], BF16)
    kT = sb("kT", [128, 2, 128], BF16)
    mqT = sb("mqT", [128, 4, 128], BF16)
    kmT = sb("kmT", [128, 4, 16])
    kmsum = sb("kmsum", [128, 4, 8])
    kmb = sb("kmb", [128, 4, 8], BF16)
    st32 = sb("st32", [128, 2, 128])
    sttmp = sb("sttmp", [128, 2, 128])
    stb = sb("stb", [128, 2, 128], BF16)
    scTm = [sb("scTm%d" % i, [128, 128], BF16) for i in range(2)]
    mixT = sb("mixT", [128, 8, 128], BF16)
    small = sb("small", [128, 64])
    g8 = sb("g8", [128, 8])
    mx8 = sb("mx8", [128, 8])
    biasn = sb("biasn", [128, 8])
    rs = sb("rs", [128, 16])
    Pm = Bar[:, 0:2048]
    sd = sb("sd", [128, 128])
    PT = [Bar[:, 2048 + i * 1024:3072 + i * 1024].rearrange("p (a b) -> p a b", a=8) for i in range(2)]
    r = sb("r", [128, 1024])
    xn = sb("xn", [128, 1024])
    x1 = sb("x1", [128, 1024])
    ta = xn[:, 0:512]
    tb = xn[:, 512:1024]
    sq = xn
    yn = r[:, 0:512]
    yret = r[:, 512:1024]
    sg = x1[:, 0:512]
    rl = [z[:, i * 512:(i + 1) * 512] for i in range(2)]
    uT = Bar[:, :].rearrange("p (a b) -> p a b", a=32)
    scTb = Bar[:, 0:1024].rearrange("p (a b) -> p a b", a=8)
    mqr = r[:, 0:512]
    mkr = [r[:, 512:1024]] * 2
    Kg = [W[:, 32768 + i * 2048:32768 + (i + 1) * 2048].rearrange("p (a b) -> p a b", a=4) for i in range(2)]
    KTg = [W[:, 36864 + i * 2048:36864 + (i + 1) * 2048].rearrange("p (a b) -> p a b", a=16) for i in range(2)]
    Vg = [W[:, 40960 + i * 2048:40960 + (i + 1) * 2048].rearrange("p (a b) -> p a b", a=4) for i in range(2)]
    PTs = W[:, 45056:47104].rearrange("p (a b) -> p a b", a=64)
    selBs = W[:, 47104:48128].rearrange("p (a b) -> p a b", a=32)
    Dm = W[:, 48128:49152]
    sts32 = W[:, 57344:59392].bitcast(F32).rearrange("p (s a e) -> p s a e", s=4, a=2)
    Sall = [W[:, 59392 + i * 2048:59392 + (i + 1) * 2048].bitcast(F32).rearrange("p (a b) -> p a b", a=32)
            for i in range(2)]
    stsb = W[:, 63488:64512].rearrange("p (s a e) -> p s a e", s=4, a=2)
    krbm = W[:, 64512:65536].rearrange("p (s n) -> p s n", s=4)
    TAIL = ["sts32", "Sall0", "Sall1", "stsb", "krbm"]
    csTs = sb("csTs", [128, 256])
    scTs = sb("scTs", [128, 8, 32], BF16)
    ptb_s = sb("ptb_s", [128, 128], I32)
    idx = sb("idx", [128, 128], I32)
    pcol_s = sb("pcol_s", [128, 1])
    dqks = sb("dqks_s", [32, 8])
    maskS = sb("maskS_s", [32, 32])
    rm = sb("rm_s", [32, 4])
    gtab = sb("gtab_s", [128, 2])
    qTs = sb("qTs", [128, 2, 32], BF16)
    kTs = sb("kTs", [128, 2, 32], BF16)
    qTsm = sb("qTsm", [128, 2, 4, 32], BF16)
    mqTs = sb("mqTs", [128, 4, 32], BF16)
    mkTs = sb("mkTs", [128, 4, 32], BF16)
    vnb = sb("vnb", [32, 512], BF16)
    PN = sb("PN", [32, 32], BF16)
    En = sb("En", [32, 32])
    gsb = sb("gsb", [32, 64])
    g32 = sb("g32", [32, 32])
    mxs = sb("mxs", [32, 8])
    sel = sb("sel", [32, 32])
    rinv = sb("rinv", [128, 32])
    ones_f = sb("ones_f", [128, 1])
    ones_b = sb("ones_b", [128, 128], BF16)

    psf = [nc.alloc_psum_tensor("psf%d" % i, [128, 512], F32) for i in range(6)]
    psb = [nc.alloc_psum_tensor("psb%d" % i, [128, 1024], BF16) for i in range(2)]

    S = Sched(n_dma=20)
    S.cut = cut
    pc = [0, 0]

    def nps():
        i = pc[0] % 4
        pc[0] += 1
        return psf[i], "psf%d" % i

    def npsb():
        i = pc[1] % 2
        pc[1] += 1
        return psb[i], "psb%d" % i

    def bc(ap, shape):
        return ap.to_broadcast(shape)

    def ld(q, dst, src, res, reads=()):
        S.dma(q, lambda e, d=dst, s=src: e.dma_start(out=d, in_=s), reads=reads, writes=res)

    ld("sp", ident[:], ident_d, ["ident"])
    ld("sp", triT[:], triT_d, ["triT"])
    ld("sp", tribias[:], tribias_d, ["tribias"])
    ld("sp", dqk[:], dqk_d, ["dqk"])
    ld("sp", cTs[:], cT, ["cTs"])
    ld("sp", badaTs[:], badaT, ["badaTs"])
    ld("sp", gate_a[:], bga, ["gate_a"])
    ld("sp", xn[:], bgf, ["xn"])
    S.op("dve", lambda e: e.tensor_copy(identb[:], ident[:]), reads=["ident"], writes=["identb"])
    S.op("dve", lambda e: e.memset(st32[:], 0.0), writes=["st32"])
    S.op("dve", lambda e: e.memset(stb[:], 0.0), writes=["stb"])
    S.op("dve", lambda e: e.memset(kmsum[:], 0.0), writes=["kmsum"])
    S.op("dve", lambda e: e.memset(kmb[:], 0.0), writes=["kmb"])

    S.op("act", lambda e: e.activation(scT[:], cTs[:], AF.Silu), reads=["cTs"], writes=["scT"])
    S.op("dve", lambda e: e.tensor_copy(scTb[:], bc(scT[:].unsqueeze(2), [128, 8, 128])),
         reads=["scT"], writes=["scTb"])
    ld("sp", csTs[:], csT_d, ["csTs"])
    ld("sp", ptb_s[:], ptb_d, ["ptb_s"])
    ld("sp", pcol_s[:], pcol_d, ["pcol_s"])
    ld("sp", dqks[:], dqks_d, ["dqks"])
    ld("sp", maskS[:], maskS_d, ["maskS"])
    ld("sp", cm, cm_d.rearrange("p (a b) -> p a b", a=4), ["cm"])
    ld("sp", rm[:], rm_d, ["rm"])
    ld("sp", gtab[:], gtab_d, ["gtab"])
    ld("sp", cmaskN, cmaskN_d.rearrange("p (a b) -> p a b", a=4), ["cmaskN"])
    S.op("dve", lambda e: e.memset(ones_f[:], 1.0), writes=["ones_f"])
    S.op("dve", lambda e: e.memset(ones_b[:], 1.0), writes=["ones_b"])
    S.op("act", lambda e: e.activation(scTs[:], csTs[:].rearrange("p (k r) -> p k r", k=8), AF.Silu),
         reads=["csTs"], writes=["scTs"])
    S.op("dve", lambda e: e.tensor_scalar(idx[:], ptb_s[:], 64.0, pcol_s[:, 0:1], ALU.mult, ALU.add),
         reads=["ptb_s", "pcol_s"], writes=["idx"])
    w_ada_v = w_ada.rearrange("(k p) n -> p k n", p=128)
    psmod, psmod_r = psf[5], "psf5"
    for g in range(12):
        sl = g % 2
        ld("pool", wada[sl], w_ada_v[:, :, g * 512:(g + 1) * 512], ["wada%d" % sl])
        ps2, pr2 = nps()

        def f(e, ps2=ps2, sl=sl):
            for k in range(8):
                ins = e.matmul(ps2[0:32, :], scTs[:, k, :], wada[sl][:, k, :], start=(k == 0), stop=(k == 7))
            return ins
        S.op("pe", f, reads=["scTs", "wada%d" % sl], writes=[pr2])
        ld("sp", mqr[0:32, :], bada32[:, g * 512:(g + 1) * 512], ["mqr"])
        S.op("dve", lambda e, ps2=ps2: e.tensor_tensor(mkr[0][0:32, :], ps2[0:32, :], mqr[0:32, :], ALU.add),
             reads=[pr2, "mqr"], writes=["mkr0"])
        S.dma("sp", lambda e, g=g: e.dma_start(out=modsd[:, g * 512:(g + 1) * 512], in_=mkr[0][0:32, :]),
              reads=["mkr0"], writes=["modsd"])
        if g in (4, 5, 10, 11):
            ps, pr = nps()

            def f(e, ps=ps, sl=sl):
                for k in range(8):
                    ins = e.matmul(ps[:, :], scTb[:, k, :], wada[sl][:, k, :], start=(k == 0), stop=(k == 7))
                return ins
            S.op("pe", f, reads=["scTb", "wada%d" % sl], writes=[pr])
            dst = gate_a if g < 6 else xn
            half = g % 2
            S.op("dve", lambda e, ps=ps, dst=dst, half=half: e.tensor_tensor(
                dst[:, half * 512:(half + 1) * 512], ps[:, :], dst[:, half * 512:(half + 1) * 512], ALU.add),
                reads=[pr], writes=["gate_a" if g < 6 else "xn"])
        else:
            def f(e, sl=sl, g=g):
                for j in range(4):
                    col = g * 4 + j
                    for k in range(8):
                        ins = e.matmul(psmod[:, col:col + 1], wada[sl][:, k, j * 128:(j + 1) * 128],
                                       scT[:, k:k + 1], start=(k == 0), stop=(k == 7))
                return ins
            S.op("pe", f, reads=["scT", "wada%d" % sl], writes=[psmod_r])
    S.op("dve", lambda e: e.tensor_tensor(modT[:], psmod[:, 0:48], badaTs[:], ALU.add),
         reads=[psmod_r, "badaTs"], writes=["modT"])
    S.op("dve", lambda e: e.tensor_scalar_add(modT[:, 8:16], modT[:, 8:16], 1.0), reads=["modT"], writes=["modT"])
    S.op("dve", lambda e: e.tensor_scalar_add(modT[:, 32:40], modT[:, 32:40], 1.0), reads=["modT"], writes=["modT"])

    S.dma("sp", lambda e: e.dma_start(out=gfd, in_=xn[:]), reads=["xn"], writes=["gfd"])
    w_in_v = w_in.rearrange("(k p) n -> p k n", p=128)
    for k2 in range(4):
        ld("pool", w_in_b[:, 2 * k2:2 * k2 + 2, :], w_in_v[:, 2 * k2:2 * k2 + 2, :], ["w_in%d" % k2])
    WIN = ["w_in%d" % i for i in range(4)]
    ld("pool", w_o_b, w_o.rearrange("(k p) n -> p k n", p=128), ["w_o"])
    ld("sp", lng[:], l1g, ["lng"])
    ld("sp", lnb[:], l1b, ["lnb"])

    def transposes_to(dst_fn, src_fn, n, reads, writes_res, evac_eng="act", scale_fn=None):
        for b0 in range(0, n, 4):
            nb = min(4, n - b0)
            ps, pr = nps()

            def f(e, ps=ps, b0=b0, nb=nb):
                for j in range(nb):
                    ins = e.transpose(ps[:, j * 128:(j + 1) * 128], src_fn(b0 + j), ident[:])
                return ins
            S.op("pe", f, reads=list(reads) + ["ident"], writes=[pr])
            if scale_fn is None:
                if evac_eng == "act":
                    S.op("act", lambda e, ps=ps, b0=b0, nb=nb: e.copy(dst_fn(b0, nb), ps[:, 0:nb * 128].rearrange("p (a b) -> p a b", a=nb)),
                         reads=[pr], writes=writes_res)
                else:
                    S.op("dve", lambda e, ps=ps, b0=b0, nb=nb: e.tensor_copy(dst_fn(b0, nb), ps[:, 0:nb * 128].rearrange("p (a b) -> p a b", a=nb)),
                         reads=[pr], writes=writes_res)
            else:
                for j in range(nb):
                    sc, bi = scale_fn(b0 + j)
                    S.op("act", lambda e, ps=ps, j=j, b0=b0, sc=sc, bi=bi: e.activation(
                        dst_fn(b0 + j, 1), ps[:, j * 128:(j + 1) * 128], AF.Identity, bias=bi, scale=sc),
                        reads=[pr, "modT"], writes=writes_res)

    def layer_norm(src_ps_list, src_res, resid, resid_res, gate, gate_res, out_t, out_res, P=128):
        for hf in range(2):
            S.op("dve", lambda e, hf=hf: e.tensor_tensor(
                r[0:P, hf * 512:(hf + 1) * 512], src_ps_list[hf][0:P, :], gate[0:P, hf * 512:(hf + 1) * 512], ALU.mult),
                reads=[src_res[hf]] + list(gate_res), writes=["r"])
        S.op("dve", lambda e: e.scalar_tensor_tensor(r[0:P, :], resid[0:P, :], ALPHA, r[0:P, :], ALU.mult, ALU.add),
             reads=[resid_res, "r"], writes=["r"])
        S.op("dve", lambda e: e.reduce_sum(small[0:P, 0:1], r[0:P, :], AX.X), reads=["r"], writes=["sm0"])
        S.op("act", lambda e: e.activation(sq[0:P, :], r[0:P, :], AF.Square), reads=["r"], writes=["sq"])
        S.op("dve", lambda e: e.reduce_sum(small[0:P, 1:2], sq[0:P, :], AX.X), reads=["sq"], writes=["sm1"])
        S.op("dve", lambda e: e.tensor_scalar_mul(small[0:P, 2:3], small[0:P, 0:1], 1.0 / 1024), reads=["sm0"], writes=["sm2"])
        S.op("dve", lambda e: e.tensor_tensor(small[0:P, 3:4], small[0:P, 2:3], small[0:P, 2:3], ALU.mult),
             reads=["sm2"], writes=["sm3"])
        S.op("dve", lambda e: e.scalar_tensor_tensor(small[0:P, 4:5], small[0:P, 1:2], 1.0 / 1024, small[0:P, 3:4],
                                                     ALU.mult, ALU.subtract), reads=["sm1", "sm3"], writes=["sm4"])
        S.op("dve", lambda e: e.tensor_scalar_add(small[0:P, 4:5], small[0:P, 4:5], LN_EPS), reads=["sm4"], writes=["sm4"])
        S.op("act", lambda e: e.sqrt(small[0:P, 7:8], small[0:P, 4:5]), reads=["sm4"], writes=["sm7"])
        S.op("dve", lambda e: e.reciprocal(small[0:P, 5:6], small[0:P, 7:8]), reads=["sm7"], writes=["sm5"])
        S.op("dve", lambda e: e.scalar_tensor_tensor(small[0:P, 6:7], small[0:P, 2:3], -1.0, small[0:P, 5:6],
                                                     ALU.mult, ALU.mult), reads=["sm2", "sm5"], writes=["sm6"])
        S.op("act", lambda e: e.activation(xn[0:P, :], r[0:P, :], AF.Identity, bias=small[0:P, 6:7], scale=small[0:P, 5:6]),
             reads=["r", "sm5", "sm6"], writes=["xn"])
        S.op("pool", lambda e: e.tensor_tensor(xn[0:P, :], xn[0:P, :], lng[0:P, :], ALU.mult), reads=["xn", "lng"], writes=["xn"])
        S.op("pool", lambda e: e.tensor_tensor(out_t[0:P, :], xn[0:P, :], lnb[0:P, :], ALU.add), reads=["xn", "lnb"], writes=[out_res])

    def do_rope(P, rt, rtr, src, H, c0, dst, dec, zres, dres, dq_t, dq_r):
        n = 8 * H
        s4 = src.rearrange("p (h t j) -> p h t j", h=4, t=2)
        ta4 = ta[0:P, 0:n].rearrange("p (h t j) -> p h t j", h=4, t=2)
        tb4 = tb[0:P, 0:n].rearrange("p (h t j) -> p h t j", h=4, t=2)
        cosb = bc(rt[0:P, c0:c0 + H].unsqueeze(1).unsqueeze(1), [P, 4, 2, H])
        snb = bc(rt[0:P, c0 + H:c0 + 2 * H].unsqueeze(1), [P, 4, H])
        spb = bc(rt[0:P, c0 + 2 * H:c0 + 3 * H].unsqueeze(1), [P, 4, H])
        S.op("dve", lambda e: e.tensor_tensor(ta4, s4, cosb, ALU.mult), reads=[zres, rtr], writes=["ta"])
        S.op("dve", lambda e: e.tensor_tensor(tb4[:, :, 0, :], s4[:, :, 1, :], snb, ALU.mult),
             reads=[zres, rtr], writes=["tb"])
        S.op("dve", lambda e: e.tensor_tensor(tb4[:, :, 1, :], s4[:, :, 0, :], spb, ALU.mult),
             reads=[zres, rtr], writes=["tb"])
        if dec is None:
            S.op("dve", lambda e: e.tensor_tensor(dst, ta[0:P, 0:n], tb[0:P, 0:n], ALU.add),
                 reads=["ta", "tb"], writes=[dres])
        else:
            S.op("dve", lambda e: e.tensor_tensor(ta[0:P, 0:n], ta[0:P, 0:n], tb[0:P, 0:n], ALU.add),
                 reads=["ta", "tb"], writes=["ta"])
            decb = bc(dq_t[0:P, dec:dec + 4].unsqueeze(2), [P, 4, 2 * H])
            S.op("dve", lambda e: e.tensor_tensor(dst.rearrange("p (h j) -> p h j", h=4),
                                                  ta[0:P, 0:n].rearrange("p (h j) -> p h j", h=4), decb, ALU.mult),
                 reads=["ta", dq_r], writes=[dres])

    def group_norm(P, pso, pso_r):
        pso4 = pso[0:P, :].rearrange("p (h n) -> p h n", h=4)
        S.op("dve", lambda e: e.reduce_sum(small[0:P, 8:12], pso4, AX.X), reads=[pso_r], writes=["g0"])
        S.op("act", lambda e: e.activation(sq[0:P, 0:512], pso[0:P, :], AF.Square), reads=[pso_r], writes=["sq"])
        S.op("dve", lambda e: e.reduce_sum(small[0:P, 12:16], sq[0:P, 0:512].rearrange("p (h n) -> p h n", h=4), AX.X),
             reads=["sq"], writes=["g1"])
        S.op("dve", lambda e: e.tensor_scalar_mul(small[0:P, 16:20], small[0:P, 8:12], 1.0 / 128), reads=["g0"], writes=["g2"])
        S.op("dve", lambda e: e.tensor_tensor(small[0:P, 20:24], small[0:P, 16:20], small[0:P, 16:20], ALU.mult),
             reads=["g2"], writes=["g3"])
        S.op("dve", lambda e: e.scalar_tensor_tensor(small[0:P, 24:28], small[0:P, 12:16], 1.0 / 128, small[0:P, 20:24],
                                                     ALU.mult, ALU.subtract), reads=["g1", "g3"], writes=["g4"])
        S.op("dve", lambda e: e.tensor_scalar_add(small[0:P, 24:28], small[0:P, 24:28], GN_EPS), reads=["g4"], writes=["g4"])
        S.op("act", lambda e: e.sqrt(small[0:P, 36:40], small[0:P, 24:28]), reads=["g4"], writes=["g7"])
        S.op("dve", lambda e: e.reciprocal(small[0:P, 28:32], small[0:P, 36:40]), reads=["g7"], writes=["g5"])
        S.op("dve", lambda e: e.scalar_tensor_tensor(small[0:P, 32:36], small[0:P, 16:20], -1.0, small[0:P, 28:32],
                                                     ALU.mult, ALU.mult), reads=["g2", "g5"], writes=["g6"])
        for h in range(4):
            S.op("act", lambda e, h=h: e.activation(yn[0:P, h * 128:(h + 1) * 128], pso[0:P, h * 128:(h + 1) * 128],
                                                    AF.Identity, bias=small[0:P, 32 + h:33 + h],
                                                    scale=small[0:P, 28 + h:29 + h]),
                 reads=[pso_r, "g5", "g6"], writes=["yn"])
        S.op("dve", lambda e: e.tensor_tensor(yret[0:P, :], yn[0:P, :], sg[0:P, :], ALU.mult),
             reads=["yn", "sg"], writes=["yret"])

    S.marks.append((S.seq, 'sample mixer'))
    P = 32
    IOA = bass.IndirectOffsetOnAxis
    rtS, rtSr = rtab[1], "rtab1"
    ld("sp", xt[0:P, :], xs_d, ["xt"])
    ld("sp", rtS[0:P, :], ropeS_d, [rtSr])
    ld("sp", sts32, st_in.rearrange("s (a q) e -> q s a e", q=128), ["sts32"])
    S.op("act", lambda e: e.copy(stsb, sts32), reads=["sts32"], writes=["stsb"])
    ld("sp", r[0:P, :], modsd[:, 1024:2048], ["r"], reads=["modsd"])
    ld("sp", xn[0:P, :], modsd[:, 0:1024], ["xn"], reads=["modsd"])
    S.op("dve", lambda e: e.scalar_tensor_tensor(r[0:P, :], r[0:P, :], 1.0, xt[0:P, :], ALU.add, ALU.mult),
         reads=["r", "xt"], writes=["r"])
    S.op("dve", lambda e: e.tensor_tensor(r[0:P, :], r[0:P, :], xn[0:P, :], ALU.add), reads=["r", "xn"], writes=["r"])

    def tr32(src_fn, n, reads, dst, dst_res, eng="act"):
        ps, pr = nps()

        def f(e, ps=ps):
            for j in range(n):
                ins = e.transpose(ps[:, j * 32:(j + 1) * 32], src_fn(j), ident[0:32, 0:32])
            return ins
        S.op("pe", f, reads=list(reads) + ["ident"], writes=[pr])
        src = ps[:, 0:n * 32].rearrange("p (a b) -> p a b", a=n)
        if eng == "act":
            S.op("act", lambda e: e.copy(dst, src), reads=[pr], writes=[dst_res])
        else:
            S.op("dve", lambda e: e.tensor_copy(dst, src), reads=[pr], writes=[dst_res])

    tr32(lambda k: r[0:P, k * 128:(k + 1) * 128], 8, ["r"], hT[:, :, 0:32], "hT")
    for g in range(6):
        ps, pr = nps()

        def f(e, ps=ps, g=g):
            for k in range(8):
                ins = e.matmul(ps[0:P, :], hT[:, k, 0:32], w_in_b[:, k, g * 512:(g + 1) * 512],
                               start=(k == 0), stop=(k == 7))
            return ins
        S.op("pe", f, reads=["hT"] + WIN, writes=[pr])
        S.op("act", lambda e, ps=ps, g=g: e.copy(z[0:P, g * 512:(g + 1) * 512], ps[0:P, :]),
             reads=[pr], writes=["z%d" % g])
    S.dma("sp", lambda e: e.dma_start(out=vs, in_=z[0:P, 2560:3072]), reads=["z5"])
    S.op("act", lambda e: e.activation(sg[0:P, :], z[0:P, 1024:1536], AF.Silu), reads=["z2"], writes=["sg"])
    S.op("act", lambda e: e.copy(rvb[0:P, :], z[0:P, 512:1024]), reads=["z1"], writes=["rvb"])
    S.op("act", lambda e: e.copy(vnb[:], z[0:P, 2560:3072]), reads=["z5"], writes=["vnb"])
    mks = mkr[0]
    do_rope(P, rtS, rtSr, z[0:P, 0:256], 32, 192, qr[0:P, :], 0, "z0", "qr", dqks, "dqks")
    do_rope(P, rtS, rtSr, z[0:P, 256:512], 32, 192, kr[0:P, :], 4, "z0", "kr", dqks, "dqks")
    do_rope(P, rtS, rtSr, z[0:P, 1536:2048], 64, 0, mqr[0:P, :], None, "z3", "mqr", dqks, "dqks")
    do_rope(P, rtS, rtSr, z[0:P, 2048:2560], 64, 0, mks[0:P, :], None, "z4", "mkr0", dqks, "dqks")
    S.dma("sp", lambda e: e.dma_start(out=ks, in_=mks[0:P, :]), reads=["mkr0"])
    S.op("dve", lambda e: e.tensor_tensor(krbm[0:P, :, :], bc(kr[0:P, :].unsqueeze(1), [P, 4, 256]),
                                          bc(rm[0:P, :].unsqueeze(2), [P, 4, 256]), ALU.mult),
         reads=["kr", "rm"], writes=["krbm"])
    tr32(lambda j: qr[0:P, j * 128:(j + 1) * 128], 2, ["qr"], qTs[:], "qTs")
    tr32(lambda j: kr[0:P, j * 128:(j + 1) * 128], 2, ["kr"], kTs[:], "kTs")
    tr32(lambda j: mqr[0:P, j * 128:(j + 1) * 128], 4, ["mqr"], mqTs[:], "mqTs")
    tr32(lambda j: mks[0:P, j * 128:(j + 1) * 128], 4, ["mkr0"], mkTs[:], "mkTs")
    S.op("dve", lambda e: e.tensor_tensor(qTsm[:], bc(qTs[:].unsqueeze(2), [128, 2, 4, 32]),
                                          bc(cm.unsqueeze(1), [128, 2, 4, 32]), ALU.mult),
         reads=["qTs", "cm"], writes=["qTsm"])
    pso, pso_r = psf[4], "psf4"
    for h in range(4):
        p_, hh = h // 2, h % 2
        lo, hi = hh * 64, hh * 64 + 64
        ps, pr = nps()
        S.op("pe", lambda e, ps=ps, p_=p_, lo=lo, hi=hi: e.matmul(
            ps[0:P, 0:32], kTs[lo:hi, p_, :], qTs[lo:hi, p_, :], start=True, stop=True),
            reads=["kTs", "qTs"], writes=[pr])
        sm = scTm[h % 2]
        smr = "scTm%d" % (h % 2)
        S.op("dve", lambda e, ps=ps, sm=sm: e.tensor_tensor(sm[0:P, 0:32], ps[0:P, 0:32], maskS[:], ALU.mult),
             reads=[pr, "maskS"], writes=[smr])

        def f(e, sm=sm, h=h, p_=p_, lo=lo, hi=hi):
            e.matmul(pso[0:P, h * 128:(h + 1) * 128], sm[0:P, 0:32], rvb[0:P, h * 128:(h + 1) * 128],
                     start=True, stop=False)
            for s_ in range(4):
                ins = e.matmul(pso[0:P, h * 128:(h + 1) * 128], qTsm[lo:hi, p_, s_, :], stsb[lo:hi, s_, p_, :],
                               start=False, stop=(s_ == 3))
            return ins
        S.op("pe", f, reads=[smr, "rvb", "qTsm", "stsb"], writes=[pso_r])
    for s_ in range(4):
        ps, pr = nps()

        def f(e, ps=ps, s_=s_):
            for p_ in range(2):
                ins = e.matmul(ps[:, p_ * 256:(p_ + 1) * 256], krbm[0:P, s_, p_ * 128:(p_ + 1) * 128],
                               rvb[0:P, p_ * 256:(p_ + 1) * 256], start=True, stop=True)
            return ins
        S.op("pe", f, reads=["krbm", "rvb"], writes=[pr])
        for hh in range(2):
            lo, hi = hh * 64, hh * 64 + 64
            S.op("dve", lambda e, ps=ps, s_=s_, hh=hh, lo=lo, hi=hi: e.tensor_tensor(
                sttmp[lo:hi, :, :], sts32[lo:hi, s_, :, :],
                ps[lo:hi, :].rearrange("q (a h e) -> q a h e", a=2, h=2)[:, :, hh, :], ALU.add),
                reads=["sts32", pr, pso_r], writes=["sttmp"])
            S.op("dve", lambda e, s_=s_, lo=lo, hi=hi: e.tensor_tensor(
                sts32[lo:hi, s_, :, :], sttmp[lo:hi, :, :], bc(gtab[lo:hi, :].unsqueeze(2), [64, 2, 128]), ALU.mult),
                reads=["sttmp", "gtab"], writes=["sts32"])
    S.dma("sp", lambda e: e.dma_start(out=ss.rearrange("s (a q) e -> q s a e", q=128), in_=sts32), reads=["sts32"])
    group_norm(P, pso, pso_r)
    tr32(lambda j: yret[0:P, j * 128:(j + 1) * 128], 4, ["yret"], mixT[:, 0:4, 0:32], "mixT")

    psO, psO_r = psf[5], "psf5"
    gcnt = [0, 0]
    for s_ in range(4):
        S.marks.append((S.seq, 'smoba %d' % s_))
        qsl = slice(s_ * 8, (s_ + 1) * 8)
        for gi in range(16):
            sl = gcnt[0] % 2
            gcnt[0] += 1
            for j in range(2):
                col = s_ * 32 + gi * 2 + j
                S.dma("pool", lambda e, sl=sl, j=j, col=col: e.indirect_dma_start(
                    out=W[:, 32768 + sl * 2048 + j * 1024:32768 + sl * 2048 + (j + 1) * 1024], out_offset=None, in_=ck,
                    in_offset=IOA(ap=idx[:, col:col + 1], axis=0)),
                    reads=["idx"], writes=["Kg%d_%d" % (sl, j)])
            for half in range(2):
                pb, pbr = npsb()

                def f(e, pb=pb, sl=sl, half=half):
                    for i in range(8):
                        jj, hh_ = (half * 8 + i) // 4, (half * 8 + i) % 4
                        ins = e.transpose(pb[:, i * 128:(i + 1) * 128], Kg[sl][:, jj, hh_ * 128:(hh_ + 1) * 128], identb[:])
                    return ins
                S.op("pe", f, reads=["Kg%d_%d" % (sl, jx) for jx in range(4)] + ["identb"], writes=[pbr])
                srcv = pb[:, :].rearrange("p (a b) -> p a b", a=8)
                if half == 0:
                    S.op("act", lambda e, sl=sl, srcv=srcv: e.copy(KTg[sl][:, 0:8, :], srcv),
                         reads=[pbr], writes=["KTg%d" % sl])
                else:
                    S.op("dve", lambda e, sl=sl, srcv=srcv: e.tensor_copy(KTg[sl][:, 8:16, :], srcv),
                         reads=[pbr], writes=["KTg%d" % sl])
            ps, pr = nps()

            def f(e, ps=ps, sl=sl, qsl=qsl):
                for i in range(16):
                    ins = e.matmul(ps[:, i * 8:(i + 1) * 8], KTg[sl][:, i, :], mqTs[:, i % 4, qsl], start=True, stop=True)
                return ins
            S.op("pe", f, reads=["KTg%d" % sl, "mqTs"], writes=[pr])
            hfS, pg0 = gi // 8, (gi % 8) * 4
            S.op("dve", lambda e, ps=ps, hfS=hfS, pg0=pg0: e.tensor_copy(
                Sall[hfS][:, pg0:pg0 + 4, :], ps[:, 0:128].rearrange("p (a b) -> p a b", a=4)),
                reads=[pr], writes=["Sall%d" % hfS])
        ps, pr = nps()

        def f(e, ps=ps):
            for pg in range(64):
                ins = e.matmul(ps[0:32, pg:pg + 1], Sall[pg // 32][:, pg % 32, :], ones_f[:, 0:1], start=True, stop=True)
            return ins
        S.op("pe", f, reads=["Sall0", "Sall1", "ones_f"], writes=[pr])
        S.op("act", lambda e, ps=ps: e.copy(gsb[:], ps[0:32, 0:64]), reads=[pr], writes=["gsb"])
        gs3 = gsb[:].rearrange("p (n t) -> p n t", t=2)
        S.op("dve", lambda e, gs3=gs3: e.tensor_tensor(g32[:], gs3[:, :, 0], gs3[:, :, 1], ALU.add),
             reads=["gsb"], writes=["g32"])
        S.op("dve", lambda e: e.max(mxs[:], g32[:]), reads=["g32"], writes=["mxs"])
        S.op("dve", lambda e: e.tensor_scalar(sel[:], g32[:], mxs[:, 2:3], None, ALU.is_ge),
             reads=["g32", "mxs"], writes=["sel"])
        Dm3 = Dm[0:32, :].rearrange("p (n q) -> p n q", n=32)
        S.op("dve", lambda e, Dm3=Dm3: e.tensor_tensor(Dm3, bc(sel[:].unsqueeze(2), [32, 32, 32]),
                                                       bc(ident[0:32, 0:32].unsqueeze(1), [32, 32, 32]), ALU.mult),
             reads=["sel", "ident"], writes=["Dm"])
        for half in range(2):
            ps, pr = nps()
            S.op("pe", lambda e, ps=ps, half=half: e.matmul(ps[:, :], ones_b[0:32, :], Dm[0:32, half * 512:(half + 1) * 512],
                                                            start=True, stop=True),
                 reads=["ones_b", "Dm"], writes=[pr])
            S.op("act", lambda e, ps=ps, half=half: e.copy(selBs[:, half * 16:(half + 1) * 16, :],
                                                           ps[:, :].rearrange("p (a b) -> p a b", a=16)),
                 reads=[pr], writes=["selBs"])
        for hfS in range(2):
            S.op("act", lambda e, hfS=hfS: e.activation(Sall[hfS], Sall[hfS], AF.Exp, scale=SCALE),
                 reads=["Sall%d" % hfS], writes=["Sall%d" % hfS])
            S.op("dve", lambda e, hfS=hfS: e.tensor_tensor(
                PTs[:, hfS * 32:(hfS + 1) * 32, :].rearrange("p (n t) q -> p n t q", t=2),
                Sall[hfS].rearrange("p (n t) q -> p n t q", t=2),
                bc(selBs[:, hfS * 16:(hfS + 1) * 16, :].unsqueeze(2), [128, 16, 2, 32]), ALU.mult),
                reads=["Sall%d" % hfS, "selBs"], writes=["PTs"])
        ps, pr = nps()

        def f(e, ps=ps, qsl=qsl):
            for h in range(4):
                ins = e.matmul(ps[0:32, h * 8:(h + 1) * 8], mkTs[:, h, :], mqTs[:, h, qsl], start=True, stop=True)
            return ins
        S.op("pe", f, reads=["mkTs", "mqTs"], writes=[pr])
        S.op("act", lambda e, ps=ps: e.activation(En[:], ps[0:32, 0:32], AF.Exp, scale=SCALE), reads=[pr], writes=["En"])
        S.op("dve", lambda e, s_=s_: e.tensor_tensor(PN[:], En[:], cmaskN[:, s_, :], ALU.mult),
             reads=["En", "cmaskN"], writes=["PN"])
        for gi in range(16):
            sl = gcnt[1] % 2
            gcnt[1] += 1
            for j in range(2):
                col = s_ * 32 + gi * 2 + j
                S.dma("pool", lambda e, sl=sl, j=j, col=col: e.indirect_dma_start(
                    out=W[:, 40960 + sl * 2048 + j * 1024:40960 + sl * 2048 + (j + 1) * 1024], out_offset=None, in_=cv,
                    in_offset=IOA(ap=idx[:, col:col + 1], axis=0)),
                    reads=["idx"], writes=["Vg%d_%d" % (sl, j)])

            def f(e, sl=sl, gi=gi):
                for j in range(4):
                    pg = gi * 4 + j
                    for h in range(4):
                        e.matmul(psO[:, h * 8:(h + 1) * 8], Vg[sl][:, j, h * 128:(h + 1) * 128], PTs[:, pg, h * 8:(h + 1) * 8],
                                 start=(pg == 0), stop=False)
                    ins = e.matmul(psO[:, 32:64], ones_b[:, :], PTs[:, pg, :], start=(pg == 0), stop=False)
                return ins
            S.op("pe", f, reads=["Vg%d_%d" % (sl, jx) for jx in range(4)] + ["PTs", "ones_b"], writes=[psO_r])

        def f(e):
            for h in range(4):
                e.matmul(psO[:, h * 8:(h + 1) * 8], vnb[0:32, h * 128:(h + 1) * 128], PN[0:32, h * 8:(h + 1) * 8],
                         start=False, stop=True)
            return e.matmul(psO[:, 32:64], ones_b[0:32, :], PN[0:32, :], start=False, stop=True)
        S.op("pe", f, reads=["vnb", "PN", "ones_b"], writes=[psO_r])
        S.op("dve", lambda e: e.reciprocal(rinv[:], psO[:, 32:64]), reads=[psO_r], writes=["rinv"])
        S.op("dve", lambda e, qsl=qsl: e.tensor_tensor(mixT[:, 4:8, qsl], psO[:, 0:32].rearrange("p (h q) -> p h q", h=4),
                                                       rinv[:].rearrange("p (h q) -> p h q", h=4), ALU.mult),
             reads=[psO_r, "rinv"], writes=["mixT"])
    S.marks.append((S.seq, 'sample oproj'))
    ld("sp", z[0:P, 0:1024], modsd[:, 2048:3072], ["z0", "z1"], reads=["modsd"])
    pss = [nps(), nps()]
    for hf in range(2):
        def f(e, hf=hf, ps=pss[hf][0]):
            for c in range(8):
                ins = e.matmul(ps[0:P, :], mixT[:, c, 0:32], w_o_b[:, c, hf * 512:(hf + 1) * 512],
                               start=(c == 0), stop=(c == 7))
            return ins
        S.op("pe", f, reads=["mixT", "w_o"], writes=[pss[hf][1]])
    layer_norm([pss[0][0], pss[1][0]], [pss[0][1], pss[1][1]], xt, "xt", z[:, 0:1024], ["z0", "z1"], x1, "x1", P=P)
    S.dma("sp", lambda e: e.dma_start(out=x1d[2048:2080, :], in_=x1[0:P, :]), reads=["x1"], writes=["x1ds"])

    def load_tile(t):
        rt_ = rtab[t % 2]
        rtr_ = "rtab%d" % (t % 2)
        ld("sp", xts[t % 2][:], x[t * 128:(t + 1) * 128, :], ["xt%d" % (t % 2)])
        ld("sp", rt_[:, 0:192], ropeM[t * 128:(t + 1) * 128, :], [rtr_])
        ld("sp", rt_[:, 192:288], ropeR[t * 128:(t + 1) * 128, :], [rtr_])

    def p1(t, stg):
        S.marks.append((S.seq, 'p1 tile %d' % t))
        rt = rtab[t % 2]
        rtr = "rtab%d" % (t % 2)
        mk = mkr[t % 2]
        mkres = "mkr%d" % (t % 2)
        xt_t, hT_t = xts[t % 2], hTs[t % 2]
        xtr, hTr = "xt%d" % (t % 2), "hT%d" % (t % 2)
        if stg == 'A':
            transposes_to(lambda b, n, hT_t=hT_t: hT_t[:, b, :], lambda k, xt_t=xt_t: xt_t[:, k * 128:(k + 1) * 128],
                          8, [xtr], [hTr], scale_fn=lambda k: (modT[:, 8 + k:9 + k], modT[:, k:k + 1]))
            for g in range(6):
                ps, pr = nps()

                def f(e, ps=ps, g=g, hT_t=hT_t):
                    for k in range(8):
                        ins = e.matmul(ps[:, :], hT_t[:, k, :], w_in_b[:, k, g * 512:(g + 1) * 512],
                                       start=(k == 0), stop=(k == 7))
                    return ins
                S.op("pe", f, reads=[hTr] + WIN, writes=[pr])
                if g % 2 == 0:
                    S.op("act", lambda e, ps=ps, g=g: e.copy(z[:, g * 512:(g + 1) * 512], ps[:, :]),
                         reads=[pr], writes=["z%d" % g])
                else:
                    S.op("dve", lambda e, ps=ps, g=g: e.tensor_copy(z[:, g * 512:(g + 1) * 512], ps[:, :]),
                         reads=[pr], writes=["z%d" % g])
        if stg == 'B':
            if t + 1 < NT:
                load_tile(t + 1)
            S.dma("sp", lambda e, t=t: e.dma_start(out=vout[t * 128:(t + 1) * 128, :], in_=z[:, 2560:3072]),
                  reads=["z5"])
            S.op("act", lambda e: e.activation(sg[:], z[:, 1024:1536], AF.Silu), reads=["z2"], writes=["sg"])
            S.op("act", lambda e: e.copy(rvb[:], z[:, 512:1024]), reads=["z1"], writes=["rvb"])
            S.op("act", lambda e, t=t: e.copy(Vb[:, t, :], z[:, 2560:3072]), reads=["z5"], writes=["Vb"])

            do_rope(128, rt, rtr, z[:, 0:256], 32, 192, qr[:], 0, "z0", "qr", dqk, "dqk")
            do_rope(128, rt, rtr, z[:, 256:512], 32, 192, kr[:], 4, "z0", "kr", dqk, "dqk")
            do_rope(128, rt, rtr, z[:, 1536:2048], 64, 0, mqr[:], None, "z3", "mqr", dqk, "dqk")
            do_rope(128, rt, rtr, z[:, 2048:2560], 64, 0, mk[:], None, "z4", mkres, dqk, "dqk")
            S.dma("sp", lambda e, t=t, mk=mk: e.dma_start(out=kout[t * 128:(t + 1) * 128, :], in_=mk[:]), reads=[mkres])
            S.op("act", lambda e: e.copy(krb[:], kr[:]), reads=["kr"], writes=["krb"])
            transposes_to(lambda b, n: qT[:, b:b + n, :], lambda j: qr[:, j * 128:(j + 1) * 128], 2, ["qr"], ["qT"])
            transposes_to(lambda b, n: kT[:, b:b + n, :], lambda j: kr[:, j * 128:(j + 1) * 128], 2, ["kr"], ["kT"])
            transposes_to(lambda b, n: mqT[:, b:b + n, :], lambda j: mqr[:, j * 128:(j + 1) * 128], 4, ["mqr"], ["mqT"])
            ps, pr = nps()

            def f(e, ps=ps, mk=mk):
                for j in range(4):
                    ins = e.transpose(ps[:, j * 128:(j + 1) * 128], mk[:, j * 128:(j + 1) * 128], ident[:])
                return ins
            S.op("pe", f, reads=[mkres, "ident"], writes=[pr])
            S.op("act", lambda e, ps=ps, t=t: e.copy(KT[:, :, t * 128:(t + 1) * 128],
                                                      ps[:, :].rearrange("p (h n) -> p h n", h=4)),
                 reads=[pr], writes=["KT"])
            S.op("dve", lambda e, ps=ps, t=t: e.reduce_sum(kmT[:, :, t], ps[:, :].rearrange("p (h n) -> p h n", h=4), AX.X),
                 reads=[pr, "KT"], writes=["kmT"])
            if t % 2 == 1:
                n = t // 2
                S.op("dve", lambda e, n=n: e.tensor_tensor(kmsum[:, :, n], kmT[:, :, 2 * n], kmT[:, :, 2 * n + 1], ALU.add),
                     reads=["kmT"], writes=["kmsum"])
                S.op("dve", lambda e, n=n: e.tensor_scalar_mul(kmb[:, :, n], kmsum[:, :, n], 1.0 / 256),
                     reads=["kmsum"], writes=["kmb"])

            S.marks.append((S.seq, 'ret %d' % t))
            pso, pso_r = psf[4], "psf4"
            for h in range(4):
                p_, hh = h // 2, h % 2
                lo, hi = hh * 64, hh * 64 + 64
                ps, pr = nps()
                S.op("pe", lambda e, ps=ps, p_=p_, lo=lo, hi=hi: e.matmul(
                    ps[:, 0:128], kT[lo:hi, p_, :], qT[lo:hi, p_, :], start=True, stop=True),
                    reads=["kT", "qT"], writes=[pr])
                sm = scTm[h % 2]
                smr = "scTm%d" % (h % 2)
                S.op("dve", lambda e, ps=ps, sm=sm: e.tensor_tensor(sm[:], ps[:, 0:128], triT[:], ALU.mult),
                     reads=[pr, "triT"], writes=[smr])

                def f(e, sm=sm, h=h, p_=p_, lo=lo, hi=hi):
                    e.matmul(pso[:, h * 128:(h + 1) * 128], sm[:], rvb[:, h * 128:(h + 1) * 128], start=True, stop=False)
                    return e.matmul(pso[:, h * 128:(h + 1) * 128], qT[lo:hi, p_, :], stb[lo:hi, p_, :],
                                    start=False, stop=True)
                S.op("pe", f, reads=[smr, "rvb", "qT", "stb"], writes=[pso_r])
            for p_ in range(2):
                ps, pr = nps()
                S.op("pe", lambda e, ps=ps, p_=p_: e.matmul(
                    ps[:, 0:256], krb[:, p_ * 128:(p_ + 1) * 128], rvb[:, p_ * 256:(p_ + 1) * 256], start=True, stop=True),
                    reads=["krb", "rvb"], writes=[pr])
                for hh in range(2):
                    h = 2 * p_ + hh
                    lo, hi = hh * 64, hh * 64 + 64
                    gC = GAM[h] ** 128
                    S.op("dve", lambda e, ps=ps, p_=p_, hh=hh, lo=lo, hi=hi: e.tensor_tensor(
                        sttmp[lo:hi, p_, :], st32[lo:hi, p_, :], ps[lo:hi, hh * 128:(hh + 1) * 128], ALU.add),
                        reads=["st32", pr, pso_r], writes=["sttmp"])
                    S.op("dve", lambda e, p_=p_, lo=lo, hi=hi, gC=gC: e.tensor_scalar_mul(
                        st32[lo:hi, p_, :], sttmp[lo:hi, p_, :], gC), reads=["sttmp"], writes=["st32"])
                    S.op("act", lambda e, p_=p_, lo=lo, hi=hi, gC=gC: e.mul(
                        stb[lo:hi, p_, :], sttmp[lo:hi, p_, :], gC), reads=["sttmp"], writes=["stb"])
            S.marks.append((S.seq, 'gn %d' % t))
            group_norm(128, pso, pso_r)
            transposes_to(lambda b, n: mixT[:, b:b + n, :], lambda j: yret[:, j * 128:(j + 1) * 128], 4,
                          ["yret"], ["mixT"])

        if stg == 'C':
            S.marks.append((S.seq, 'moba %d' % t))
            own = t // 2
            nkt = t + 1
            psO, psO_r = psf[5], "psf5"
            for h in range(4):
                if own >= 4:
                    ps, pr = nps()
                    S.op("pe", lambda e, ps=ps, h=h: e.matmul(ps[:, 0:8], mqT[:, h, :], kmb[:, h, :], start=True, stop=True),
                         reads=["mqT", "kmb"], writes=[pr])
                    S.op("dve", lambda e: e.memset(g8[:], -1e30), writes=["g8"])
                    S.op("dve", lambda e, ps=ps, own=own: e.tensor_copy(g8[:, 0:own], ps[:, 0:own]),
                         reads=[pr], writes=["g8"])
                    S.op("dve", lambda e: e.max(mx8[:], g8[:]), reads=["g8"], writes=["mx8"])
                    S.op("dve", lambda e: e.tensor_scalar(biasn[:], g8[:], mx8[:, 2:3], 1.0, ALU.is_ge, ALU.subtract),
                         reads=["g8", "mx8"], writes=["biasn"])
                    S.op("dve", lambda e: e.tensor_scalar_mul(biasn[:], biasn[:], -NEG), reads=["biasn"], writes=["biasn"])
                else:
                    S.op("dve", lambda e: e.memset(biasn[:], 0.0), writes=["biasn"])
                S.op("dve", lambda e: e.memset(rs[:], 0.0), writes=["rs"])
                for n0 in range(0, own, 2):
                    nb = min(2, own - n0)
                    ps, pr = nps()
                    S.op("pe", lambda e, ps=ps, h=h, n0=n0, nb=nb: e.matmul(
                        ps[:, 0:nb * 256], mqT[:, h, :], KT[:, h, n0 * 256:(n0 + nb) * 256], start=True, stop=True),
                        reads=["mqT", "KT"], writes=[pr])
                    for j in range(nb):
                        n = n0 + j
                        S.op("act", lambda e, ps=ps, j=j, n=n: e.activation(
                            Pm[:, n * 256:(n + 1) * 256], ps[:, j * 256:(j + 1) * 256], AF.Exp,
                            bias=biasn[:, n:n + 1], scale=SCALE, accum_out=rs[:, n:n + 1]),
                            reads=[pr, "biasn", "rs"], writes=["Pm", "rs"])
                ps, pr = nps()
                k0 = own * 256
                nown = (t + 1) * 128 - k0
                S.op("pe", lambda e, ps=ps, h=h, k0=k0, nown=nown: e.matmul(
                    ps[:, 0:nown], mqT[:, h, :], KT[:, h, k0:k0 + nown], start=True, stop=True),
                    reads=["mqT", "KT"], writes=[pr])
                if nown == 256:
                    S.op("act", lambda e, ps=ps, k0=k0: e.activation(
                        Pm[:, k0:k0 + 128], ps[:, 0:128], AF.Exp, scale=SCALE, accum_out=rs[:, 8:9]),
                        reads=[pr, "rs"], writes=["Pm", "rs"])
                d0 = nown - 128
                S.op("dve", lambda e, ps=ps, d0=d0: e.tensor_tensor(sd[:], ps[:, d0:d0 + 128], tribias[:], ALU.add),
                     reads=[pr, "tribias"], writes=["sd"])
                S.op("act", lambda e, t=t: e.activation(Pm[:, t * 128:(t + 1) * 128], sd[:], AF.Exp, scale=SCALE,
                                                        accum_out=rs[:, 9:10]),
                     reads=["sd", "rs"], writes=["Pm", "rs"])
                S.op("dve", lambda e: e.reduce_sum(small[:, 40:41], rs[:, 0:10], AX.X), reads=["rs"], writes=["m0"])
                S.op("dve", lambda e: e.reciprocal(small[:, 41:42], small[:, 40:41]), reads=["m0"], writes=["m1"])
                S.op("dve", lambda e, nkt=nkt: e.tensor_scalar_mul(Pm[:, 0:nkt * 128], Pm[:, 0:nkt * 128], small[:, 41:42]),
                     reads=["Pm", "m1"], writes=["Pm"])
                for k8 in range(0, nkt, 8):
                    nn = min(8, nkt - k8)
                    pb, pbr = npsb()
                    ptt = PT[(k8 // 8) % 2]
                    ptr = "PT%d" % ((k8 // 8) % 2)

                    def f(e, pb=pb, k8=k8, nn=nn):
                        for j in range(nn):
                            ins = e.transpose(pb[:, j * 128:(j + 1) * 128], Pm[:, (k8 + j) * 128:(k8 + j + 1) * 128],
                                              identb[:])
                        return ins
                    S.op("pe", f, reads=["Pm", "identb"], writes=[pbr])
                    S.op("dve", lambda e, pb=pb, nn=nn, ptt=ptt: e.tensor_copy(
                        ptt[:, 0:nn, :], pb[:, 0:nn * 128].rearrange("p (a b) -> p a b", a=nn)),
                        reads=[pbr], writes=[ptr])

                    def f2(e, ptt=ptt, k8=k8, nn=nn, h=h, nkt=nkt):
                        for j in range(nn):
                            kt = k8 + j
                            ins = e.matmul(psO[:, h * 128:(h + 1) * 128], Vb[:, kt, h * 128:(h + 1) * 128], ptt[:, j, :],
                                           start=(kt == 0), stop=(kt == nkt - 1))
                        return ins
                    S.op("pe", f2, reads=[ptr, "Vb"], writes=[psO_r])
            S.op("act", lambda e: e.copy(mixT[:, 4:8, :], psO[:, :].rearrange("p (h n) -> p h n", h=4)),
                 reads=[psO_r], writes=["mixT"])

            S.marks.append((S.seq, 'oproj %d' % t))
            pss = [nps(), nps()]
            for hf in range(2):
                def f(e, hf=hf, ps=pss[hf][0]):
                    for c in range(8):
                        ins = e.matmul(ps[:, :], mixT[:, c, :], w_o_b[:, c, hf * 512:(hf + 1) * 512],
                                       start=(c == 0), stop=(c == 7))
                    return ins
                S.op("pe", f, reads=["mixT", "w_o"], writes=[pss[hf][1]])
            layer_norm([pss[0][0], pss[1][0]], [pss[0][1], pss[1][1]], xt_t, xtr, gate_a, ["gate_a"], x1, "x1")
            S.dma("sp", lambda e, t=t: e.dma_start(out=x1d[t * 128:(t + 1) * 128, :], in_=x1[:]), reads=["x1"],
                  writes=["x1d%d" % t])


    load_tile(0)
    p1(0, 'A')
    p1(0, 'B')
    for t in range(NT):
        if t + 1 < NT:
            p1(t + 1, 'A')
        p1(t, 'C')
        if t + 1 < NT:
            p1(t + 1, 'B')

    for p_ in range(2):
        S.dma("sp", lambda e, p_=p_: e.dma_start(out=sout[p_ * 128:(p_ + 1) * 128, :], in_=st32[:, p_, :]),
              reads=["st32"])

    S.marks.append((S.seq, 'phase2'))
    P1 = WIN + ["w_o"]
    w_up_v = w_up.rearrange("(k p) n -> p k n", p=128)
    for k2 in range(4):
        ld("pool", w_up_b[:, 2 * k2:2 * k2 + 2, :], w_up_v[:, 2 * k2:2 * k2 + 2, :], ["w_up%d" % k2] + P1)
    WUP = ["w_up%d" % i for i in range(4)]
    w_down_v = w_down.rearrange("(k p) n -> p k n", p=128)
    for k4 in range(4):
        ld("pool", w_down_b[:, 8 * k4:8 * k4 + 8, :], w_down_v[:, 8 * k4:8 * k4 + 8, :],
           ["w_dn%d" % k4, "KT", "Vb", "wada0", "wada1"] + TAIL)
    WDN = ["w_dn%d" % i for i in range(4)]
    ld("sp", lng[:], l2g, ["lng"])
    ld("sp", lnb[:], l2b, ["lnb"])
    ld("sp", gate_a[:], gfd, ["gate_a"], reads=["gfd"])
    S.marks.append((S.seq, 'sample ffn'))
    ld("sp", xt[0:P, :], x1d[2048:2080, :], ["xt"], reads=["x1ds"])
    ld("sp", r[0:P, :], modsd[:, 4096:5120], ["r"], reads=["modsd"])
    ld("sp", xn[0:P, :], modsd[:, 3072:4096], ["xn"], reads=["modsd"])
    ld("sp", z[0:P, 2048:3072], modsd[:, 5120:6144], ["z4", "z5"], reads=["modsd"])
    S.op("dve", lambda e: e.scalar_tensor_tensor(r[0:P, :], r[0:P, :], 1.0, xt[0:P, :], ALU.add, ALU.mult),
         reads=["r", "xt"], writes=["r"])
    S.op("dve", lambda e: e.tensor_tensor(r[0:P, :], r[0:P, :], xn[0:P, :], ALU.add), reads=["r", "xn"], writes=["r"])
    tr32(lambda k: r[0:P, k * 128:(k + 1) * 128], 8, ["r"], hT[:, :, 0:32], "hT")
    for g in range(8):
        ps, pr = nps()

        def f(e, ps=ps, g=g):
            for j in range(4):
                fc = g * 4 + j
                for k in range(8):
                    ins = e.matmul(ps[:, j * 32:(j + 1) * 32], w_up_b[:, k, fc * 128:(fc + 1) * 128], hT[:, k, 0:32],
                                   start=(k == 0), stop=(k == 7))
            return ins
        S.op("pe", f, reads=["hT"] + WUP, writes=[pr])
        rr = rl[g % 2]
        rrr = "rl%d" % (g % 2)
        S.op("dve", lambda e, ps=ps, rr=rr: e.tensor_scalar_max(rr[:, 0:128], ps[:, 0:128], 0.0), reads=[pr], writes=[rrr])
        S.op("act", lambda e, rr=rr, g=g: e.activation(
            uT[:, 4 * g:4 * g + 4, 0:32], rr[:, 0:128].rearrange("p (a b) -> p a b", a=4), AF.Square),
            reads=[rrr], writes=["uT"])
    pss = [nps(), nps()]
    for hf in range(2):
        def f(e, hf=hf, ps=pss[hf][0]):
            for c in range(32):
                ins = e.matmul(ps[0:P, :], uT[:, c, 0:32], w_down_b[:, c, hf * 512:(hf + 1) * 512],
                               start=(c == 0), stop=(c == 31))
            return ins
        S.op("pe", f, reads=["uT"] + WDN, writes=[pss[hf][1]])
    layer_norm([pss[0][0], pss[1][0]], [pss[0][1], pss[1][1]], xt, "xt", z[:, 2048:3072], ["z4", "z5"], x1, "x1", P=P)
    S.dma("sp", lambda e: e.dma_start(out=ys, in_=x1[0:P, :]), reads=["x1"])
    def load_tile2(t):
        ld("sp", xts[t % 2][:], x1d[t * 128:(t + 1) * 128, :], ["xt%d" % (t % 2)], reads=["x1d%d" % t])

    pss2 = {}

    def p2(t, stg):
        xt_t, hT_t = xts[t % 2], hTs[t % 2]
        xtr, hTr = "xt%d" % (t % 2), "hT%d" % (t % 2)
        if stg == 'A':
            transposes_to(lambda b, n, hT_t=hT_t: hT_t[:, b, :], lambda k, xt_t=xt_t: xt_t[:, k * 128:(k + 1) * 128],
                          8, [xtr], [hTr], scale_fn=lambda k: (modT[:, 32 + k:33 + k], modT[:, 24 + k:25 + k]))
        if stg == 'B':
            if t + 1 < NT:
                load_tile2(t + 1)
            for g in range(8):
                ps, pr = nps()

                def f(e, ps=ps, g=g, hT_t=hT_t):
                    for j in range(4):
                        fc = g * 4 + j
                        for k in range(8):
                            ins = e.matmul(ps[:, j * 128:(j + 1) * 128], w_up_b[:, k, fc * 128:(fc + 1) * 128], hT_t[:, k, :],
                                           start=(k == 0), stop=(k == 7))
                    return ins
                S.op("pe", f, reads=[hTr] + WUP, writes=[pr])
                rr = rl[g % 2]
                rrr = "rl%d" % (g % 2)
                S.op("dve", lambda e, ps=ps, rr=rr: e.tensor_scalar_max(rr[:], ps[:, :], 0.0), reads=[pr], writes=[rrr])
                S.op("act", lambda e, rr=rr, g=g: e.activation(
                    uT[:, 4 * g:4 * g + 4, :], rr[:].rearrange("p (a b) -> p a b", a=4), AF.Square),
                    reads=[rrr], writes=["uT"])
            pss = [nps(), nps()]
            pss2[t] = pss
            for hf in range(2):
                def f(e, hf=hf, ps=pss[hf][0]):
                    for c in range(32):
                        ins = e.matmul(ps[:, :], uT[:, c, :], w_down_b[:, c, hf * 512:(hf + 1) * 512],
                                       start=(c == 0), stop=(c == 31))
                    return ins
                S.op("pe", f, reads=["uT"] + WDN, writes=[pss[hf][1]])
        if stg == 'C':
            pss = pss2[t]
            layer_norm([pss[0][0], pss[1][0]], [pss[0][1], pss[1][1]], xt_t, xtr, gate_a, ["gate_a"], x1, "x1")
            S.dma("sp", lambda e, t=t: e.dma_start(out=y[t * 128:(t + 1) * 128, :], in_=x1[:]), reads=["x1"])


    load_tile2(0)
    p2(0, 'A')
    p2(0, 'B')
    for t in range(NT):
        if t + 1 < NT:
            p2(t + 1, 'A')
        p2(t, 'C')
        if t + 1 < NT:
            p2(t + 1, 'B')

    S.marks.append((S.seq, 'end'))
    if marks is not None:
        marks.extend(S.marks)
    sems = {}
    for k in ("pe", "act", "dve", "pool"):
        sems[k] = nc.alloc_semaphore("s_" + k)
    for i in range(S.n_dma):
        sems["d%d" % i] = nc.alloc_semaphore("s_d%d" % i)
    with nc.Block() as block:
        @block.sync
        def _(e):
            S.emit("sp", e, sems, final_wait=True)

        @block.tensor
        def _(e):
            S.emit("pe", e, sems)

        @block.scalar
        def _(e):
            S.emit("act", e, sems)

        @block.vector
        def _(e):
            S.emit("dve", e, sems)

        @block.gpsimd
        def _(e):
            S.emit("pool", e, sems)
    return nc


_CONST = {}


def _consts():
    if _CONST:
        return _CONST
    pos = np.arange(2048, dtype=np.float32)

    def tab(half):
        inv = np.power(np.float32(10000.0), -np.arange(half, dtype=np.float32) / np.float32(half)).astype(np.float32)
        ang = (pos[:, None] * inv[None, :]).astype(np.float32)
        c, s = np.cos(ang).astype(np.float32), np.sin(ang).astype(np.float32)
        return np.ascontiguousarray(np.concatenate([c, -s, s], axis=1))
    _CONST["ropeM"] = tab(64)
    _CONST["ropeR"] = tab(32)
    i = np.arange(128, dtype=np.float64)
    dq = np.stack([np.power(GAM[h], i + 1.0) for h in range(4)], axis=1)
    dk = np.stack([np.power(GAM[h], -(i + 1.0)) / 8.0 for h in range(4)], axis=1)
    _CONST["dqk"] = np.ascontiguousarray(np.concatenate([dq, dk], axis=1).astype(np.float32))
    jj, ii = np.meshgrid(np.arange(128), np.arange(128), indexing="ij")
    _CONST["triT"] = (ii >= jj).astype(np.float32)
    _CONST["tribias"] = np.where(ii >= jj, 0.0, NEG).astype(np.float32).T.copy()
    _CONST["ident"] = np.eye(128, dtype=np.float32)
    return _CONST


def _sample_consts():
    if "ropeS" in _CONST:
        return _CONST
    pos = (8192 + (np.arange(32) % 8)).astype(np.float32)

    def tab(half):
        inv = np.power(np.float32(10000.0), -np.arange(half, dtype=np.float32) / np.float32(half)).astype(np.float32)
        ang = (pos[:, None] * inv[None, :]).astype(np.float32)
        c, s = np.cos(ang).astype(np.float32), np.sin(ang).astype(np.float32)
        return np.concatenate([c, -s, s], axis=1)
    _CONST["ropeS"] = np.ascontiguousarray(np.concatenate([tab(64), tab(32)], axis=1))
    i = (np.arange(32) % 8).astype(np.float64)
    dq = np.stack([np.power(GAM[h], i + 1.0) for h in range(4)], axis=1)
    dk = np.stack([np.power(GAM[h], -(i + 1.0)) / 8.0 for h in range(4)], axis=1)
    _CONST["dqks"] = np.ascontiguousarray(np.concatenate([dq, dk], axis=1).astype(np.float32))
    rr = np.arange(32)
    _CONST["maskS"] = ((rr[:, None] // 8 == rr[None, :] // 8) & (rr[None, :] >= rr[:, None])).astype(np.float32)
    cm = (rr[None, :] // 8 == np.arange(4)[:, None]).astype(np.float32).reshape(1, 128)
    _CONST["cm"] = np.ascontiguousarray(np.broadcast_to(cm, (128, 128)))
    _CONST["rm"] = (rr[:, None] // 8 == np.arange(4)[None, :]).astype(np.float32)
    q = np.arange(128)
    _CONST["gtab"] = np.array([[GAM[2 * a + (qq // 64)] ** 8 for a in range(2)] for qq in q], dtype=np.float32)
    cmn = np.zeros((32, 4, 4, 8), np.float32)
    for sp in range(4):
        for j in range(8):
            for qq in range(8):
                if j <= qq:
                    cmn[sp * 8 + j, sp, :, qq] = 1.0
    _CONST["cmaskN"] = cmn.reshape(32, 128)
    _CONST["pcol"] = (np.arange(128) % 64).astype(np.float32)[:, None].copy()
    return _CONST


def make_in_maps(x_prompt, x_sample, cache_k, cache_v, state_ret, page_table, c_prompt, c_sample,
                 w_ada, b_ada, w_in, w_o, ln1_g, ln1_b, w_up, w_down, ln2_g, ln2_b, cores=range(8)):
    f = lambda a: np.ascontiguousarray(np.asarray(a, dtype=np.float32))
    C = _consts()
    _sample_consts()
    b_ada0 = f(b_ada)[0]
    ckf = f(cache_k).reshape(-1, 1024)
    cvf = f(cache_v).reshape(-1, 1024)
    shared = {
        "w_ada": f(w_ada)[0], "badaT": np.ascontiguousarray(b_ada0.reshape(48, 128).T),
        "bga": np.ascontiguousarray(np.broadcast_to(b_ada0[2048:3072], (128, 1024))),
        "bgf": np.ascontiguousarray(np.broadcast_to(b_ada0[5120:6144], (128, 1024))),
        "bada32": np.ascontiguousarray(np.broadcast_to(b_ada0, (32, 6144))),
        "w_in": f(w_in)[0], "w_o": f(w_o)[0], "w_up": f(w_up)[0], "w_down": f(w_down)[0],
        "l1g": np.ascontiguousarray(np.broadcast_to(f(ln1_g)[0], (128, 1024))),
        "l1b": np.ascontiguousarray(np.broadcast_to(f(ln1_b)[0], (128, 1024))),
        "l2g": np.ascontiguousarray(np.broadcast_to(f(ln2_g)[0], (128, 1024))),
        "l2b": np.ascontiguousarray(np.broadcast_to(f(ln2_b)[0], (128, 1024))),
        "ck": ckf, "cv": cvf,
    }
    for k in ("ident", "ropeM", "ropeR", "dqk", "triT", "tribias", "ropeS", "dqks", "maskS", "cm", "rm", "gtab",
              "cmaskN", "pcol"):
        shared[k] = C[k]
    xp, xsm, cp, csm = f(x_prompt), f(x_sample), f(c_prompt), f(c_sample)
    st = f(state_ret)[0]
    pt = np.asarray(page_table).astype(np.int32)
    in_maps = []
    for c in cores:
        m = dict(shared)
        m["x"] = xp[c]
        m["cT"] = np.ascontiguousarray(cp[c].reshape(8, 128).T)
        m["xs"] = np.ascontiguousarray(xsm[4 * c:4 * c + 4].reshape(32, 1024))
        crow = np.repeat(csm[4 * c:4 * c + 4], 8, axis=0)
        m["csT"] = np.ascontiguousarray(crow.reshape(32, 8, 128).transpose(2, 1, 0).reshape(128, 256))
        pt4 = pt[4 * c:4 * c + 4].reshape(4, 32, 2)
        m["ptb"] = np.ascontiguousarray(np.concatenate(
            [np.broadcast_to(pt4[:, :, hh].reshape(1, 128), (64, 128)) for hh in range(2)], axis=0))
        m["st_in"] = np.ascontiguousarray(st[4 * c:4 * c + 4].reshape(4, 256, 128))
        in_maps.append(m)
    return in_maps


def kernel(x_prompt, x_sample, cache_k, cache_v, state_ret, page_table, c_prompt, c_sample,
           w_ada, b_ada, w_in, w_o, ln1_g, ln1_b, w_up, w_down, ln2_g, ln2_b):
    nc = build_nc()
    in_maps = make_in_maps(x_prompt, x_sample, cache_k, cache_v, state_ret, page_table, c_prompt, c_sample,
                           w_ada, b_ada, w_in, w_o, ln1_g, ln1_b, w_up, w_down, ln2_g, ln2_b)
    res = run_bass_kernel_spmd(nc, in_maps, core_ids=list(range(8)))
    R = res.results
    g = lambda c, k: np.asarray(R[c][k]).astype(np.float32)
    y_p = np.stack([g(c, "y") for c in range(8)])
    k_p = np.stack([g(c, "kout").reshape(2048, 4, 128) for c in range(8)])[None]
    v_p = np.stack([g(c, "vout").reshape(2048, 4, 128) for c in range(8)])[None]
    s_p = np.stack([g(c, "sout").reshape(4, 64, 128) for c in range(8)])[None]
    y_s = np.concatenate([g(c, "ys").reshape(4, 8, 1024) for c in range(8)])
    k_s = np.concatenate([g(c, "ks").reshape(4, 8, 4, 128) for c in range(8)])[None]
    v_s = np.concatenate([g(c, "vs").reshape(4, 8, 4, 128) for c in range(8)])[None]
    s_s = np.concatenate([g(c, "ss").reshape(4, 4, 64, 128) for c in range(8)])[None]
    return (y_p, y_s, k_p, v_p, s_p, k_s, v_s, s_s)
```

```python
import math
import numpy as np
import concourse.bass as bass
import concourse.mybir as mybir
from concourse.bass_utils import run_bass_kernel_spmd

F32 = mybir.dt.float32
BF16 = mybir.dt.bfloat16
I32 = mybir.dt.int32
AF = mybir.ActivationFunctionType
ALU = mybir.AluOpType
AX = mybir.AxisListType

NT = 16
ALPHA = 2.0 ** 0.25
LN_EPS = 1e-5
GN_EPS = 1e-6
NEG = -30000.0
SCALE = 128.0 ** -0.5
GAM = [1.0 - 2.0 ** (-5.0 - h) for h in range(4)]


class Sched:
    ENG = ("pe", "act", "dve", "pool", "sp")

    def __init__(self, n_dma):
        self.ops = {e: [] for e in self.ENG}
        self.cnt = {e: 0 for e in self.ENG}
        self.lastw = {}
        self.readers = {}
        self.n_dma = n_dma
        self.dma_val = [0] * n_dma
        self.dma_next = 0
        self.seq = 0
        self.cut = None
        self.marks = []
        self.alias = {"ta": ["xnA"], "tb": ["xnB"], "sq": ["xnA", "xnB"], "xn": ["xnA", "xnB"],
                      "yn": ["rA"], "yret": ["rB"], "r": ["rA", "rB"], "sg": ["x1A"], "x1": ["x1A", "x1B"],
                      "rl0": ["z0"], "rl1": ["z1"], "uT": ["Pm", "PT0", "PT1"], "mkr1": ["rB"], "mkr0": ["rB"],
                      "mqr": ["rA"], "scTb": ["Pm"],
                      "KT": ["KT", "KTg0", "KTg1"] + ["Kg%d_%d" % (a, b) for a in range(2) for b in range(4)],
                      "Vb": ["Vb", "PTs", "selBs", "Dm"] + ["Vg%d_%d" % (a, b) for a in range(2) for b in range(4)],
                      "cm": ["rtab0"], "cmaskN": ["rtab0"], "xt": ["xt0"], "hT": ["hT0"]}

    def _exp(self, names):
        out = []
        for n in names:
            out += self.alias.get(n, [n])
        return out

    def _deps(self, reads, writes):
        reads, writes = self._exp(reads), self._exp(writes)
        toks = []
        for r in reads:
            if r in self.lastw:
                toks.append(self.lastw[r])
        for w in writes:
            if w in self.lastw:
                toks.append(self.lastw[w])
            toks += self.readers.get(w, [])
        return toks

    def _commit(self, tok, reads, writes):
        reads, writes = self._exp(reads), self._exp(writes)
        for r in reads:
            self.readers.setdefault(r, []).append(tok)
        for w in writes:
            self.lastw[w] = tok
            self.readers[w] = []

    def op(self, eng, fn, reads=(), writes=()):
        writes = list(writes) + [r for r in reads if r.startswith("ps")]
        toks = self._deps(reads, writes)
        self.cnt[eng] += 1
        tok = (eng, self.cnt[eng])
        self.seq += 1
        self.ops[eng].append((fn, toks, tok, self.seq))
        self._commit(tok, reads, writes)

    def dma(self, q, fn, reads=(), writes=()):
        toks = self._deps(reads, writes)
        i = self.dma_next
        self.dma_next = (i + 1) % self.n_dma
        if self.dma_val[i] > 0:
            toks.append(("d%d" % i, self.dma_val[i]))
        self.dma_val[i] += 16
        tok = ("d%d" % i, self.dma_val[i])
        self.seq += 1
        self.ops[q].append((fn, toks, tok, self.seq))
        self._commit(tok, reads, writes)

    def emit(self, eng_name, e, sems, final_wait=False):
        seen = {}
        for fn, toks, tok, seq in self.ops[eng_name]:
            if self.cut is not None and seq > self.cut:
                continue
            need = {}
            for k, v in toks:
                if k == eng_name and eng_name in ("pe", "sp"):
                    continue
                if v > need.get(k, 0):
                    need[k] = v
            for k, v in need.items():
                if seen.get(k, 0) >= v:
                    continue
                e.wait_ge(sems[k], v)
                seen[k] = v
            ins = fn(e)
            ins.then_inc(sems[tok[0]], 16 if tok[0][1:].isdigit() else 1)
        if final_wait:
            fin = {}
            for en in self.ENG:
                for fn, toks, tok, seq in self.ops[en]:
                    if self.cut is not None and seq > self.cut:
                        continue
                    if tok[0][1:].isdigit():
                        fin[tok[0]] = max(fin.get(tok[0], 0), tok[1])
            for k, v in fin.items():
                e.wait_ge(sems[k], v)


def build_nc(cut=None, marks=None):
    nc = bass.Bass("TRN2", target_bir_lowering=False)

    def din(name, shape, dt=F32):
        return nc.dram_tensor(name, shape, dt, kind="ExternalInput").ap()

    def dout(name, shape, dt=F32):
        return nc.dram_tensor(name, shape, dt, kind="ExternalOutput").ap()

    x = din("x", [2048, 1024])
    cT = din("cT", [128, 8])
    w_ada = din("w_ada", [1024, 6144])
    badaT = din("badaT", [128, 48])
    bga = din("bga", [128, 1024])
    bgf = din("bgf", [128, 1024])
    w_in = din("w_in", [1024, 3072])
    w_o = din("w_o", [1024, 1024])
    w_up = din("w_up", [1024, 4096])
    w_down = din("w_down", [4096, 1024])
    l1g = din("l1g", [128, 1024])
    l1b = din("l1b", [128, 1024])
    l2g = din("l2g", [128, 1024])
    l2b = din("l2b", [128, 1024])
    ident_d = din("ident", [128, 128])
    ropeM = din("ropeM", [2048, 192])
    ropeR = din("ropeR", [2048, 96])
    dqk_d = din("dqk", [128, 8])
    triT_d = din("triT", [128, 128])
    tribias_d = din("tribias", [128, 128])
    y = dout("y", [2048, 1024])
    kout = dout("kout", [2048, 512])
    vout = dout("vout", [2048, 512])
    sout = dout("sout", [256, 128])
    x1d = nc.dram_tensor("x1d", [2080, 1024], F32).ap()
    gfd = nc.dram_tensor("gfd", [128, 1024], F32).ap()
    modsd = nc.dram_tensor("modsd", [32, 6144], F32).ap()
    xs_d = din("xs", [32, 1024])
    csT_d = din("csT", [128, 256])
    bada32 = din("bada32", [32, 6144])
    ptb_d = din("ptb", [128, 128], I32)
    pcol_d = din("pcol", [128, 1])
    st_in = din("st_in", [4, 256, 128])
    ck = din("ck", [163840, 1024])
    cv = din("cv", [163840, 1024])
    ropeS_d = din("ropeS", [32, 288])
    dqks_d = din("dqks", [32, 8])
    maskS_d = din("maskS", [32, 32])
    cm_d = din("cm", [128, 128])
    rm_d = din("rm", [32, 4])
    gtab_d = din("gtab", [128, 2])
    cmaskN_d = din("cmaskN", [32, 128])
    ys = dout("ys", [32, 1024])
    ks = dout("ks", [32, 512])
    vs = dout("vs", [32, 512])
    ss = dout("ss", [4, 256, 128])

    def sb(name, shape, dt=F32):
        return nc.alloc_sbuf_tensor(name, shape, dt)

    W = sb("W", [128, 65536], BF16)
    w_in_b = W[:, 0:24576].rearrange("p (k n) -> p k n", k=8)
    w_o_b = W[:, 24576:32768].rearrange("p (k n) -> p k n", k=8)
    KT = W[:, 32768:40960].rearrange("p (h n) -> p h n", h=4)
    Vb = W[:, 40960:49152].rearrange("p (t n) -> p t n", t=16)
    wada = [W[:, 49152 + i * 4096:49152 + (i + 1) * 4096].rearrange("p (k n) -> p k n", k=8) for i in range(2)]
    w_up_b = W[:, 0:32768].rearrange("p (k n) -> p k n", k=8)
    w_down_b = W[:, 32768:65536].rearrange("p (k n) -> p k n", k=32)

    ident = sb("ident_s", [128, 128])
    identb = sb("identb", [128, 128], BF16)
    triT = sb("triT_s", [128, 128])
    tribias = sb("tribias_s", [128, 128])
    dqk = sb("dqk_s", [128, 8])
    cTs = sb("cTs", [128, 8])
    scT = sb("scT", [128, 8], BF16)
    badaTs = sb("badaTs", [128, 48])
    modT = sb("modT", [128, 48])
    gate_a = sb("gate_a", [128, 1024])
    lng = sb("lng", [128, 1024])
    lnb = sb("lnb", [128, 1024])
    xts = [sb("xt%d" % i, [128, 1024]) for i in range(2)]
    hTs = [sb("hT%d" % i, [128, 8, 128], BF16) for i in range(2)]
    xt, hT = xts[0], hTs[0]
    z = sb("z", [128, 3072])
    Bar = sb("Bar", [128, 4096], BF16)
    rtab = [sb("rtab%d" % i, [128, 288]) for i in range(2)]
    cm = rtab[0][:, 0:128].rearrange("p (a b) -> p a b", a=4)
    cmaskN = rtab[0][0:32, 128:256].rearrange("p (a b) -> p a b", a=4)
    qr = sb("qr", [128, 256])
    kr = sb("kr", [128, 256])
    krb = sb("krb", [128, 256], BF16)
    rvb = sb("rvb", [128, 512], BF16)
    qT = sb("qT", [128, 2, 128], BF16)
    kT = sb("kT", [128, 2, 128], BF16)
    mqT = sb("mqT", [128, 4, 128], BF16)
    kmT = sb("kmT", [128, 4, 16])
    kmsum = sb("kmsum", [128, 4, 8])
    kmb = sb("kmb", [128, 4, 8], BF16)
    st32 = sb("st32", [128, 2, 128])
    sttmp = sb("sttmp", [128, 2, 128])
    stb = sb("stb", [128, 2, 128], BF16)
    scTm = [sb("scTm%d" % i, [128, 128], BF16) for i in range(2)]
    mixT = sb("mixT", [128, 8, 128], BF16)
    small = sb("small", [128, 64])
    g8s = [sb("g8_%d" % i, [128, 8]) for i in range(2)]
    mx8s = [sb("mx8_%d" % i, [128, 8]) for i in range(2)]
    biasns = [sb("biasn_%d" % i, [128, 8]) for i in range(2)]
    rss = [sb("rs_%d" % i, [128, 16]) for i in range(2)]
    sm2s = [sb("sm2_%d" % i, [128, 2]) for i in range(2)]
    Pm = Bar[:, 0:2048]
    sd = sb("sd", [128, 128])
    PT = [Bar[:, 2048 + i * 1024:3072 + i * 1024].rearrange("p (a b) -> p a b", a=8) for i in range(2)]
    r = sb("r", [128, 1024])
    xn = sb("xn", [128, 1024])
    x1 = sb("x1", [128, 1024])
    ta = xn[:, 0:512]
    tb = xn[:, 512:1024]
    sq = xn
    yn = r[:, 0:512]
    yret = r[:, 512:1024]
    sg = x1[:, 0:512]
    rl = [z[:, i * 512:(i + 1) * 512] for i in range(2)]
    uT = Bar[:, :].rearrange("p (a b) -> p a b", a=32)
    scTb = Bar[:, 0:1024].rearrange("p (a b) -> p a b", a=8)
    mqr = r[:, 0:512]
    mkr = [r[:, 512:1024]] * 2
    Kg = [W[:, 32768 + i * 2048:32768 + (i + 1) * 2048].rearrange("p (a b) -> p a b", a=4) for i in range(2)]
    KTg = [W[:, 36864 + i * 2048:36864 + (i + 1) * 2048].rearrange("p (a b) -> p a b", a=16) for i in range(2)]
    Vg = [W[:, 40960 + i * 2048:40960 + (i + 1) * 2048].rearrange("p (a b) -> p a b", a=4) for i in range(2)]
    PTs = W[:, 45056:47104].rearrange("p (a b) -> p a b", a=64)
    selBs = W[:, 47104:48128].rearrange("p (a b) -> p a b", a=32)
    Dm = W[:, 48128:49152]
    sts32 = W[:, 57344:59392].bitcast(F32).rearrange("p (s a e) -> p s a e", s=4, a=2)
    Sall = [W[:, 59392 + i * 2048:59392 + (i + 1) * 2048].bitcast(F32).rearrange("p (a b) -> p a b", a=32)
            for i in range(2)]
    stsb = W[:, 63488:64512].rearrange("p (s a e) -> p s a e", s=4, a=2)
    krbm = W[:, 64512:65536].rearrange("p (s n) -> p s n", s=4)
    TAIL = ["sts32", "Sall0", "Sall1", "stsb", "krbm"]
    csTs = sb("csTs", [128, 256])
    scTs = sb("scTs", [128, 8, 32], BF16)
    ptb_s = sb("ptb_s", [128, 128], I32)
    idx = sb("idx", [128, 128], I32)
    pcol_s = sb("pcol_s", [128, 1])
    dqks = sb("dqks_s", [32, 8])
    maskS = sb("maskS_s", [32, 32])
    rm = sb("rm_s", [32, 4])
    gtab = sb("gtab_s", [128, 2])
    qTs = sb("qTs", [128, 2, 32], BF16)
    kTs = sb("kTs", [128, 2, 32], BF16)
    qTsm = sb("qTsm", [128, 2, 4, 32], BF16)
    mqTs = sb("mqTs", [128, 4, 32], BF16)
    mkTs = sb("mkTs", [128, 4, 32], BF16)
    vnb = sb("vnb", [32, 512], BF16)
    PN = sb("PN", [32, 32], BF16)
    En = sb("En", [32, 32])
    gsb = sb("gsb", [32, 64])
    g32 = sb("g32", [32, 32])
    mxs = sb("mxs", [32, 8])
    sel = sb("sel", [32, 32])
    rinv = sb("rinv", [128, 32])
    ones_f = sb("ones_f", [128, 1])
    ones_b = sb("ones_b", [128, 128], BF16)

    psf = [nc.alloc_psum_tensor("psf%d" % i, [128, 512], F32) for i in range(6)]
    psb = [nc.alloc_psum_tensor("psb%d" % i, [128, 1024], BF16) for i in range(2)]

    S = Sched(n_dma=20)
    S.cut = cut
    pc = [0, 0]

    def nps():
        i = pc[0] % 4
        pc[0] += 1
        return psf[i], "psf%d" % i

    def npsb():
        i = pc[1] % 2
        pc[1] += 1
        return psb[i], "psb%d" % i

    def bc(ap, shape):
        return ap.to_broadcast(shape)

    def ld(q, dst, src, res, reads=()):
        S.dma(q, lambda e, d=dst, s=src: e.dma_start(out=d, in_=s), reads=reads, writes=res)

    ld("sp", ident[:], ident_d, ["ident"])
    ld("sp", triT[:], triT_d, ["triT"])
    ld("sp", tribias[:], tribias_d, ["tribias"])
    ld("sp", dqk[:], dqk_d, ["dqk"])
    ld("sp", cTs[:], cT, ["cTs"])
    ld("sp", badaTs[:], badaT, ["badaTs"])
    ld("sp", gate_a[:], bga, ["gate_a"])
    ld("sp", xn[:], bgf, ["xn"])
    S.op("dve", lambda e: e.tensor_copy(identb[:], ident[:]), reads=["ident"], writes=["identb"])
    S.op("dve", lambda e: e.memset(st32[:], 0.0), writes=["st32"])
    S.op("dve", lambda e: e.memset(stb[:], 0.0), writes=["stb"])
    S.op("dve", lambda e: e.memset(kmsum[:], 0.0), writes=["kmsum"])
    S.op("dve", lambda e: e.memset(kmb[:], 0.0), writes=["kmb"])

    S.op("act", lambda e: e.activation(scT[:], cTs[:], AF.Silu), reads=["cTs"], writes=["scT"])
    S.op("dve", lambda e: e.tensor_copy(scTb[:], bc(scT[:].unsqueeze(2), [128, 8, 128])),
         reads=["scT"], writes=["scTb"])
    ld("sp", csTs[:], csT_d, ["csTs"])
    ld("sp", ptb_s[:], ptb_d, ["ptb_s"])
    ld("sp", pcol_s[:], pcol_d, ["pcol_s"])
    ld("sp", dqks[:], dqks_d, ["dqks"])
    ld("sp", maskS[:], maskS_d, ["maskS"])
    ld("sp", cm, cm_d.rearrange("p (a b) -> p a b", a=4), ["cm"])
    ld("sp", rm[:], rm_d, ["rm"])
    ld("sp", gtab[:], gtab_d, ["gtab"])
    ld("sp", cmaskN, cmaskN_d.rearrange("p (a b) -> p a b", a=4), ["cmaskN"])
    S.op("dve", lambda e: e.memset(ones_f[:], 1.0), writes=["ones_f"])
    S.op("dve", lambda e: e.memset(ones_b[:], 1.0), writes=["ones_b"])
    S.op("act", lambda e: e.activation(scTs[:], csTs[:].rearrange("p (k r) -> p k r", k=8), AF.Silu),
         reads=["csTs"], writes=["scTs"])
    S.op("dve", lambda e: e.tensor_scalar(idx[:], ptb_s[:], 64.0, pcol_s[:, 0:1], ALU.mult, ALU.add),
         reads=["ptb_s", "pcol_s"], writes=["idx"])
    w_ada_v = w_ada.rearrange("(k p) n -> p k n", p=128)
    psmod, psmod_r = psf[5], "psf5"
    for g in range(12):
        sl = g % 2
        ld("pool", wada[sl], w_ada_v[:, :, g * 512:(g + 1) * 512], ["wada%d" % sl])
        ps2, pr2 = nps()

        def f(e, ps2=ps2, sl=sl):
            for k in range(8):
                ins = e.matmul(ps2[0:32, :], scTs[:, k, :], wada[sl][:, k, :], start=(k == 0), stop=(k == 7))
            return ins
        S.op("pe", f, reads=["scTs", "wada%d" % sl], writes=[pr2])
        ld("sp", mqr[0:32, :], bada32[:, g * 512:(g + 1) * 512], ["mqr"])
        S.op("dve", lambda e, ps2=ps2: e.tensor_tensor(mkr[0][0:32, :], ps2[0:32, :], mqr[0:32, :], ALU.add),
             reads=[pr2, "mqr"], writes=["mkr0"])
        S.dma("sp", lambda e, g=g: e.dma_start(out=modsd[:, g * 512:(g + 1) * 512], in_=mkr[0][0:32, :]),
              reads=["mkr0"], writes=["modsd"])
        if g in (4, 5, 10, 11):
            ps, pr = nps()

            def f(e, ps=ps, sl=sl):
                for k in range(8):
                    ins = e.matmul(ps[:, :], scTb[:, k, :], wada[sl][:, k, :], start=(k == 0), stop=(k == 7))
                return ins
            S.op("pe", f, reads=["scTb", "wada%d" % sl], writes=[pr])
            dst = gate_a if g < 6 else xn
            half = g % 2
            S.op("dve", lambda e, ps=ps, dst=dst, half=half: e.tensor_tensor(
                dst[:, half * 512:(half + 1) * 512], ps[:, :], dst[:, half * 512:(half + 1) * 512], ALU.add),
                reads=[pr], writes=["gate_a" if g < 6 else "xn"])
        else:
            def f(e, sl=sl, g=g):
                for j in range(4):
                    col = g * 4 + j
                    for k in range(8):
                        ins = e.matmul(psmod[:, col:col + 1], wada[sl][:, k, j * 128:(j + 1) * 128],
                                       scT[:, k:k + 1], start=(k == 0), stop=(k == 7))
                return ins
            S.op("pe", f, reads=["scT", "wada%d" % sl], writes=[psmod_r])
    S.op("dve", lambda e: e.tensor_tensor(modT[:], psmod[:, 0:48], badaTs[:], ALU.add),
         reads=[psmod_r, "badaTs"], writes=["modT"])
    S.op("dve", lambda e: e.tensor_scalar_add(modT[:, 8:16], modT[:, 8:16], 1.0), reads=["modT"], writes=["modT"])
    S.op("dve", lambda e: e.tensor_scalar_add(modT[:, 32:40], modT[:, 32:40], 1.0), reads=["modT"], writes=["modT"])

    S.dma("sp", lambda e: e.dma_start(out=gfd, in_=xn[:]), reads=["xn"], writes=["gfd"])
    w_in_v = w_in.rearrange("(k p) n -> p k n", p=128)
    for k2 in range(4):
        ld("pool", w_in_b[:, 2 * k2:2 * k2 + 2, :], w_in_v[:, 2 * k2:2 * k2 + 2, :], ["w_in%d" % k2])
    WIN = ["w_in%d" % i for i in range(4)]
    ld("pool", w_o_b, w_o.rearrange("(k p) n -> p k n", p=128), ["w_o"])
    ld("sp", lng[:], l1g, ["lng"])
    ld("sp", lnb[:], l1b, ["lnb"])

    def transposes_to(dst_fn, src_fn, n, reads, writes_res, evac_eng="act", scale_fn=None):
        for b0 in range(0, n, 4):
            nb = min(4, n - b0)
            ps, pr = nps()

            def f(e, ps=ps, b0=b0, nb=nb):
                for j in range(nb):
                    ins = e.transpose(ps[:, j * 128:(j + 1) * 128], src_fn(b0 + j), ident[:])
                return ins
            S.op("pe", f, reads=list(reads) + ["ident"], writes=[pr])
            if scale_fn is None:
                if evac_eng == "act":
                    S.op("act", lambda e, ps=ps, b0=b0, nb=nb: e.copy(dst_fn(b0, nb), ps[:, 0:nb * 128].rearrange("p (a b) -> p a b", a=nb)),
                         reads=[pr], writes=writes_res)
                else:
                    S.op("dve", lambda e, ps=ps, b0=b0, nb=nb: e.tensor_copy(dst_fn(b0, nb), ps[:, 0:nb * 128].rearrange("p (a b) -> p a b", a=nb)),
                         reads=[pr], writes=writes_res)
            else:
                for j in range(nb):
                    sc, bi = scale_fn(b0 + j)
                    S.op("act", lambda e, ps=ps, j=j, b0=b0, sc=sc, bi=bi: e.activation(
                        dst_fn(b0 + j, 1), ps[:, j * 128:(j + 1) * 128], AF.Identity, bias=bi, scale=sc),
                        reads=[pr, "modT"], writes=writes_res)

    def layer_norm(src_ps_list, src_res, resid, resid_res, gate, gate_res, out_t, out_res, P=128):
        for hf in range(2):
            S.op("dve", lambda e, hf=hf: e.tensor_tensor(
                r[0:P, hf * 512:(hf + 1) * 512], src_ps_list[hf][0:P, :], gate[0:P, hf * 512:(hf + 1) * 512], ALU.mult),
                reads=[src_res[hf]] + list(gate_res), writes=["r"])
        S.op("dve", lambda e: e.scalar_tensor_tensor(r[0:P, :], resid[0:P, :], ALPHA, r[0:P, :], ALU.mult, ALU.add),
             reads=[resid_res, "r"], writes=["r"])
        S.op("dve", lambda e: e.reduce_sum(small[0:P, 0:1], r[0:P, :], AX.X), reads=["r"], writes=["sm0"])
        S.op("act", lambda e: e.activation(sq[0:P, :], r[0:P, :], AF.Square), reads=["r"], writes=["sq"])
        S.op("dve", lambda e: e.reduce_sum(small[0:P, 1:2], sq[0:P, :], AX.X), reads=["sq"], writes=["sm1"])
        S.op("dve", lambda e: e.tensor_scalar_mul(small[0:P, 2:3], small[0:P, 0:1], 1.0 / 1024), reads=["sm0"], writes=["sm2"])
        S.op("dve", lambda e: e.tensor_tensor(small[0:P, 3:4], small[0:P, 2:3], small[0:P, 2:3], ALU.mult),
             reads=["sm2"], writes=["sm3"])
        S.op("dve", lambda e: e.scalar_tensor_tensor(small[0:P, 4:5], small[0:P, 1:2], 1.0 / 1024, small[0:P, 3:4],
                                                     ALU.mult, ALU.subtract), reads=["sm1", "sm3"], writes=["sm4"])
        S.op("dve", lambda e: e.tensor_scalar_add(small[0:P, 4:5], small[0:P, 4:5], LN_EPS), reads=["sm4"], writes=["sm4"])
        S.op("act", lambda e: e.sqrt(small[0:P, 7:8], small[0:P, 4:5]), reads=["sm4"], writes=["sm7"])
        S.op("dve", lambda e: e.reciprocal(small[0:P, 5:6], small[0:P, 7:8]), reads=["sm7"], writes=["sm5"])
        S.op("dve", lambda e: e.scalar_tensor_tensor(small[0:P, 6:7], small[0:P, 2:3], -1.0, small[0:P, 5:6],
                                                     ALU.mult, ALU.mult), reads=["sm2", "sm5"], writes=["sm6"])
        S.op("act", lambda e: e.activation(xn[0:P, :], r[0:P, :], AF.Identity, bias=small[0:P, 6:7], scale=small[0:P, 5:6]),
             reads=["r", "sm5", "sm6"], writes=["xn"])
        S.op("dve", lambda e: e.tensor_tensor(xn[0:P, :], xn[0:P, :], lng[0:P, :], ALU.mult), reads=["xn", "lng"], writes=["xn"])
        S.op("dve", lambda e: e.tensor_tensor(out_t[0:P, :], xn[0:P, :], lnb[0:P, :], ALU.add), reads=["xn", "lnb"], writes=[out_res])

    def do_rope(P, rt, rtr, src, H, c0, dst, dec, zres, dres, dq_t, dq_r):
        n = 8 * H
        s4 = src.rearrange("p (h t j) -> p h t j", h=4, t=2)
        ta4 = ta[0:P, 0:n].rearrange("p (h t j) -> p h t j", h=4, t=2)
        tb4 = tb[0:P, 0:n].rearrange("p (h t j) -> p h t j", h=4, t=2)
        cosb = bc(rt[0:P, c0:c0 + H].unsqueeze(1).unsqueeze(1), [P, 4, 2, H])
        snb = bc(rt[0:P, c0 + H:c0 + 2 * H].unsqueeze(1), [P, 4, H])
        spb = bc(rt[0:P, c0 + 2 * H:c0 + 3 * H].unsqueeze(1), [P, 4, H])
        S.op("dve", lambda e: e.tensor_tensor(ta4, s4, cosb, ALU.mult), reads=[zres, rtr], writes=["ta"])
        S.op("dve", lambda e: e.tensor_tensor(tb4[:, :, 0, :], s4[:, :, 1, :], snb, ALU.mult),
             reads=[zres, rtr], writes=["tb"])
        S.op("dve", lambda e: e.tensor_tensor(tb4[:, :, 1, :], s4[:, :, 0, :], spb, ALU.mult),
             reads=[zres, rtr], writes=["tb"])
        if dec is None:
            S.op("dve", lambda e: e.tensor_tensor(dst, ta[0:P, 0:n], tb[0:P, 0:n], ALU.add),
                 reads=["ta", "tb"], writes=[dres])
        else:
            S.op("dve", lambda e: e.tensor_tensor(ta[0:P, 0:n], ta[0:P, 0:n], tb[0:P, 0:n], ALU.add),
                 reads=["ta", "tb"], writes=["ta"])
            decb = bc(dq_t[0:P, dec:dec + 4].unsqueeze(2), [P, 4, 2 * H])
            S.op("dve", lambda e: e.tensor_tensor(dst.rearrange("p (h j) -> p h j", h=4),
                                                  ta[0:P, 0:n].rearrange("p (h j) -> p h j", h=4), decb, ALU.mult),
                 reads=["ta", dq_r], writes=[dres])

    def group_norm(P, pso, pso_r):
        pso4 = pso[0:P, :].rearrange("p (h n) -> p h n", h=4)
        S.op("dve", lambda e: e.reduce_sum(small[0:P, 8:12], pso4, AX.X), reads=[pso_r], writes=["g0"])
        S.op("act", lambda e: e.activation(sq[0:P, 0:512], pso[0:P, :], AF.Square), reads=[pso_r], writes=["sq"])
        S.op("dve", lambda e: e.reduce_sum(small[0:P, 12:16], sq[0:P, 0:512].rearrange("p (h n) -> p h n", h=4), AX.X),
             reads=["sq"], writes=["g1"])
        S.op("dve", lambda e: e.tensor_scalar_mul(small[0:P, 16:20], small[0:P, 8:12], 1.0 / 128), reads=["g0"], writes=["g2"])
        S.op("dve", lambda e: e.tensor_tensor(small[0:P, 20:24], small[0:P, 16:20], small[0:P, 16:20], ALU.mult),
             reads=["g2"], writes=["g3"])
        S.op("dve", lambda e: e.scalar_tensor_tensor(small[0:P, 24:28], small[0:P, 12:16], 1.0 / 128, small[0:P, 20:24],
                                                     ALU.mult, ALU.subtract), reads=["g1", "g3"], writes=["g4"])
        S.op("dve", lambda e: e.tensor_scalar_add(small[0:P, 24:28], small[0:P, 24:28], GN_EPS), reads=["g4"], writes=["g4"])
        S.op("act", lambda e: e.sqrt(small[0:P, 36:40], small[0:P, 24:28]), reads=["g4"], writes=["g7"])
        S.op("dve", lambda e: e.reciprocal(small[0:P, 28:32], small[0:P, 36:40]), reads=["g7"], writes=["g5"])
        S.op("dve", lambda e: e.scalar_tensor_tensor(small[0:P, 32:36], small[0:P, 16:20], -1.0, small[0:P, 28:32],
                                                     ALU.mult, ALU.mult), reads=["g2", "g5"], writes=["g6"])
        for h in range(4):
            S.op("act", lambda e, h=h: e.activation(yn[0:P, h * 128:(h + 1) * 128], pso[0:P, h * 128:(h + 1) * 128],
                                                    AF.Identity, bias=small[0:P, 32 + h:33 + h],
                                                    scale=small[0:P, 28 + h:29 + h]),
                 reads=[pso_r, "g5", "g6"], writes=["yn"])
        S.op("dve", lambda e: e.tensor_tensor(yret[0:P, :], yn[0:P, :], sg[0:P, :], ALU.mult),
             reads=["yn", "sg"], writes=["yret"])

    S.marks.append((S.seq, 'sample mixer'))
    P = 32
    IOA = bass.IndirectOffsetOnAxis
    rtS, rtSr = rtab[1], "rtab1"
    ld("sp", xt[0:P, :], xs_d, ["xt"])
    ld("sp", rtS[0:P, :], ropeS_d, [rtSr])
    ld("sp", sts32, st_in.rearrange("s (a q) e -> q s a e", q=128), ["sts32"])
    S.op("act", lambda e: e.copy(stsb, sts32), reads=["sts32"], writes=["stsb"])
    ld("sp", r[0:P, :], modsd[:, 1024:2048], ["r"], reads=["modsd"])
    ld("sp", xn[0:P, :], modsd[:, 0:1024], ["xn"], reads=["modsd"])
    S.op("dve", lambda e: e.scalar_tensor_tensor(r[0:P, :], r[0:P, :], 1.0, xt[0:P, :], ALU.add, ALU.mult),
         reads=["r", "xt"], writes=["r"])
    S.op("dve", lambda e: e.tensor_tensor(r[0:P, :], r[0:P, :], xn[0:P, :], ALU.add), reads=["r", "xn"], writes=["r"])

    def tr32(src_fn, n, reads, dst, dst_res, eng="act"):
        ps, pr = nps()

        def f(e, ps=ps):
            for j in range(n):
                ins = e.transpose(ps[:, j * 32:(j + 1) * 32], src_fn(j), ident[0:32, 0:32])
            return ins
        S.op("pe", f, reads=list(reads) + ["ident"], writes=[pr])
        src = ps[:, 0:n * 32].rearrange("p (a b) -> p a b", a=n)
        if eng == "act":
            S.op("act", lambda e: e.copy(dst, src), reads=[pr], writes=[dst_res])
        else:
            S.op("dve", lambda e: e.tensor_copy(dst, src), reads=[pr], writes=[dst_res])

    tr32(lambda k: r[0:P, k * 128:(k + 1) * 128], 8, ["r"], hT[:, :, 0:32], "hT")
    for g in range(6):
        ps, pr = nps()

        def f(e, ps=ps, g=g):
            for k in range(8):
                ins = e.matmul(ps[0:P, :], hT[:, k, 0:32], w_in_b[:, k, g * 512:(g + 1) * 512],
                               start=(k == 0), stop=(k == 7))
            return ins
        S.op("pe", f, reads=["hT"] + WIN, writes=[pr])
        S.op("act", lambda e, ps=ps, g=g: e.copy(z[0:P, g * 512:(g + 1) * 512], ps[0:P, :]),
             reads=[pr], writes=["z%d" % g])
    S.dma("sp", lambda e: e.dma_start(out=vs, in_=z[0:P, 2560:3072]), reads=["z5"])
    S.op("act", lambda e: e.activation(sg[0:P, :], z[0:P, 1024:1536], AF.Silu), reads=["z2"], writes=["sg"])
    S.op("act", lambda e: e.copy(rvb[0:P, :], z[0:P, 512:1024]), reads=["z1"], writes=["rvb"])
    S.op("act", lambda e: e.copy(vnb[:], z[0:P, 2560:3072]), reads=["z5"], writes=["vnb"])
    mks = mkr[0]
    do_rope(P, rtS, rtSr, z[0:P, 0:256], 32, 192, qr[0:P, :], 0, "z0", "qr", dqks, "dqks")
    do_rope(P, rtS, rtSr, z[0:P, 256:512], 32, 192, kr[0:P, :], 4, "z0", "kr", dqks, "dqks")
    do_rope(P, rtS, rtSr, z[0:P, 1536:2048], 64, 0, mqr[0:P, :], None, "z3", "mqr", dqks, "dqks")
    do_rope(P, rtS, rtSr, z[0:P, 2048:2560], 64, 0, mks[0:P, :], None, "z4", "mkr0", dqks, "dqks")
    S.dma("sp", lambda e: e.dma_start(out=ks, in_=mks[0:P, :]), reads=["mkr0"])
    S.op("dve", lambda e: e.tensor_tensor(krbm[0:P, :, :], bc(kr[0:P, :].unsqueeze(1), [P, 4, 256]),
                                          bc(rm[0:P, :].unsqueeze(2), [P, 4, 256]), ALU.mult),
         reads=["kr", "rm"], writes=["krbm"])
    tr32(lambda j: qr[0:P, j * 128:(j + 1) * 128], 2, ["qr"], qTs[:], "qTs")
    tr32(lambda j: kr[0:P, j * 128:(j + 1) * 128], 2, ["kr"], kTs[:], "kTs")
    tr32(lambda j: mqr[0:P, j * 128:(j + 1) * 128], 4, ["mqr"], mqTs[:], "mqTs")
    tr32(lambda j: mks[0:P, j * 128:(j + 1) * 128], 4, ["mkr0"], mkTs[:], "mkTs")
    S.op("dve", lambda e: e.tensor_tensor(qTsm[:], bc(qTs[:].unsqueeze(2), [128, 2, 4, 32]),
                                          bc(cm.unsqueeze(1), [128, 2, 4, 32]), ALU.mult),
         reads=["qTs", "cm"], writes=["qTsm"])
    pso, pso_r = psf[4], "psf4"
    for h in range(4):
        p_, hh = h // 2, h % 2
        lo, hi = hh * 64, hh * 64 + 64
        ps, pr = nps()
        S.op("pe", lambda e, ps=ps, p_=p_, lo=lo, hi=hi: e.matmul(
            ps[0:P, 0:32], kTs[lo:hi, p_, :], qTs[lo:hi, p_, :], start=True, stop=True),
            reads=["kTs", "qTs"], writes=[pr])
        sm = scTm[h % 2]
        smr = "scTm%d" % (h % 2)
        S.op("dve", lambda e, ps=ps, sm=sm: e.tensor_tensor(sm[0:P, 0:32], ps[0:P, 0:32], maskS[:], ALU.mult),
             reads=[pr, "maskS"], writes=[smr])

        def f(e, sm=sm, h=h, p_=p_, lo=lo, hi=hi):
            e.matmul(pso[0:P, h * 128:(h + 1) * 128], sm[0:P, 0:32], rvb[0:P, h * 128:(h + 1) * 128],
                     start=True, stop=False)
            for s_ in range(4):
                ins = e.matmul(pso[0:P, h * 128:(h + 1) * 128], qTsm[lo:hi, p_, s_, :], stsb[lo:hi, s_, p_, :],
                               start=False, stop=(s_ == 3))
            return ins
        S.op("pe", f, reads=[smr, "rvb", "qTsm", "stsb"], writes=[pso_r])
    for s_ in range(4):
        ps, pr = nps()

        def f(e, ps=ps, s_=s_):
            for p_ in range(2):
                ins = e.matmul(ps[:, p_ * 256:(p_ + 1) * 256], krbm[0:P, s_, p_ * 128:(p_ + 1) * 128],
                               rvb[0:P, p_ * 256:(p_ + 1) * 256], start=True, stop=True)
            return ins
        S.op("pe", f, reads=["krbm", "rvb"], writes=[pr])
        for hh in range(2):
            lo, hi = hh * 64, hh * 64 + 64
            S.op("dve", lambda e, ps=ps, s_=s_, hh=hh, lo=lo, hi=hi: e.tensor_tensor(
                sttmp[lo:hi, :, :], sts32[lo:hi, s_, :, :],
                ps[lo:hi, :].rearrange("q (a h e) -> q a h e", a=2, h=2)[:, :, hh, :], ALU.add),
                reads=["sts32", pr, pso_r], writes=["sttmp"])
            S.op("dve", lambda e, s_=s_, lo=lo, hi=hi: e.tensor_tensor(
                sts32[lo:hi, s_, :, :], sttmp[lo:hi, :, :], bc(gtab[lo:hi, :].unsqueeze(2), [64, 2, 128]), ALU.mult),
                reads=["sttmp", "gtab"], writes=["sts32"])
    S.dma("sp", lambda e: e.dma_start(out=ss.rearrange("s (a q) e -> q s a e", q=128), in_=sts32), reads=["sts32"])
    group_norm(P, pso, pso_r)
    tr32(lambda j: yret[0:P, j * 128:(j + 1) * 128], 4, ["yret"], mixT[:, 0:4, 0:32], "mixT")

    psO, psO_r = psf[5], "psf5"
    gcnt = [0, 0]
    for s_ in range(4):
        S.marks.append((S.seq, 'smoba %d' % s_))
        qsl = slice(s_ * 8, (s_ + 1) * 8)
        for gi in range(16):
            sl = gcnt[0] % 2
            gcnt[0] += 1
            for j in range(2):
                col = s_ * 32 + gi * 2 + j
                S.dma("pool", lambda e, sl=sl, j=j, col=col: e.indirect_dma_start(
                    out=W[:, 32768 + sl * 2048 + j * 1024:32768 + sl * 2048 + (j + 1) * 1024], out_offset=None, in_=ck,
                    in_offset=IOA(ap=idx[:, col:col + 1], axis=0)),
                    reads=["idx"], writes=["Kg%d_%d" % (sl, j)])
            for half in range(2):
                pb, pbr = npsb()

                def f(e, pb=pb, sl=sl, half=half):
                    for i in range(8):
                        jj, hh_ = (half * 8 + i) // 4, (half * 8 + i) % 4
                        ins = e.transpose(pb[:, i * 128:(i + 1) * 128], Kg[sl][:, jj, hh_ * 128:(hh_ + 1) * 128], identb[:])
                    return ins
                S.op("pe", f, reads=["Kg%d_%d" % (sl, jx) for jx in range(4)] + ["identb"], writes=[pbr])
                srcv = pb[:, :].rearrange("p (a b) -> p a b", a=8)
                if half == 0:
                    S.op("act", lambda e, sl=sl, srcv=srcv: e.copy(KTg[sl][:, 0:8, :], srcv),
                         reads=[pbr], writes=["KTg%d" % sl])
                else:
                    S.op("dve", lambda e, sl=sl, srcv=srcv: e.tensor_copy(KTg[sl][:, 8:16, :], srcv),
                         reads=[pbr], writes=["KTg%d" % sl])
            ps, pr = nps()

            def f(e, ps=ps, sl=sl, qsl=qsl):
                for i in range(16):
                    ins = e.matmul(ps[:, i * 8:(i + 1) * 8], KTg[sl][:, i, :], mqTs[:, i % 4, qsl], start=True, stop=True)
                return ins
            S.op("pe", f, reads=["KTg%d" % sl, "mqTs"], writes=[pr])
            hfS, pg0 = gi // 8, (gi % 8) * 4
            S.op("dve", lambda e, ps=ps, hfS=hfS, pg0=pg0: e.tensor_copy(
                Sall[hfS][:, pg0:pg0 + 4, :], ps[:, 0:128].rearrange("p (a b) -> p a b", a=4)),
                reads=[pr], writes=["Sall%d" % hfS])
        ps, pr = nps()

        def f(e, ps=ps):
            for pg in range(64):
                ins = e.matmul(ps[0:32, pg:pg + 1], Sall[pg // 32][:, pg % 32, :], ones_f[:, 0:1], start=True, stop=True)
            return ins
        S.op("pe", f, reads=["Sall0", "Sall1", "ones_f"], writes=[pr])
        S.op("act", lambda e, ps=ps: e.copy(gsb[:], ps[0:32, 0:64]), reads=[pr], writes=["gsb"])
        gs3 = gsb[:].rearrange("p (n t) -> p n t", t=2)
        S.op("dve", lambda e, gs3=gs3: e.tensor_tensor(g32[:], gs3[:, :, 0], gs3[:, :, 1], ALU.add),
             reads=["gsb"], writes=["g32"])
        S.op("dve", lambda e: e.max(mxs[:], g32[:]), reads=["g32"], writes=["mxs"])
        S.op("dve", lambda e: e.tensor_scalar(sel[:], g32[:], mxs[:, 2:3], None, ALU.is_ge),
             reads=["g32", "mxs"], writes=["sel"])
        Dm3 = Dm[0:32, :].rearrange("p (n q) -> p n q", n=32)
        S.op("dve", lambda e, Dm3=Dm3: e.tensor_tensor(Dm3, bc(sel[:].unsqueeze(2), [32, 32, 32]),
                                                       bc(ident[0:32, 0:32].unsqueeze(1), [32, 32, 32]), ALU.mult),
             reads=["sel", "ident"], writes=["Dm"])
        for half in range(2):
            ps, pr = nps()
            S.op("pe", lambda e, ps=ps, half=half: e.matmul(ps[:, :], ones_b[0:32, :], Dm[0:32, half * 512:(half + 1) * 512],
                                                            start=True, stop=True),
                 reads=["ones_b", "Dm"], writes=[pr])
            S.op("act", lambda e, ps=ps, half=half: e.copy(selBs[:, half * 16:(half + 1) * 16, :],
                                                           ps[:, :].rearrange("p (a b) -> p a b", a=16)),
                 reads=[pr], writes=["selBs"])
        for hfS in range(2):
            S.op("act", lambda e, hfS=hfS: e.activation(Sall[hfS], Sall[hfS], AF.Exp, scale=SCALE),
                 reads=["Sall%d" % hfS], writes=["Sall%d" % hfS])
            S.op("dve", lambda e, hfS=hfS: e.tensor_tensor(
                PTs[:, hfS * 32:(hfS + 1) * 32, :].rearrange("p (n t) q -> p n t q", t=2),
                Sall[hfS].rearrange("p (n t) q -> p n t q", t=2),
                bc(selBs[:, hfS * 16:(hfS + 1) * 16, :].unsqueeze(2), [128, 16, 2, 32]), ALU.mult),
                reads=["Sall%d" % hfS, "selBs"], writes=["PTs"])
        ps, pr = nps()

        def f(e, ps=ps, qsl=qsl):
            for h in range(4):
                ins = e.matmul(ps[0:32, h * 8:(h + 1) * 8], mkTs[:, h, :], mqTs[:, h, qsl], start=True, stop=True)
            return ins
        S.op("pe", f, reads=["mkTs", "mqTs"], writes=[pr])
        S.op("act", lambda e, ps=ps: e.activation(En[:], ps[0:32, 0:32], AF.Exp, scale=SCALE), reads=[pr], writes=["En"])
        S.op("dve", lambda e, s_=s_: e.tensor_tensor(PN[:], En[:], cmaskN[:, s_, :], ALU.mult),
             reads=["En", "cmaskN"], writes=["PN"])
        for gi in range(16):
            sl = gcnt[1] % 2
            gcnt[1] += 1
            for j in range(2):
                col = s_ * 32 + gi * 2 + j
                S.dma("pool", lambda e, sl=sl, j=j, col=col: e.indirect_dma_start(
                    out=W[:, 40960 + sl * 2048 + j * 1024:40960 + sl * 2048 + (j + 1) * 1024], out_offset=None, in_=cv,
                    in_offset=IOA(ap=idx[:, col:col + 1], axis=0)),
                    reads=["idx"], writes=["Vg%d_%d" % (sl, j)])

            def f(e, sl=sl, gi=gi):
                for j in range(4):
                    pg = gi * 4 + j
                    for h in range(4):
                        e.matmul(psO[:, h * 8:(h + 1) * 8], Vg[sl][:, j, h * 128:(h + 1) * 128], PTs[:, pg, h * 8:(h + 1) * 8],
                                 start=(pg == 0), stop=False)
                    ins = e.matmul(psO[:, 32:64], ones_b[:, :], PTs[:, pg, :], start=(pg == 0), stop=False)
                return ins
            S.op("pe", f, reads=["Vg%d_%d" % (sl, jx) for jx in range(4)] + ["PTs", "ones_b"], writes=[psO_r])

        def f(e):
            for h in range(4):
                e.matmul(psO[:, h * 8:(h + 1) * 8], vnb[0:32, h * 128:(h + 1) * 128], PN[0:32, h * 8:(h + 1) * 8],
                         start=False, stop=True)
            return e.matmul(psO[:, 32:64], ones_b[0:32, :], PN[0:32, :], start=False, stop=True)
        S.op("pe", f, reads=["vnb", "PN", "ones_b"], writes=[psO_r])
        S.op("dve", lambda e: e.reciprocal(rinv[:], psO[:, 32:64]), reads=[psO_r], writes=["rinv"])
        S.op("dve", lambda e, qsl=qsl: e.tensor_tensor(mixT[:, 4:8, qsl], psO[:, 0:32].rearrange("p (h q) -> p h q", h=4),
                                                       rinv[:].rearrange("p (h q) -> p h q", h=4), ALU.mult),
             reads=[psO_r, "rinv"], writes=["mixT"])
    S.marks.append((S.seq, 'sample oproj'))
    ld("sp", z[0:P, 0:1024], modsd[:, 2048:3072], ["z0", "z1"], reads=["modsd"])
    pss = [nps(), nps()]
    for hf in range(2):
        def f(e, hf=hf, ps=pss[hf][0]):
            for c in range(8):
                ins = e.matmul(ps[0:P, :], mixT[:, c, 0:32], w_o_b[:, c, hf * 512:(hf + 1) * 512],
                               start=(c == 0), stop=(c == 7))
            return ins
        S.op("pe", f, reads=["mixT", "w_o"], writes=[pss[hf][1]])
    layer_norm([pss[0][0], pss[1][0]], [pss[0][1], pss[1][1]], xt, "xt", z[:, 0:1024], ["z0", "z1"], x1, "x1", P=P)
    S.dma("sp", lambda e: e.dma_start(out=x1d[2048:2080, :], in_=x1[0:P, :]), reads=["x1"], writes=["x1ds"])

    def load_tile(t):
        rt_ = rtab[t % 2]
        rtr_ = "rtab%d" % (t % 2)
        ld("sp", xts[t % 2][:], x[t * 128:(t + 1) * 128, :], ["xt%d" % (t % 2)])
        ld("sp", rt_[:, 0:192], ropeM[t * 128:(t + 1) * 128, :], [rtr_])
        ld("sp", rt_[:, 192:288], ropeR[t * 128:(t + 1) * 128, :], [rtr_])

    def p1(t, stg):
        S.marks.append((S.seq, 'p1 tile %d' % t))
        rt = rtab[t % 2]
        rtr = "rtab%d" % (t % 2)
        mk = mkr[t % 2]
        mkres = "mkr%d" % (t % 2)
        xt_t, hT_t = xts[t % 2], hTs[t % 2]
        xtr, hTr = "xt%d" % (t % 2), "hT%d" % (t % 2)
        if stg == 'A':
            transposes_to(lambda b, n, hT_t=hT_t: hT_t[:, b, :], lambda k, xt_t=xt_t: xt_t[:, k * 128:(k + 1) * 128],
                          8, [xtr], [hTr], scale_fn=lambda k: (modT[:, 8 + k:9 + k], modT[:, k:k + 1]))
            for g in range(6):
                ps, pr = nps()

                def f(e, ps=ps, g=g, hT_t=hT_t):
                    for k in range(8):
                        ins = e.matmul(ps[:, :], hT_t[:, k, :], w_in_b[:, k, g * 512:(g + 1) * 512],
                                       start=(k == 0), stop=(k == 7))
                    return ins
                S.op("pe", f, reads=[hTr] + WIN, writes=[pr])
                if g % 2 == 0:
                    S.op("act", lambda e, ps=ps, g=g: e.copy(z[:, g * 512:(g + 1) * 512], ps[:, :]),
                         reads=[pr], writes=["z%d" % g])
                else:
                    S.op("dve", lambda e, ps=ps, g=g: e.tensor_copy(z[:, g * 512:(g + 1) * 512], ps[:, :]),
                         reads=[pr], writes=["z%d" % g])
        if stg == 'B':
            if t + 1 < NT:
                load_tile(t + 1)
            S.dma("sp", lambda e, t=t: e.dma_start(out=vout[t * 128:(t + 1) * 128, :], in_=z[:, 2560:3072]),
                  reads=["z5"])
            S.op("act", lambda e: e.activation(sg[:], z[:, 1024:1536], AF.Silu), reads=["z2"], writes=["sg"])
            S.op("act", lambda e: e.copy(rvb[:], z[:, 512:1024]), reads=["z1"], writes=["rvb"])
            S.op("act", lambda e, t=t: e.copy(Vb[:, t, :], z[:, 2560:3072]), reads=["z5"], writes=["Vb"])

            do_rope(128, rt, rtr, z[:, 0:256], 32, 192, qr[:], 0, "z0", "qr", dqk, "dqk")
            do_rope(128, rt, rtr, z[:, 256:512], 32, 192, kr[:], 4, "z0", "kr", dqk, "dqk")
            do_rope(128, rt, rtr, z[:, 1536:2048], 64, 0, mqr[:], None, "z3", "mqr", dqk, "dqk")
            do_rope(128, rt, rtr, z[:, 2048:2560], 64, 0, mk[:], None, "z4", mkres, dqk, "dqk")
            S.dma("sp", lambda e, t=t, mk=mk: e.dma_start(out=kout[t * 128:(t + 1) * 128, :], in_=mk[:]), reads=[mkres])
            S.op("act", lambda e: e.copy(krb[:], kr[:]), reads=["kr"], writes=["krb"])
            transposes_to(lambda b, n: qT[:, b:b + n, :], lambda j: qr[:, j * 128:(j + 1) * 128], 2, ["qr"], ["qT"])
            transposes_to(lambda b, n: kT[:, b:b + n, :], lambda j: kr[:, j * 128:(j + 1) * 128], 2, ["kr"], ["kT"])
            transposes_to(lambda b, n: mqT[:, b:b + n, :], lambda j: mqr[:, j * 128:(j + 1) * 128], 4, ["mqr"], ["mqT"])
            ps, pr = nps()

            def f(e, ps=ps, mk=mk):
                for j in range(4):
                    ins = e.transpose(ps[:, j * 128:(j + 1) * 128], mk[:, j * 128:(j + 1) * 128], ident[:])
                return ins
            S.op("pe", f, reads=[mkres, "ident"], writes=[pr])
            S.op("act", lambda e, ps=ps, t=t: e.copy(KT[:, :, t * 128:(t + 1) * 128],
                                                      ps[:, :].rearrange("p (h n) -> p h n", h=4)),
                 reads=[pr], writes=["KT"])
            S.op("dve", lambda e, ps=ps, t=t: e.reduce_sum(kmT[:, :, t], ps[:, :].rearrange("p (h n) -> p h n", h=4), AX.X),
                 reads=[pr, "KT"], writes=["kmT"])
            if t % 2 == 1:
                n = t // 2
                S.op("dve", lambda e, n=n: e.tensor_tensor(kmsum[:, :, n], kmT[:, :, 2 * n], kmT[:, :, 2 * n + 1], ALU.add),
                     reads=["kmT"], writes=["kmsum"])
                S.op("dve", lambda e, n=n: e.tensor_scalar_mul(kmb[:, :, n], kmsum[:, :, n], 1.0 / 256),
                     reads=["kmsum"], writes=["kmb"])

            S.marks.append((S.seq, 'ret %d' % t))
            pso, pso_r = psf[4], "psf4"
            for h in range(4):
                p_, hh = h // 2, h % 2
                lo, hi = hh * 64, hh * 64 + 64
                ps, pr = nps()
                S.op("pe", lambda e, ps=ps, p_=p_, lo=lo, hi=hi: e.matmul(
                    ps[:, 0:128], kT[lo:hi, p_, :], qT[lo:hi, p_, :], start=True, stop=True),
                    reads=["kT", "qT"], writes=[pr])
                sm = scTm[h % 2]
                smr = "scTm%d" % (h % 2)
                S.op("dve", lambda e, ps=ps, sm=sm: e.tensor_tensor(sm[:], ps[:, 0:128], triT[:], ALU.mult),
                     reads=[pr, "triT"], writes=[smr])

                def f(e, sm=sm, h=h, p_=p_, lo=lo, hi=hi):
                    e.matmul(pso[:, h * 128:(h + 1) * 128], sm[:], rvb[:, h * 128:(h + 1) * 128], start=True, stop=False)
                    return e.matmul(pso[:, h * 128:(h + 1) * 128], qT[lo:hi, p_, :], stb[lo:hi, p_, :],
                                    start=False, stop=True)
                S.op("pe", f, reads=[smr, "rvb", "qT", "stb"], writes=[pso_r])
            for p_ in range(2):
                ps, pr = nps()
                S.op("pe", lambda e, ps=ps, p_=p_: e.matmul(
                    ps[:, 0:256], krb[:, p_ * 128:(p_ + 1) * 128], rvb[:, p_ * 256:(p_ + 1) * 256], start=True, stop=True),
                    reads=["krb", "rvb"], writes=[pr])
                for hh in range(2):
                    h = 2 * p_ + hh
                    lo, hi = hh * 64, hh * 64 + 64
                    gC = GAM[h] ** 128
                    S.op("dve", lambda e, ps=ps, p_=p_, hh=hh, lo=lo, hi=hi: e.tensor_tensor(
                        sttmp[lo:hi, p_, :], st32[lo:hi, p_, :], ps[lo:hi, hh * 128:(hh + 1) * 128], ALU.add),
                        reads=["st32", pr, pso_r], writes=["sttmp"])
                    S.op("dve", lambda e, p_=p_, lo=lo, hi=hi, gC=gC: e.tensor_scalar_mul(
                        st32[lo:hi, p_, :], sttmp[lo:hi, p_, :], gC), reads=["sttmp"], writes=["st32"])
                    S.op("act", lambda e, p_=p_, lo=lo, hi=hi, gC=gC: e.mul(
                        stb[lo:hi, p_, :], sttmp[lo:hi, p_, :], gC), reads=["sttmp"], writes=["stb"])
            S.marks.append((S.seq, 'gn %d' % t))
            group_norm(128, pso, pso_r)
            transposes_to(lambda b, n: mixT[:, b:b + n, :], lambda j: yret[:, j * 128:(j + 1) * 128], 4,
                          ["yret"], ["mixT"])

        if stg == 'C':
            S.marks.append((S.seq, 'moba %d' % t))
            own = t // 2
            nkt = t + 1
            psO, psO_r = psf[5], "psf5"
            for h in range(4):
                hp = h % 2
                g8, mx8, biasn, rs, sm2 = g8s[hp], mx8s[hp], biasns[hp], rss[hp], sm2s[hp]
                g8r, mx8r, biasnr, rsr, m0r, m1r = ['%s%d' % (nm, hp) for nm in ('g8', 'mx8', 'biasn', 'rs', 'm0', 'm1')]
                if own >= 4:
                    ps, pr = nps()
                    S.op("pe", lambda e, g8=g8, mx8=mx8, biasn=biasn, rs=rs, sm2=sm2, ps=ps, h=h: e.matmul(ps[:, 0:8], mqT[:, h, :], kmb[:, h, :], start=True, stop=True),
                         reads=["mqT", "kmb"], writes=[pr])
                    S.op("dve", lambda e, g8=g8, mx8=mx8, biasn=biasn, rs=rs, sm2=sm2: e.memset(g8[:], -1e30), writes=[g8r])
                    S.op("dve", lambda e, g8=g8, mx8=mx8, biasn=biasn, rs=rs, sm2=sm2, ps=ps, own=own: e.tensor_copy(g8[:, 0:own], ps[:, 0:own]),
                         reads=[pr], writes=[g8r])
                    S.op("dve", lambda e, g8=g8, mx8=mx8, biasn=biasn, rs=rs, sm2=sm2: e.max(mx8[:], g8[:]), reads=[g8r], writes=[mx8r])
                    S.op("dve", lambda e, g8=g8, mx8=mx8, biasn=biasn, rs=rs, sm2=sm2: e.tensor_scalar(biasn[:], g8[:], mx8[:, 2:3], 1.0, ALU.is_ge, ALU.subtract),
                         reads=[g8r, mx8r], writes=[biasnr])
                    S.op("dve", lambda e, g8=g8, mx8=mx8, biasn=biasn, rs=rs, sm2=sm2: e.tensor_scalar_mul(biasn[:], biasn[:], -NEG), reads=[biasnr], writes=[biasnr])
                else:
                    S.op("dve", lambda e, g8=g8, mx8=mx8, biasn=biasn, rs=rs, sm2=sm2: e.memset(biasn[:], 0.0), writes=[biasnr])
                S.op("dve", lambda e, g8=g8, mx8=mx8, biasn=biasn, rs=rs, sm2=sm2: e.memset(rs[:], 0.0), writes=[rsr])
                for n0 in range(0, own, 2):
                    nb = min(2, own - n0)
                    ps, pr = nps()
                    S.op("pe", lambda e, g8=g8, mx8=mx8, biasn=biasn, rs=rs, sm2=sm2, ps=ps, h=h, n0=n0, nb=nb: e.matmul(
                        ps[:, 0:nb * 256], mqT[:, h, :], KT[:, h, n0 * 256:(n0 + nb) * 256], start=True, stop=True),
                        reads=["mqT", "KT"], writes=[pr])
                    for j in range(nb):
                        n = n0 + j
                        S.op("act", lambda e, g8=g8, mx8=mx8, biasn=biasn, rs=rs, sm2=sm2, ps=ps, j=j, n=n: e.activation(
                            Pm[:, n * 256:(n + 1) * 256], ps[:, j * 256:(j + 1) * 256], AF.Exp,
                            bias=biasn[:, n:n + 1], scale=SCALE, accum_out=rs[:, n:n + 1]),
                            reads=[pr, biasnr, rsr], writes=["Pm", rsr])
                ps, pr = nps()
                k0 = own * 256
                nown = (t + 1) * 128 - k0
                S.op("pe", lambda e, g8=g8, mx8=mx8, biasn=biasn, rs=rs, sm2=sm2, ps=ps, h=h, k0=k0, nown=nown: e.matmul(
                    ps[:, 0:nown], mqT[:, h, :], KT[:, h, k0:k0 + nown], start=True, stop=True),
                    reads=["mqT", "KT"], writes=[pr])
                if nown == 256:
                    S.op("act", lambda e, g8=g8, mx8=mx8, biasn=biasn, rs=rs, sm2=sm2, ps=ps, k0=k0: e.activation(
                        Pm[:, k0:k0 + 128], ps[:, 0:128], AF.Exp, scale=SCALE, accum_out=rs[:, 8:9]),
                        reads=[pr, rsr], writes=["Pm", rsr])
                d0 = nown - 128
                S.op("dve", lambda e, g8=g8, mx8=mx8, biasn=biasn, rs=rs, sm2=sm2, ps=ps, d0=d0: e.tensor_tensor(sd[:], ps[:, d0:d0 + 128], tribias[:], ALU.add),
                     reads=[pr, "tribias"], writes=["sd"])
                S.op("act", lambda e, g8=g8, mx8=mx8, biasn=biasn, rs=rs, sm2=sm2, t=t: e.activation(Pm[:, t * 128:(t + 1) * 128], sd[:], AF.Exp, scale=SCALE,
                                                        accum_out=rs[:, 9:10]),
                     reads=["sd", rsr], writes=["Pm", rsr])
                S.op("dve", lambda e, g8=g8, mx8=mx8, biasn=biasn, rs=rs, sm2=sm2: e.reduce_sum(sm2[:, 0:1], rs[:, 0:10], AX.X), reads=[rsr], writes=[m0r])
                S.op("dve", lambda e, g8=g8, mx8=mx8, biasn=biasn, rs=rs, sm2=sm2: e.reciprocal(sm2[:, 1:2], sm2[:, 0:1]), reads=[m0r], writes=[m1r])
                S.op("dve", lambda e, g8=g8, mx8=mx8, biasn=biasn, rs=rs, sm2=sm2, nkt=nkt: e.tensor_scalar_mul(Pm[:, 0:nkt * 128], Pm[:, 0:nkt * 128], sm2[:, 1:2]),
                     reads=["Pm", m1r], writes=["Pm"])
                for k8 in range(0, nkt, 8):
                    nn = min(8, nkt - k8)
                    pb, pbr = npsb()
                    ptt = PT[(k8 // 8) % 2]
                    ptr = "PT%d" % ((k8 // 8) % 2)

                    def f(e, pb=pb, k8=k8, nn=nn):
                        for j in range(nn):
                            ins = e.transpose(pb[:, j * 128:(j + 1) * 128], Pm[:, (k8 + j) * 128:(k8 + j + 1) * 128],
                                              identb[:])
                        return ins
                    S.op("pe", f, reads=["Pm", "identb"], writes=[pbr])
                    S.op("dve", lambda e, g8=g8, mx8=mx8, biasn=biasn, rs=rs, sm2=sm2, pb=pb, nn=nn, ptt=ptt: e.tensor_copy(
                        ptt[:, 0:nn, :], pb[:, 0:nn * 128].rearrange("p (a b) -> p a b", a=nn)),
                        reads=[pbr], writes=[ptr])

                    def f2(e, ptt=ptt, k8=k8, nn=nn, h=h, nkt=nkt):
                        for j in range(nn):
                            kt = k8 + j
                            ins = e.matmul(psO[:, h * 128:(h + 1) * 128], Vb[:, kt, h * 128:(h + 1) * 128], ptt[:, j, :],
                                           start=(kt == 0), stop=(kt == nkt - 1))
                        return ins
                    S.op("pe", f2, reads=[ptr, "Vb"], writes=[psO_r])
            S.op("act", lambda e, g8=g8, mx8=mx8, biasn=biasn, rs=rs, sm2=sm2: e.copy(mixT[:, 4:8, :], psO[:, :].rearrange("p (h n) -> p h n", h=4)),
                 reads=[psO_r], writes=["mixT"])

            S.marks.append((S.seq, 'oproj %d' % t))
            pss = [nps(), nps()]
            for hf in range(2):
                def f(e, hf=hf, ps=pss[hf][0]):
                    for c in range(8):
                        ins = e.matmul(ps[:, :], mixT[:, c, :], w_o_b[:, c, hf * 512:(hf + 1) * 512],
                                       start=(c == 0), stop=(c == 7))
                    return ins
                S.op("pe", f, reads=["mixT", "w_o"], writes=[pss[hf][1]])
            layer_norm([pss[0][0], pss[1][0]], [pss[0][1], pss[1][1]], xt_t, xtr, gate_a, ["gate_a"], x1, "x1")
            S.dma("sp", lambda e, t=t: e.dma_start(out=x1d[t * 128:(t + 1) * 128, :], in_=x1[:]), reads=["x1"],
                  writes=["x1d%d" % t])


    load_tile(0)
    p1(0, 'A')
    p1(0, 'B')
    for t in range(NT):
        if t + 1 < NT:
            p1(t + 1, 'A')
        p1(t, 'C')
        if t + 1 < NT:
            p1(t + 1, 'B')

    for p_ in range(2):
        S.dma("sp", lambda e, p_=p_: e.dma_start(out=sout[p_ * 128:(p_ + 1) * 128, :], in_=st32[:, p_, :]),
              reads=["st32"])

    S.marks.append((S.seq, 'phase2'))
    P1 = WIN + ["w_o"]
    w_up_v = w_up.rearrange("(k p) n -> p k n", p=128)
    for k2 in range(4):
        ld("pool", w_up_b[:, 2 * k2:2 * k2 + 2, :], w_up_v[:, 2 * k2:2 * k2 + 2, :], ["w_up%d" % k2] + P1)
    WUP = ["w_up%d" % i for i in range(4)]
    w_down_v = w_down.rearrange("(k p) n -> p k n", p=128)
    for k4 in range(4):
        ld("pool", w_down_b[:, 8 * k4:8 * k4 + 8, :], w_down_v[:, 8 * k4:8 * k4 + 8, :],
           ["w_dn%d" % k4, "KT", "Vb", "wada0", "wada1"] + TAIL)
    WDN = ["w_dn%d" % i for i in range(4)]
    ld("sp", lng[:], l2g, ["lng"])
    ld("sp", lnb[:], l2b, ["lnb"])
    ld("sp", gate_a[:], gfd, ["gate_a"], reads=["gfd"])
    S.marks.append((S.seq, 'sample ffn'))
    ld("sp", xt[0:P, :], x1d[2048:2080, :], ["xt"], reads=["x1ds"])
    ld("sp", r[0:P, :], modsd[:, 4096:5120], ["r"], reads=["modsd"])
    ld("sp", xn[0:P, :], modsd[:, 3072:4096], ["xn"], reads=["modsd"])
    ld("sp", z[0:P, 2048:3072], modsd[:, 5120:6144], ["z4", "z5"], reads=["modsd"])
    S.op("dve", lambda e: e.scalar_tensor_tensor(r[0:P, :], r[0:P, :], 1.0, xt[0:P, :], ALU.add, ALU.mult),
         reads=["r", "xt"], writes=["r"])
    S.op("dve", lambda e: e.tensor_tensor(r[0:P, :], r[0:P, :], xn[0:P, :], ALU.add), reads=["r", "xn"], writes=["r"])
    tr32(lambda k: r[0:P, k * 128:(k + 1) * 128], 8, ["r"], hT[:, :, 0:32], "hT")
    for g in range(8):
        ps, pr = nps()

        def f(e, ps=ps, g=g):
            for j in range(4):
                fc = g * 4 + j
                for k in range(8):
                    ins = e.matmul(ps[:, j * 32:(j + 1) * 32], w_up_b[:, k, fc * 128:(fc + 1) * 128], hT[:, k, 0:32],
                                   start=(k == 0), stop=(k == 7))
            return ins
        S.op("pe", f, reads=["hT"] + WUP, writes=[pr])
        rr = rl[g % 2]
        rrr = "rl%d" % (g % 2)
        S.op("dve", lambda e, ps=ps, rr=rr: e.tensor_scalar_max(rr[:, 0:128], ps[:, 0:128], 0.0), reads=[pr], writes=[rrr])
        S.op("act", lambda e, rr=rr, g=g: e.activation(
            uT[:, 4 * g:4 * g + 4, 0:32], rr[:, 0:128].rearrange("p (a b) -> p a b", a=4), AF.Square),
            reads=[rrr], writes=["uT"])
    pss = [nps(), nps()]
    for hf in range(2):
        def f(e, hf=hf, ps=pss[hf][0]):
            for c in range(32):
                ins = e.matmul(ps[0:P, :], uT[:, c, 0:32], w_down_b[:, c, hf * 512:(hf + 1) * 512],
                               start=(c == 0), stop=(c == 31))
            return ins
        S.op("pe", f, reads=["uT"] + WDN, writes=[pss[hf][1]])
    layer_norm([pss[0][0], pss[1][0]], [pss[0][1], pss[1][1]], xt, "xt", z[:, 2048:3072], ["z4", "z5"], x1, "x1", P=P)
    S.dma("sp", lambda e: e.dma_start(out=ys, in_=x1[0:P, :]), reads=["x1"])
    def load_tile2(t):
        ld("sp", xts[t % 2][:], x1d[t * 128:(t + 1) * 128, :], ["xt%d" % (t % 2)], reads=["x1d%d" % t])

    pss2 = {}

    def p2(t, stg):
        xt_t, hT_t = xts[t % 2], hTs[t % 2]
        xtr, hTr = "xt%d" % (t % 2), "hT%d" % (t % 2)
        if stg == 'A':
            transposes_to(lambda b, n, hT_t=hT_t: hT_t[:, b, :], lambda k, xt_t=xt_t: xt_t[:, k * 128:(k + 1) * 128],
                          8, [xtr], [hTr], scale_fn=lambda k: (modT[:, 32 + k:33 + k], modT[:, 24 + k:25 + k]))
        if stg == 'B':
            if t + 1 < NT:
                load_tile2(t + 1)
            for g in range(8):
                ps, pr = nps()

                def f(e, ps=ps, g=g, hT_t=hT_t):
                    for j in range(4):
                        fc = g * 4 + j
                        for k in range(8):
                            ins = e.matmul(ps[:, j * 128:(j + 1) * 128], w_up_b[:, k, fc * 128:(fc + 1) * 128], hT_t[:, k, :],
                                           start=(k == 0), stop=(k == 7))
                    return ins
                S.op("pe", f, reads=[hTr] + WUP, writes=[pr])
                rr = rl[g % 2]
                rrr = "rl%d" % (g % 2)
                S.op("dve", lambda e, ps=ps, rr=rr: e.tensor_scalar_max(rr[:], ps[:, :], 0.0), reads=[pr], writes=[rrr])
                S.op("act", lambda e, rr=rr, g=g: e.activation(
                    uT[:, 4 * g:4 * g + 4, :], rr[:].rearrange("p (a b) -> p a b", a=4), AF.Square),
                    reads=[rrr], writes=["uT"])
            pss = [nps(), nps()]
            pss2[t] = pss
            for hf in range(2):
                def f(e, hf=hf, ps=pss[hf][0]):
                    for c in range(32):
                        ins = e.matmul(ps[:, :], uT[:, c, :], w_down_b[:, c, hf * 512:(hf + 1) * 512],
                                       start=(c == 0), stop=(c == 31))
                    return ins
                S.op("pe", f, reads=["uT"] + WDN, writes=[pss[hf][1]])
        if stg == 'C':
            pss = pss2[t]
            layer_norm([pss[0][0], pss[1][0]], [pss[0][1], pss[1][1]], xt_t, xtr, gate_a, ["gate_a"], x1, "x1")
            S.dma("sp", lambda e, t=t: e.dma_start(out=y[t * 128:(t + 1) * 128, :], in_=x1[:]), reads=["x1"])


    load_tile2(0)
    p2(0, 'A')
    p2(0, 'B')
    for t in range(NT):
        if t + 1 < NT:
            p2(t + 1, 'A')
        p2(t, 'C')
        if t + 1 < NT:
            p2(t + 1, 'B')

    S.marks.append((S.seq, 'end'))
    if marks is not None:
        marks.extend(S.marks)
    sems = {}
    for k in ("pe", "act", "dve", "pool"):
        sems[k] = nc.alloc_semaphore("s_" + k)
    for i in range(S.n_dma):
        sems["d%d" % i] = nc.alloc_semaphore("s_d%d" % i)
    with nc.Block() as block:
        @block.sync
        def _(e):
            S.emit("sp", e, sems, final_wait=True)

        @block.tensor
        def _(e):
            S.emit("pe", e, sems)

        @block.scalar
        def _(e):
            S.emit("act", e, sems)

        @block.vector
        def _(e):
            S.emit("dve", e, sems)

        @block.gpsimd
        def _(e):
            S.emit("pool", e, sems)
    return nc


_CONST = {}


def _consts():
    if _CONST:
        return _CONST
    pos = np.arange(2048, dtype=np.float32)

    def tab(half):
        inv = np.power(np.float32(10000.0), -np.arange(half, dtype=np.float32) / np.float32(half)).astype(np.float32)
        ang = (pos[:, None] * inv[None, :]).astype(np.float32)
        c, s = np.cos(ang).astype(np.float32), np.sin(ang).astype(np.float32)
        return np.ascontiguousarray(np.concatenate([c, -s, s], axis=1))
    _CONST["ropeM"] = tab(64)
    _CONST["ropeR"] = tab(32)
    i = np.arange(128, dtype=np.float64)
    dq = np.stack([np.power(GAM[h], i + 1.0) for h in range(4)], axis=1)
    dk = np.stack([np.power(GAM[h], -(i + 1.0)) / 8.0 for h in range(4)], axis=1)
    _CONST["dqk"] = np.ascontiguousarray(np.concatenate([dq, dk], axis=1).astype(np.float32))
    jj, ii = np.meshgrid(np.arange(128), np.arange(128), indexing="ij")
    _CONST["triT"] = (ii >= jj).astype(np.float32)
    _CONST["tribias"] = np.where(ii >= jj, 0.0, NEG).astype(np.float32).T.copy()
    _CONST["ident"] = np.eye(128, dtype=np.float32)
    return _CONST


def _sample_consts():
    if "ropeS" in _CONST:
        return _CONST
    pos = (8192 + (np.arange(32) % 8)).astype(np.float32)

    def tab(half):
        inv = np.power(np.float32(10000.0), -np.arange(half, dtype=np.float32) / np.float32(half)).astype(np.float32)
        ang = (pos[:, None] * inv[None, :]).astype(np.float32)
        c, s = np.cos(ang).astype(np.float32), np.sin(ang).astype(np.float32)
        return np.concatenate([c, -s, s], axis=1)
    _CONST["ropeS"] = np.ascontiguousarray(np.concatenate([tab(64), tab(32)], axis=1))
    i = (np.arange(32) % 8).astype(np.float64)
    dq = np.stack([np.power(GAM[h], i + 1.0) for h in range(4)], axis=1)
    dk = np.stack([np.power(GAM[h], -(i + 1.0)) / 8.0 for h in range(4)], axis=1)
    _CONST["dqks"] = np.ascontiguousarray(np.concatenate([dq, dk], axis=1).astype(np.float32))
    rr = np.arange(32)
    _CONST["maskS"] = ((rr[:, None] // 8 == rr[None, :] // 8) & (rr[None, :] >= rr[:, None])).astype(np.float32)
    cm = (rr[None, :] // 8 == np.arange(4)[:, None]).astype(np.float32).reshape(1, 128)
    _CONST["cm"] = np.ascontiguousarray(np.broadcast_to(cm, (128, 128)))
    _CONST["rm"] = (rr[:, None] // 8 == np.arange(4)[None, :]).astype(np.float32)
    q = np.arange(128)
    _CONST["gtab"] = np.array([[GAM[2 * a + (qq // 64)] ** 8 for a in range(2)] for qq in q], dtype=np.float32)
    cmn = np.zeros((32, 4, 4, 8), np.float32)
    for sp in range(4):
        for j in range(8):
            for qq in range(8):
                if j <= qq:
                    cmn[sp * 8 + j, sp, :, qq] = 1.0
    _CONST["cmaskN"] = cmn.reshape(32, 128)
    _CONST["pcol"] = (np.arange(128) % 64).astype(np.float32)[:, None].copy()
    return _CONST


def make_in_maps(x_prompt, x_sample, cache_k, cache_v, state_ret, page_table, c_prompt, c_sample,
                 w_ada, b_ada, w_in, w_o, ln1_g, ln1_b, w_up, w_down, ln2_g, ln2_b, cores=range(8)):
    f = lambda a: np.ascontiguousarray(np.asarray(a, dtype=np.float32))
    C = _consts()
    _sample_consts()
    b_ada0 = f(b_ada)[0]
    ckf = f(cache_k).reshape(-1, 1024)
    cvf = f(cache_v).reshape(-1, 1024)
    shared = {
        "w_ada": f(w_ada)[0], "badaT": np.ascontiguousarray(b_ada0.reshape(48, 128).T),
        "bga": np.ascontiguousarray(np.broadcast_to(b_ada0[2048:3072], (128, 1024))),
        "bgf": np.ascontiguousarray(np.broadcast_to(b_ada0[5120:6144], (128, 1024))),
        "bada32": np.ascontiguousarray(np.broadcast_to(b_ada0, (32, 6144))),
        "w_in": f(w_in)[0], "w_o": f(w_o)[0], "w_up": f(w_up)[0], "w_down": f(w_down)[0],
        "l1g": np.ascontiguousarray(np.broadcast_to(f(ln1_g)[0], (128, 1024))),
        "l1b": np.ascontiguousarray(np.broadcast_to(f(ln1_b)[0], (128, 1024))),
        "l2g": np.ascontiguousarray(np.broadcast_to(f(ln2_g)[0], (128, 1024))),
        "l2b": np.ascontiguousarray(np.broadcast_to(f(ln2_b)[0], (128, 1024))),
        "ck": ckf, "cv": cvf,
    }
    for k in ("ident", "ropeM", "ropeR", "dqk", "triT", "tribias", "ropeS", "dqks", "maskS", "cm", "rm", "gtab",
              "cmaskN", "pcol"):
        shared[k] = C[k]
    xp, xsm, cp, csm = f(x_prompt), f(x_sample), f(c_prompt), f(c_sample)
    st = f(state_ret)[0]
    pt = np.asarray(page_table).astype(np.int32)
    in_maps = []
    for c in cores:
        m = dict(shared)
        m["x"] = xp[c]
        m["cT"] = np.ascontiguousarray(cp[c].reshape(8, 128).T)
        m["xs"] = np.ascontiguousarray(xsm[4 * c:4 * c + 4].reshape(32, 1024))
        crow = np.repeat(csm[4 * c:4 * c + 4], 8, axis=0)
        m["csT"] = np.ascontiguousarray(crow.reshape(32, 8, 128).transpose(2, 1, 0).reshape(128, 256))
        pt4 = pt[4 * c:4 * c + 4].reshape(4, 32, 2)
        m["ptb"] = np.ascontiguousarray(np.concatenate(
            [np.broadcast_to(pt4[:, :, hh].reshape(1, 128), (64, 128)) for hh in range(2)], axis=0))
        m["st_in"] = np.ascontiguousarray(st[4 * c:4 * c + 4].reshape(4, 256, 128))
        in_maps.append(m)
    return in_maps


def kernel(x_prompt, x_sample, cache_k, cache_v, state_ret, page_table, c_prompt, c_sample,
           w_ada, b_ada, w_in, w_o, ln1_g, ln1_b, w_up, w_down, ln2_g, ln2_b):
    nc = build_nc()
    in_maps = make_in_maps(x_prompt, x_sample, cache_k, cache_v, state_ret, page_table, c_prompt, c_sample,
                           w_ada, b_ada, w_in, w_o, ln1_g, ln1_b, w_up, w_down, ln2_g, ln2_b)
    res = run_bass_kernel_spmd(nc, in_maps, core_ids=list(range(8)))
    R = res.results
    g = lambda c, k: np.asarray(R[c][k]).astype(np.float32)
    y_p = np.stack([g(c, "y") for c in range(8)])
    k_p = np.stack([g(c, "kout").reshape(2048, 4, 128) for c in range(8)])[None]
    v_p = np.stack([g(c, "vout").reshape(2048, 4, 128) for c in range(8)])[None]
    s_p = np.stack([g(c, "sout").reshape(4, 64, 128) for c in range(8)])[None]
    y_s = np.concatenate([g(c, "ys").reshape(4, 8, 1024) for c in range(8)])
    k_s = np.concatenate([g(c, "ks").reshape(4, 8, 4, 128) for c in range(8)])[None]
    v_s = np.concatenate([g(c, "vs").reshape(4, 8, 4, 128) for c in range(8)])[None]
    s_s = np.concatenate([g(c, "ss").reshape(4, 4, 64, 128) for c in range(8)])[None]
    return (y_p, y_s, k_p, v_p, s_p, k_s, v_s, s_s)
```
